# Optimizing a Trainium2 kernel written in Bass

```python
import math
import jax
import jax.numpy as jnp
from jax import lax
import numpy as np

D_MODEL = 1024
BATCH = 16
SEQ = 2048
DEPTH = 2

N_GROUPS = 4
GROUP_WIDTH = D_MODEL // N_GROUPS
HEADS = 4
HEAD_DIM = GROUP_WIDTH // HEADS
GDN_CONV = 4
GDN_CHUNK = 64
HGRN_CHUNK = 16
MOBA_BLOCK = 256
MOBA_TOPK = 3
MOBA_Q_CHUNK = 32
DIFF_DIM = HEAD_DIM // 2
ATTN_Q_BLOCK = 128
NORM_EPS = 1e-6
GDN_COLS = 4 * GROUP_WIDTH + 2 * HEADS
HGRN_COLS = 4 * GROUP_WIDTH
MOBA_COLS = 4 * GROUP_WIDTH
DIFF_COLS = 4 * GROUP_WIDTH
IN_COLS = GDN_COLS + HGRN_COLS + MOBA_COLS + DIFF_COLS

kernel_name = 'hybrid_parallel_heads_gdn_hgrn2_moba_diffattn'


def rmsnorm(x, g):
    xf = x.astype(jnp.float32)
    y = xf * lax.rsqrt(jnp.mean(xf * xf, axis=-1, keepdims=True) + NORM_EPS)
    return y * g.astype(jnp.float32)


def l2norm(x):
    return x * lax.rsqrt(jnp.sum(x * x, axis=-1, keepdims=True) + NORM_EPS)


def to_heads(t):
    B, T, _ = t.shape
    return t.reshape(B, T, HEADS, -1).transpose(0, 2, 1, 3)


def from_heads(t):
    B, H, T, d = t.shape
    return t.transpose(0, 2, 1, 3).reshape(B, T, H * d)


def causal_conv(x, w):
    K = w.shape[0]
    T = x.shape[1]
    xp = jnp.pad(x, ((0, 0), (K - 1, 0), (0, 0)))
    return sum(xp[:, j:j + T] * w[j] for j in range(K))


def gated_delta_rule(q, k, v, g, beta):
    B, H, T, dk = q.shape
    dv = v.shape[-1]
    C = GDN_CHUNK
    N = T // C

    def chunks(t):
        return t.reshape(B, H, N, C, *t.shape[3:])

    q = chunks(q * dk ** -0.5)
    k = chunks(k)
    v = chunks(v)
    beta = chunks(beta)
    g = jnp.cumsum(chunks(g), axis=-1)
    lower = jnp.tril(jnp.ones((C, C), dtype=bool))
    strict = jnp.tril(jnp.ones((C, C), dtype=bool), -1)
    decay = jnp.exp(jnp.where(lower, g[..., :, None] - g[..., None, :], -jnp.inf))
    k_beta = k * beta[..., None]
    a_kk = jnp.where(strict, jnp.einsum('bhncd,bhnsd->bhncs', k_beta, k) * decay, 0.0)
    eye = jnp.eye(C, dtype=q.dtype)
    t_inv = lax.linalg.triangular_solve(eye + a_kk, jnp.broadcast_to(eye, a_kk.shape),
                                        left_side=True, lower=True, unit_diagonal=True)
    u = t_inv @ (v * beta[..., None])
    w = t_inv @ (k_beta * jnp.exp(g)[..., None])
    a_qk = jnp.einsum('bhncd,bhnsd->bhncs', q, k) * decay
    q_dec = q * jnp.exp(g)[..., None]
    k_dec = k * jnp.exp(g[..., -1:] - g)[..., None]
    d_last = jnp.exp(g[..., -1])

    def step(S, inp):
        q_c, k_c, u_c, w_c, a_c, d_c = inp
        v_new = u_c - w_c @ S
        o = q_c @ S + a_c @ v_new
        S = S * d_c[..., None, None] + jnp.einsum('bhcd,bhce->bhde', k_c, v_new)
        return S, o

    xs = tuple(jnp.moveaxis(t, 2, 0) for t in (q_dec, k_dec, u, w, a_qk, d_last))
    _, o = lax.scan(step, jnp.zeros((B, H, dk, dv), q.dtype), xs)
    return jnp.moveaxis(o, 0, 2).reshape(B, H, T, dv)


def gdn_branch(p, conv_w, a_log, dt_bias, norm_g):
    W = GROUP_WIDTH
    qkv, a, b, z = jnp.split(p, [3 * W, 3 * W + HEADS, 3 * W + 2 * HEADS], axis=-1)
    qkv = jax.nn.silu(causal_conv(qkv, conv_w.astype(jnp.float32)))
    q, k, v = jnp.split(qkv, 3, axis=-1)
    q = l2norm(to_heads(q))
    k = l2norm(to_heads(k))
    v = to_heads(v)
    g = -jnp.exp(a_log.astype(jnp.float32)) * jax.nn.softplus(a + dt_bias.astype(jnp.float32))
    beta = jax.nn.sigmoid(b)
    o = gated_delta_rule(q, k, v, g.transpose(0, 2, 1), beta.transpose(0, 2, 1))
    return from_heads(rmsnorm(o, norm_g)) * jax.nn.silu(z)


def hgrn2_recurrence(q, k, v, log_f):
    B, H, T, dk = q.shape
    dv = v.shape[-1]
    C = HGRN_CHUNK
    N = T // C
    MID = C // 2

    def chunks(t):
        return t.reshape(B, H, N, C, t.shape[-1])

    q, k, v = chunks(q), chunks(k), chunks(v)
    G = jnp.cumsum(chunks(log_f), axis=3)
    G_ref = G[..., MID:MID + 1, :]
    causal = jnp.tril(jnp.ones((C, C), dtype=bool))
    a_qk = jnp.einsum('bhncd,bhnsd->bhncs', q * jnp.exp(G - G_ref), k * jnp.exp(G_ref - G))
    o_intra = jnp.where(causal, a_qk, 0.0) @ v
    q_dec = q * jnp.exp(G)
    k_dec = k * jnp.exp(G[..., -1:, :] - G)
    d_last = jnp.exp(G[..., -1, :])

    def step(S, inp):
        q_c, k_c, v_c, d_c, oi = inp
        o = q_c @ S + oi
        S = S * d_c[..., :, None] + jnp.einsum('bhcd,bhce->bhde', k_c, v_c)
        return S, o

    xs = tuple(jnp.moveaxis(t, 2, 0) for t in (q_dec, k_dec, v, d_last, o_intra))
    _, o = lax.scan(step, jnp.zeros((B, H, dk, dv), q.dtype), xs)
    return jnp.moveaxis(o, 0, 2).reshape(B, H, T, dv)


def hgrn2_branch(p, lb, norm_g):
    q, f, i, z = jnp.split(p, 4, axis=-1)
    log_f = jnp.logaddexp(jnp.log(lb), jnp.log1p(-lb) + jax.nn.log_sigmoid(f))
    k = (1.0 - lb) * jax.nn.sigmoid(-f)
    q = jax.nn.silu(q)
    o = hgrn2_recurrence(to_heads(q), to_heads(k), to_heads(i), to_heads(log_f))
    return from_heads(rmsnorm(o, norm_g)) * jax.nn.silu(z)


def moba_attention(q, k, v, slopes):
    B, H, T, d = q.shape
    Tp = -(-T // MOBA_BLOCK) * MOBA_BLOCK
    pad = ((0, 0), (0, 0), (0, Tp - T), (0, 0))
    q, k, v = jnp.pad(q, pad), jnp.pad(k, pad), jnp.pad(v, pad)
    nb = Tp // MOBA_BLOCK
    kk = min(MOBA_TOPK, nb)
    scale = d ** -0.5
    kb = k.reshape(B, H, nb, MOBA_BLOCK, d)
    vb = v.reshape(B, H, nb, MOBA_BLOCK, d)
    gate = jnp.einsum('bhtd,bhnd->bhtn', q, kb.mean(axis=3))
    past = jnp.arange(nb)[None, :] < (jnp.arange(Tp) // MOBA_BLOCK)[:, None]
    _, sel = lax.top_k(jnp.where(past, gate, -jnp.inf), kk)
    bi = jnp.arange(B)[:, None, None, None]
    hi = jnp.arange(H)[None, :, None, None]
    blk = jnp.arange(MOBA_BLOCK)

    def chunk_fn(c):
        t0 = c * MOBA_Q_CHUNK
        qc = lax.dynamic_slice_in_dim(q, t0, MOBA_Q_CHUNK, axis=2)
        sel_c = lax.dynamic_slice_in_dim(sel, t0, MOBA_Q_CHUNK, axis=2)
        pos_q = t0 + jnp.arange(MOBA_Q_CHUNK)
        own = t0 // MOBA_BLOCK
        k_sel = kb[bi, hi, sel_c]
        v_sel = vb[bi, hi, sel_c]
        s_sel = jnp.einsum('bhqd,bhqnsd->bhqns', qc, k_sel) * scale
        dist_sel = pos_q[None, None, :, None, None] - (sel_c[..., None] * MOBA_BLOCK + blk)
        s_sel = s_sel - slopes[None, :, None, None, None] * dist_sel
        s_sel = jnp.where((sel_c < own)[..., None], s_sel, -jnp.inf)
        k_own = lax.dynamic_slice_in_dim(kb, own, 1, axis=2)[:, :, 0]
        v_own = lax.dynamic_slice_in_dim(vb, own, 1, axis=2)[:, :, 0]
        dist_own = pos_q[:, None] - (own * MOBA_BLOCK + blk)[None, :]
        s_own = jnp.einsum('bhqd,bhsd->bhqs', qc, k_own) * scale - slopes[None, :, None, None] * dist_own
        s_own = jnp.where(dist_own >= 0, s_own, -jnp.inf)
        logits = jnp.concatenate([s_sel.reshape(B, H, MOBA_Q_CHUNK, kk * MOBA_BLOCK), s_own], axis=-1)
        pr = jax.nn.softmax(logits.astype(jnp.float32), axis=-1)
        p_sel = pr[..., :kk * MOBA_BLOCK].reshape(B, H, MOBA_Q_CHUNK, kk, MOBA_BLOCK)
        p_own = pr[..., kk * MOBA_BLOCK:]
        return (jnp.einsum('bhqns,bhqnsd->bhqd', p_sel, v_sel)
                + jnp.einsum('bhqs,bhsd->bhqd', p_own, v_own))

    outs = lax.map(chunk_fn, jnp.arange(Tp // MOBA_Q_CHUNK))
    return jnp.moveaxis(outs, 0, 2).reshape(B, H, Tp, d)[:, :, :T]


def moba_branch(p, slopes):
    q, k, v, z = jnp.split(p, 4, axis=-1)
    o = moba_attention(to_heads(q), to_heads(k), to_heads(v), slopes)
    return from_heads(o) * jax.nn.silu(z)


def differential_attention(q1, q2, k1, k2, v, lam, slopes):
    T, d = q1.shape[2], q1.shape[3]
    scale = d ** -0.5
    outs = []
    for s in range(0, T, ATTN_Q_BLOCK):
        e = s + ATTN_Q_BLOCK
        dist = jnp.arange(s, e)[:, None] - jnp.arange(e)[None, :]
        bias = jnp.where(dist >= 0, -slopes[:, None, None] * dist, -jnp.inf)
        a1 = jax.nn.softmax(jnp.einsum('bhqd,bhkd->bhqk', q1[:, :, s:e], k1[:, :, :e]) * scale + bias, axis=-1)
        a2 = jax.nn.softmax(jnp.einsum('bhqd,bhkd->bhqk', q2[:, :, s:e], k2[:, :, :e]) * scale + bias, axis=-1)
        outs.append(jnp.einsum('bhqk,bhkd->bhqd', a1 - lam * a2, v[:, :, :e]))
    return jnp.concatenate(outs, axis=2)


def diff_branch(p, lq1, lk1, lq2, lk2, norm_g, lam_init, slopes):
    B, T, _ = p.shape
    q, k, v, z = jnp.split(p, 4, axis=-1)
    q = q.reshape(B, T, HEADS, 2, DIFF_DIM).transpose(3, 0, 2, 1, 4)
    k = k.reshape(B, T, HEADS, 2, DIFF_DIM).transpose(3, 0, 2, 1, 4)
    f32 = jnp.float32
    lam = (jnp.exp(jnp.dot(lq1.astype(f32), lk1.astype(f32)))
           - jnp.exp(jnp.dot(lq2.astype(f32), lk2.astype(f32))) + lam_init)
    o = differential_attention(q[0], q[1], k[0], k[1], to_heads(v), lam, slopes)
    o = rmsnorm(o, norm_g) * (1.0 - lam_init)
    return from_heads(o) * jax.nn.silu(z)


def setup_inputs(seed: int = 0) -> dict:
    key = jax.random.key(seed)
    ks = jax.random.split(key, 16)
    f32 = jnp.float32

    def nrm(k, shape, s):
        return s * jax.random.normal(k, shape, f32)

    def gain(k, shape):
        return 1.0 + nrm(k, shape, 0.02)

    dt = jnp.exp(jax.random.uniform(ks[5], (DEPTH, HEADS), f32, math.log(1e-3), math.log(1e-1)))
    return {
        'x': nrm(ks[0], (BATCH, SEQ, D_MODEL), 1.0),
        'pre_norm_g': gain(ks[1], (DEPTH, D_MODEL)),
        'post_norm_g': gain(ks[2], (DEPTH, D_MODEL)),
        'w_in': nrm(ks[3], (DEPTH, D_MODEL, IN_COLS), D_MODEL ** -0.5),
        'conv_w': nrm(ks[4], (DEPTH, GDN_CONV, 3 * GROUP_WIDTH), GDN_CONV ** -0.5),
        'gdn_a_log': jnp.log(jax.random.uniform(ks[6], (DEPTH, HEADS), f32, 1.0, 16.0)),
        'gdn_dt_bias': dt + jnp.log(-jnp.expm1(-dt)),
        'gdn_norm_g': gain(ks[7], (DEPTH, HEAD_DIM)),
        'hgrn_lb': nrm(ks[8], (DEPTH, GROUP_WIDTH), 0.1),
        'hgrn_norm_g': gain(ks[9], (DEPTH, HEAD_DIM)),
        'diff_lq1': nrm(ks[10], (DEPTH, DIFF_DIM), 0.1),
        'diff_lk1': nrm(ks[11], (DEPTH, DIFF_DIM), 0.1),
        'diff_lq2': nrm(ks[12], (DEPTH, DIFF_DIM), 0.1),
        'diff_lk2': nrm(ks[13], (DEPTH, DIFF_DIM), 0.1),
        'diff_norm_g': gain(ks[14], (DEPTH, HEAD_DIM)),
        'w_out': nrm(ks[15], (DEPTH, D_MODEL, D_MODEL), D_MODEL ** -0.5),
    }


def reference(x, pre_norm_g, post_norm_g, w_in, conv_w, gdn_a_log, gdn_dt_bias, gdn_norm_g,
              hgrn_lb, hgrn_norm_g, diff_lq1, diff_lk1, diff_lq2, diff_lk2, diff_norm_g, w_out):
    f32 = jnp.float32
    alibi = 2.0 ** (-jnp.arange(1, 2 * HEADS + 1, dtype=f32))
    slopes_diff = alibi[0::2]
    slopes_moba = alibi[1::2]
    lb_all = jnp.cumsum(jax.nn.softmax(hgrn_lb.astype(f32), axis=0), axis=0)
    lb_all = lb_all - lb_all[0]
    splits = [GDN_COLS, GDN_COLS + HGRN_COLS, GDN_COLS + HGRN_COLS + MOBA_COLS]
    for l in range(DEPTH):
        h = rmsnorm(x, pre_norm_g[l])
        proj = jnp.einsum('btd,dc->btc', h, w_in[l].astype(f32))
        p_gdn, p_hgrn, p_moba, p_diff = jnp.split(proj, splits, axis=-1)
        lam_init = 0.8 - 0.6 * math.exp(-0.3 * l)
        y = jnp.concatenate([
            gdn_branch(p_gdn, conv_w[l], gdn_a_log[l], gdn_dt_bias[l], gdn_norm_g[l]),
            hgrn2_branch(p_hgrn, lb_all[l], hgrn_norm_g[l]),
            moba_branch(p_moba, slopes_moba),
            diff_branch(p_diff, diff_lq1[l], diff_lk1[l], diff_lq2[l], diff_lk2[l], diff_norm_g[l],
                        lam_init, slopes_diff),
        ], axis=-1)
        y = jnp.einsum('btc,cd->btd', y, w_out[l].astype(f32))
        x = x + rmsnorm(y, post_norm_g[l]).astype(x.dtype)
    return x
```

```python
import math
import numpy as np
import concourse.bass as bass
import concourse.mybir as mybir
from concourse.bass_utils import run_bass_kernel_spmd

F32 = mybir.dt.float32
BF16 = mybir.dt.bfloat16
AF = mybir.ActivationFunctionType
ALU = mybir.AluOpType
AX = mybir.AxisListType

D_MODEL = 1024
HD = 64
EPS = 1e-6
BIG = 30000.0
N_CORES = 8


class View:
    def __init__(self, buf, ap):
        self.buf = buf
        self.ap = ap

    def __getitem__(self, k):
        return View(self.buf, self.ap[k])

    def rearrange(self, *a, **k):
        return View(self.buf, self.ap.rearrange(*a, **k))

    def to_broadcast(self, *a, **k):
        return View(self.buf, self.ap.to_broadcast(*a, **k))

    def unsqueeze(self, *a, **k):
        return View(self.buf, self.ap.unsqueeze(*a, **k))

    def bitcast(self, *a, **k):
        return View(self.buf, self.ap.bitcast(*a, **k))


class Buf:
    def __init__(self, t, name):
        self.t = t
        self.name = name
        self.last_w = None
        self.reads = {}
        self.dma_sem = None
        self.is_psum = False

    def __getitem__(self, k):
        return View(self, self.t[k])


class Prog:
    ENG = ("pe", "act", "dve", "pool", "sp")

    def __init__(self, nc):
        self.nc = nc
        self.streams = {e: [] for e in self.ENG}
        self.sems = {}
        self.cnt = {}
        self.waited = {e: {} for e in self.ENG}
        self.marks = []
        for e in ("pe", "act", "dve", "pool"):
            self.sems[e] = nc.alloc_semaphore("s_" + e)
            self.cnt[e] = 0

    def mark(self, label):
        self.marks.append((label, dict(self.cnt)))

    def sb(self, name, shape, dtype=F32):
        return Buf(self.nc.alloc_sbuf_tensor(name, list(shape), dtype), name)

    def ps(self, name, shape, dtype=F32):
        b = Buf(self.nc.alloc_psum_tensor(name, list(shape), dtype), name)
        b.is_psum = True
        return b

    def dram(self, ap, name):
        return Buf(ap, name)

    def _deps(self, eng, reads, writes):
        need = {}

        def add(k, c):
            if k == "pe" and eng == "pe":
                return
            if need.get(k, 0) < c:
                need[k] = c
        for b in reads:
            if b.last_w is not None:
                add(*b.last_w)
            if b.is_psum:
                for k, c in b.reads.items():
                    if k != eng:
                        add(k, c)
        for b in writes:
            if b.last_w is not None:
                add(*b.last_w)
            for k, c in b.reads.items():
                add(k, c)
        out = []
        w = self.waited[eng]
        for k, c in need.items():
            if w.get(k, 0) < c:
                w[k] = c
                out.append((k, c))
        return out

    def op(self, eng, fn, reads=(), writes=()):
        reads = list(dict.fromkeys(reads))
        writes = list(dict.fromkeys(writes))
        waits = self._deps(eng, reads, writes)
        self.cnt[eng] += 1
        c = self.cnt[eng]
        self.streams[eng].append((waits, fn, (eng, 1)))
        for b in reads:
            b.reads[eng] = c
        for b in writes:
            b.last_w = (eng, c)
            b.reads = {}

    def dma(self, out, in_, q="sp"):
        ob, ib = out.buf, in_.buf
        owner = ob if ob.dma_sem is not None else (ib if ib.dma_sem is not None else ob)
        if owner.dma_sem is None:
            key = "dma%d" % len(self.sems)
            self.sems[key] = self.nc.alloc_semaphore(key)
            self.cnt[key] = 0
            owner.dma_sem = key
        key = owner.dma_sem
        waits = self._deps(q, [ib], [ob])
        self.cnt[key] += 16
        c = self.cnt[key]
        oa, ia = out.ap, in_.ap
        self.streams[q].append((waits, lambda e: e.dma_start(out=oa, in_=ia), (key, 16)))
        ib.reads[key] = c
        ob.last_w = (key, c)
        ob.reads = {}

    def final_wait(self, eng, bufs):
        waits = self._deps(eng, bufs, bufs)
        self.streams[eng].append((waits, None, None))

    def emit(self):
        nc = self.nc
        with nc.Block() as block:
            def mk(ename):
                def body(e):
                    for waits, fn, inc in self.streams[ename]:
                        for k, c in waits:
                            e.wait_ge(self.sems[k], c)
                        if fn is None:
                            continue
                        ins = fn(e)
                        ins.then_inc(self.sems[inc[0]], inc[1])
                return body
            if self.streams["pe"]:
                block.tensor(mk("pe"))
            if self.streams["act"]:
                block.scalar(mk("act"))
            if self.streams["dve"]:
                block.vector(mk("dve"))
            if self.streams["pool"]:
                block.gpsimd(mk("pool"))
            if self.streams["sp"]:
                block.sync(mk("sp"))

    def mm(self, out, lhsT, rhs, start=True, stop=True):
        o, l, r = out.ap, lhsT.ap, rhs.ap
        self.op("pe", lambda e: e.matmul(o, l, r, start=start, stop=stop),
                reads=[lhsT.buf, rhs.buf], writes=[out.buf])

    def tr(self, out, in_, ident):
        o, i, d = out.ap, in_.ap, ident.ap
        self.op("pe", lambda e: e.transpose(o, i, d), reads=[in_.buf, ident.buf], writes=[out.buf])

    def act(self, out, in_, func, bias=None, scale=1.0, eng="act"):
        o, i = out.ap, in_.ap
        rd = [in_.buf]
        kw = {}
        if isinstance(bias, View):
            rd.append(bias.buf)
            kw["bias"] = bias.ap
        elif bias is not None:
            kw["bias"] = float(bias)
        if isinstance(scale, View):
            rd.append(scale.buf)
            kw["scale"] = scale.ap
        else:
            kw["scale"] = float(scale)
        self.op("act", lambda e: e.activation(o, i, func, **kw), reads=rd, writes=[out.buf])

    def tt(self, out, in0, in1, op, eng="dve"):
        o, a, b = out.ap, in0.ap, in1.ap
        self.op(eng, lambda e: e.tensor_tensor(o, a, b, op), reads=[in0.buf, in1.buf], writes=[out.buf])

    def ts(self, out, in0, s1, s2, op0, op1=None, eng="dve"):
        o, a = out.ap, in0.ap
        rd = [in0.buf]
        v1 = s1
        if isinstance(s1, View):
            rd.append(s1.buf)
            v1 = s1.ap
        v2 = s2
        if isinstance(s2, View):
            rd.append(s2.buf)
            v2 = s2.ap
        if op1 is None:
            self.op(eng, lambda e: e.tensor_scalar(o, a, v1, None, op0), reads=rd, writes=[out.buf])
        else:
            self.op(eng, lambda e: e.tensor_scalar(o, a, v1, v2, op0, op1), reads=rd, writes=[out.buf])

    def stt(self, out, in0, scalar, in1, op0, op1, eng="dve"):
        o, a, b = out.ap, in0.ap, in1.ap
        rd = [in0.buf, in1.buf]
        sv = scalar
        if isinstance(scalar, View):
            rd.append(scalar.buf)
            sv = scalar.ap
        self.op(eng, lambda e: e.scalar_tensor_tensor(o, a, sv, b, op0, op1), reads=rd, writes=[out.buf])

    def cp(self, out, in_, eng="dve"):
        o, i = out.ap, in_.ap
        if eng == "act":
            self.op("act", lambda e: e.copy(o, i), reads=[in_.buf], writes=[out.buf])
        else:
            self.op(eng, lambda e: e.tensor_copy(o, i), reads=[in_.buf], writes=[out.buf])

    def memset(self, out, val, eng="dve"):
        o = out.ap
        self.op(eng, lambda e: e.memset(o, val), writes=[out.buf])

    def red(self, out, in_, op, eng="dve"):
        o, i = out.ap, in_.ap
        self.op(eng, lambda e: e.tensor_reduce(o, i, AX.X, op), reads=[in_.buf], writes=[out.buf])

    def recip(self, out, in_):
        o, i = out.ap, in_.ap
        self.op("dve", lambda e: e.reciprocal(o, i), reads=[in_.buf], writes=[out.buf])

    def scan(self, out, d0, d1):
        o, a, b = out.ap, d0.ap, d1.ap
        self.op("dve", lambda e: e.tensor_tensor_scan(o, a, b, 0.0, ALU.mult, ALU.add),
                reads=[d0.buf, d1.buf], writes=[out.buf])


C32 = {}
_o = 0
for _n, _w in (("ident", 128), ("blk64", 128), ("blktri", 128), ("scanm", 512), ("selden", 64), ("place", 256), ("e2", 2), ("rm", 2), ("arel", 16)):
    C32[_n] = (_o, _w)
    _o += _w
NC32 = _o
C16 = {}
_o = 0
for _n, _w in (("ident", 128), ("lvl", 6 * 128), ("caus01", 128), ("causneg", 128), ("hm2", 2), ("hm4", 4),
               ("ka", 16 * 128), ("oh", 16 * 128), ("qab", 512), ("m01", 128), ("m01T", 128)):
    C16[_n] = (_o, _w)
    _o += _w
NC16 = _o


def make_consts():
    c32 = np.zeros((128, NC32), np.float32)
    c16 = np.zeros((128, NC16), np.float32)
    p = np.arange(128)[:, None]
    c = np.arange(128)[None, :]
    same = (p // 64) == (c // 64)

    def put(dst, tab, name, arr):
        o, w = tab[name]
        dst[:, o:o + w] = arr
    put(c32, C32, "ident", (p == c).astype(np.float32))
    put(c32, C32, "blk64", same.astype(np.float32))
    put(c32, C32, "blktri", (same & (p <= c)).astype(np.float32))
    sm = np.ones((128, 512), np.float32)
    sm[:, ::64] = 0.0
    put(c32, C32, "scanm", sm)
    sd = np.zeros((128, 64), np.float32)
    sd[64, :] = 1.0
    put(c32, C32, "selden", sd)
    pl = np.zeros((128, 256), np.float32)
    for r in range(64):
        pl[r, r] = 1.0
        pl[r, 128 + 64 + r] = 1.0
    put(c32, C32, "place", pl)
    e2 = np.zeros((128, 2), np.float32)
    e2[0, 0] = 1.0
    e2[64, 1] = 1.0
    put(c32, C32, "e2", e2)
    arel = np.zeros((128, 16), np.float32)
    for r in range(16):
        arel[:, r] = np.arange(128) + 128.0 * (r - 12) - 256.0
    put(c32, C32, "arel", arel)
    rm = np.zeros((128, 2), np.float32)
    rm[:64, 0] = 1.0
    rm[64:, 1] = 1.0
    put(c32, C32, "rm", rm)

    put(c16, C16, "ident", (p == c).astype(np.float32))
    lv = np.zeros((128, 6, 128), np.float32)
    for l in range(1, 7):
        s = 2 ** l
        lv[:, l - 1, :] = ((p // s) == (c // s)) & ((p % s) >= s // 2) & ((c % s) < s // 2)
    put(c16, C16, "lvl", lv.reshape(128, 768))
    put(c16, C16, "m01", (same & (p >= c)).astype(np.float32))
    put(c16, C16, "m01T", (same & (c >= p)).astype(np.float32))
    put(c16, C16, "caus01", (p <= c).astype(np.float32))
    put(c16, C16, "causneg", np.where(p > c, -BIG, 0.0))
    hm2 = np.zeros((128, 2), np.float32)
    hm2[:64, 0] = 1.0
    hm2[64:, 1] = 1.0
    put(c16, C16, "hm2", hm2)
    hm4 = np.zeros((128, 4), np.float32)
    for g in range(4):
        hm4[g * 32:(g + 1) * 32, g] = 1.0
    put(c16, C16, "hm4", hm4)
    ka = np.zeros((128, 16, 128), np.float32)
    for ri in range(16):
        ka[0, ri, :] = np.arange(128)
        ka[1, ri, :] = 1.0
        ka[2, ri, :] = 128.0 * (ri - 12)
        ka[3, ri, :] = 1.0
    put(c16, C16, "ka", ka.reshape(128, 2048))
    oh = np.zeros((128, 16, 128), np.float32)
    for r in range(16):
        oh[r, r, :] = 1.0
    put(c16, C16, "oh", oh.reshape(128, 2048))
    qab = np.zeros((128, 512), np.float32)
    il = np.arange(512)
    qab[0, :] = 1.0
    qab[1, :] = -(il % 128)
    qab[2, :] = 1.0
    qab[3, :] = -128.0 * (il // 128)
    put(c16, C16, "qab", qab)
    return c32, c16


PAR = {}
_o = 0
for _n, _w in (("preg", 8), ("postg", 8), ("conv", 24), ("gdng", 1), ("hgrng", 1), ("diffg", 1),
               ("lbl", 2), ("lb0", 2), ("alog", 4), ("dtb", 4), ("lq1", 32), ("lk1", 32), ("lq2", 32), ("lk2", 32)):
    PAR[_n] = (_o, _w)
    _o += _w
NPAR = _o

CMB = {}
_i = 0
for _br, _names in (("gdn", ("q", "k", "v", "z")), ("hgrn", ("q", "f", "z")), ("moba", ("q", "k", "z")),
                    ("diff", ("q", "k", "z"))):
    for _nm in _names:
        for _p in range(2):
            CMB[(_br, _nm, _p)] = _i
            _i += 1
for _br in ("gdn", "hgrn", "moba", "diff"):
    for _p in range(2):
        CMB[(_br, "tm", _p)] = _i
        _i += 1
for _d in range(8):
    CMB[("out", "w", _d)] = _i
    _i += 1
NWB = _i

GDN_BASE, HGRN_BASE, MOBA_BASE, DIFF_BASE = 0, 1032, 2056, 3080


def pack_weights(w_in, w_out):
    depth = w_in.shape[0]
    out = np.zeros((depth, NWB, 128, 1024), np.float32)

    def blockify(wcols):
        return wcols.reshape(8, 128, 128).transpose(1, 0, 2).reshape(128, 1024)
    for l in range(depth):
        W = w_in[l]
        def cols(base, off, p):
            return W[:, base + off + p * 128: base + off + (p + 1) * 128]
        for p in range(2):
            out[l, CMB[("gdn", "q", p)]] = blockify(cols(GDN_BASE, 0, p))
            out[l, CMB[("gdn", "k", p)]] = blockify(cols(GDN_BASE, 256, p))
            out[l, CMB[("gdn", "v", p)]] = blockify(cols(GDN_BASE, 512, p))
            out[l, CMB[("gdn", "z", p)]] = blockify(cols(GDN_BASE, 776, p))
            ab = np.zeros((1024, 128), np.float32)
            ab[:, 0:2] = W[:, GDN_BASE + 768 + 2 * p: GDN_BASE + 768 + 2 * p + 2]
            ab[:, 2:4] = W[:, GDN_BASE + 772 + 2 * p: GDN_BASE + 772 + 2 * p + 2]
            out[l, CMB[("gdn", "tm", p)]] = blockify(ab)
            out[l, CMB[("hgrn", "q", p)]] = blockify(cols(HGRN_BASE, 0, p))
            out[l, CMB[("hgrn", "f", p)]] = blockify(cols(HGRN_BASE, 256, p))
            out[l, CMB[("hgrn", "tm", p)]] = blockify(cols(HGRN_BASE, 512, p))
            out[l, CMB[("hgrn", "z", p)]] = blockify(cols(HGRN_BASE, 768, p))
            for br, base in (("moba", MOBA_BASE), ("diff", DIFF_BASE)):
                out[l, CMB[(br, "q", p)]] = blockify(cols(base, 0, p))
                out[l, CMB[(br, "k", p)]] = blockify(cols(base, 256, p))
                out[l, CMB[(br, "tm", p)]] = blockify(cols(base, 512, p))
                out[l, CMB[(br, "z", p)]] = blockify(cols(base, 768, p))
        for d in range(8):
            out[l, CMB[("out", "w", d)]] = blockify(w_out[l][:, d * 128:(d + 1) * 128])
    return out


def pack_params(inp):
    depth = inp["pre_norm_g"].shape[0]
    par = np.zeros((depth, 128, NPAR), np.float32)

    def put(l, name, arr):
        o, w = PAR[name]
        par[l, :, o:o + w] = arr
    for l in range(depth):
        put(l, "preg", inp["pre_norm_g"][l].reshape(8, 128).T)
        put(l, "postg", inp["post_norm_g"][l].reshape(8, 128).T)
        cw = inp["conv_w"][l]
        put(l, "conv", cw.reshape(4, 6, 128).transpose(2, 1, 0).reshape(128, 24))
        put(l, "gdng", np.tile(inp["gdn_norm_g"][l], 2)[:, None])
        put(l, "hgrng", np.tile(inp["hgrn_norm_g"][l], 2)[:, None])
        put(l, "diffg", np.tile(inp["diff_norm_g"][l], 2)[:, None])
        put(l, "lbl", inp["hgrn_lb"][l].reshape(2, 128).T)
        put(l, "lb0", inp["hgrn_lb"][0].reshape(2, 128).T)
        put(l, "alog", np.broadcast_to(inp["gdn_a_log"][l][None, :], (128, 4)))
        put(l, "dtb", np.broadcast_to(inp["gdn_dt_bias"][l][None, :], (128, 4)))
        put(l, "lq1", np.broadcast_to(inp["diff_lq1"][l][None, :], (128, 32)))
        put(l, "lk1", np.broadcast_to(inp["diff_lk1"][l][None, :], (128, 32)))
        put(l, "lq2", np.broadcast_to(inp["diff_lq2"][l][None, :], (128, 32)))
        put(l, "lk2", np.broadcast_to(inp["diff_lk2"][l][None, :], (128, 32)))
    return par


def build(T=2048, NSEQ=2, DEPTH=2, branches=("gdn", "hgrn", "moba", "diff"), tap=False):
    assert T % 512 == 0
    NT = T // 128
    NB = T // 512
    nc = bass.Bass("TRN2", target_bir_lowering=False)
    x_d = nc.dram_tensor("x", [NSEQ, 128, 8 * T], F32, kind="ExternalInput").ap()
    w_d = nc.dram_tensor("w", [DEPTH, NWB, 128, 1024], F32, kind="ExternalInput").ap()
    c32_d = nc.dram_tensor("c32", [128, NC32], F32, kind="ExternalInput").ap()
    c16_d = nc.dram_tensor("c16", [128, NC16], F32, kind="ExternalInput").ap()
    par_d = nc.dram_tensor("par", [DEPTH, 128, NPAR], F32, kind="ExternalInput").ap()
    y_d = nc.dram_tensor("y", [NSEQ, 128, 8 * T], F32, kind="ExternalOutput").ap()
    if tap:
        tap_d = nc.dram_tensor("tap", [NSEQ, DEPTH, 128, 8 * T], F32, kind="ExternalOutput").ap()

    P = Prog(nc)
    xD, wD, c32D, c16D, parD, yD = (P.dram(x_d, "x"), P.dram(w_d, "w"), P.dram(c32_d, "c32"),
                                    P.dram(c16_d, "c16"), P.dram(par_d, "par"), P.dram(y_d, "y"))
    tapD = P.dram(tap_d, "tap") if tap else None
    XT = P.sb("XT", [128, 8, T])
    HT = P.sb("HT", [128, 8, T], BF16)
    Y = P.sb("Y", [128, 8, T], BF16)
    K32 = P.sb("K32", [128, NC32])
    K16 = P.sb("K16", [128, NC16], BF16)
    PARS = [P.sb("PAR%d" % l, [128, NPAR]) for l in range(DEPTH)]
    NWS = 4
    WS = [P.sb("WS%d" % i, [128, 8, 128], BF16) for i in range(NWS)]
    NF, NH = 7, 10
    F = [P.sb("F%d" % i, [128, 512]) for i in range(NF)]
    H = [P.sb("H%d" % i, [128, 512], BF16) for i in range(NH)]
    SMB = [P.sb("SMB%d" % i, [128, 256], BF16) for i in range(29)]
    TTS = [P.sb("TT%d" % i, [128, 512], BF16) for i in range(2)]
    SMF = [P.sb("SMF%d" % i, [128, 256]) for i in range(2)]
    KT = P.sb("KT", [128, T], BF16)
    VB = P.sb("VB", [128, max(NT * 2 * 65, 2080)], BF16)
    KTs = P.sb("KTs", [128, max(T, 1040)], BF16) if T < 1040 else None
    TOK = [P.sb("TOK%d" % i, [128, NT * 4 if i == 0 else NT * 2]) for i in range(10)]
    SS = P.sb("SS", [128, 128])
    SSB = P.sb("SSB", [128, 128], BF16)
    SSB2 = P.sb("SSB2", [128, 128], BF16)
    COL = P.sb("COL", [128, 16])
    KS = P.sb("KS", [128, 2, 8])
    KSr = P.sb("KSr", [128, 8])
    DL = P.sb("DL", [128, NT * 2])
    ABI = P.sb("ABI", [128, 2, 16])
    PS = [P.ps("PS%d" % i, [128, 512]) for i in range(8)]

    def k32(name):
        o, w = C32[name]
        return K32[:, o:o + w]

    def k16(name):
        o, w = C16[name]
        return K16[:, o:o + w]

    P.dma(K32[:, :], c32D[:, :])
    P.dma(K16[:, :], c16D[:, :], q="pool")
    for l in range(DEPTH):
        P.dma(PARS[l][:, :], parD[l])

    wstate = {"next_slot": 0}

    def load_w(l, key):
        s = WS[wstate["next_slot"] % NWS]
        wstate["next_slot"] += 1
        P.dma(s[:, :, :], wD[l, CMB[key]].rearrange("p (k c) -> p k c", k=8), q="pool")
        return s

    def par(l, name, j=0, w=1):
        o, _ = PAR[name]
        return PARS[l][:, o + j:o + j + w]

    def rsqrt_from(out, in_, scale, eps):
        P.act(out, in_, AF.Ln, bias=eps, scale=scale)
        P.act(out, out, AF.Exp, scale=-0.5)

    def proj_cm(ps, wslot, src, cols):
        for k in range(8):
            P.mm(ps[:, :], wslot[:, k, :], src[:, k, cols], start=(k == 0), stop=(k == 7))

    def head_norm_gate(l, ops, gcol, yv, extra=1.0):
        osb, sq, rs = F[4], F[5], F[6]
        P.cp(osb[:, :], ops[:, :], eng="act")
        P.act(sq[:, :], ops[:, :], AF.Square)
        P.mm(PS[7][:, :], k32("blk64"), sq[:, :])
        rsqrt_from(rs[:, :], PS[7][:, :], 1.0 / HD, EPS)
        P.stt(osb[:, :], osb[:, :], gcol, rs[:, :], ALU.mult, ALU.mult)
        if extra != 1.0:
            P.stt(yv, osb[:, :], float(extra), yv, ALU.mult, ALU.mult)
        else:
            P.tt(yv, osb[:, :], yv, ALU.mult)

    def z_gate(l, wz, yblk):
        for n in range(NB):
            cols = slice(n * 512, (n + 1) * 512)
            ps = PS[5 + (n % 2)]
            proj_cm(ps, wz, HT, cols)
            P.act(Y[:, yblk, cols], ps[:, :], AF.Silu)

    def prenorm(l):
        for n in range(NB):
            cols = slice(n * 512, (n + 1) * 512)
            for k in range(8):
                sq = H[k % 2]
                P.act(sq[:, :], XT[:, k, cols], AF.Square)
                P.mm(PS[n % 2][:, :], ONESB[:, :], sq[:, :], start=(k == 0), stop=(k == 7))
            rs = F[2 + (n % 2)]
            rsqrt_from(rs[:, :], PS[n % 2][:, :], 1.0 / D_MODEL, EPS)
            for k in range(8):
                P.stt(HT[:, k, cols], XT[:, k, cols], par(l, "preg", k), rs[:, :], ALU.mult, ALU.mult)

    def attention(l, p, kind):
        yblk = (4 if kind == "moba" else 6) + p
        slopes = [2.0 ** -(2 * h + 2) for h in range(4)] if kind == "moba" else [2.0 ** -(2 * h + 1) for h in range(4)]
        dh = 64 if kind == "moba" else 32
        scale = dh ** -0.5
        wq = load_w(l, (kind, "q", p))
        wk = load_w(l, (kind, "k", p))
        wz = load_w(l, (kind, "z", p))
        wv = load_w(l, (kind, "tm", p))
        z_gate(l, wz, yblk)
        if kind == "moba":
            P.memset(KSr[:, :], 0.0)
        for n in range(NB):
            cols = slice(n * 512, (n + 1) * 512)
            ps = PS[5 + (n % 2)]
            proj_cm(ps, wk, HT, cols)
            P.cp(KT[:, cols], ps[:, :], eng="act")
            if kind == "moba":
                P.cp(F[3][:, :], ps[:, :])
                P.red(KSr[:, 2 * n:2 * n + 2], F[3][:, :].rearrange("p (b c) -> p b c", c=256), ALU.add)
        if kind == "moba":
            for hp in range(2):
                P.ts(KS[:, hp, :], KSr[:, :], k32("rm")[:, hp:hp + 1], None, ALU.mult)
        VA = VB[:, 0:NT * 2 * 65].rearrange("p (t h c) -> p t h c", t=NT, h=2)
        P.memset(VA[:, :, :, 64:65], 1.0)
        for t in range(NT):
            ps = PS[5 + (t % 2)]
            for k in range(8):
                P.mm(ps[:, 0:128], HT[:, k, t * 128:(t + 1) * 128], wv[:, k, :], start=(k == 0), stop=(k == 7))
            P.cp(VA[:, t, :, 0:64], ps[:, 0:128].rearrange("p (h c) -> p h c", h=2), eng="act")
        if kind == "diff":
            lam_init = 0.8 - 0.6 * math.exp(-0.3 * l)
            tmp = SMF[0]
            P.tt(tmp[:, 0:32], par(l, "lq1", 0, 32), par(l, "lk1", 0, 32), ALU.mult)
            P.red(COL[:, 0:1], tmp[:, 0:32], ALU.add)
            P.tt(tmp[:, 32:64], par(l, "lq2", 0, 32), par(l, "lk2", 0, 32), ALU.mult)
            P.red(COL[:, 1:2], tmp[:, 32:64], ALU.add)
            P.act(COL[:, 2:4], COL[:, 0:2], AF.Exp)
            P.tt(COL[:, 4:5], COL[:, 3:4], COL[:, 2:3], ALU.subtract)
            P.ts(COL[:, 5:6], COL[:, 4:5], -lam_init, None, ALU.add)
            P.ts(COL[:, 6:7], par(l, "diffg"), 1.0 - lam_init, None, ALU.mult)
        nmask = 2 if kind == "moba" else 4
        mask_tab = k16("hm2") if kind == "moba" else k16("hm4")
        QZ = [H[0], H[1], H[2], H[3]]
        QA = [H[4], H[5]]
        PT = [H[6], H[7], H[8]]
        OC = [F[0], F[1]]
        RC = [F[2], F[3]]
        for b_ in OC:
            P.memset(b_[:, :], 0.0)
        for hp_, b_ in enumerate(QA):
            P.memset(b_[:, :], 0.0)
            P.ts(b_[0:4, :], k16("qab")[0:4, :], float(slopes[2 * p + hp_]), None, ALU.mult)
            P.ts(ABI[:, hp_, :], k32("arel"), float(slopes[2 * p + hp_]), None, ALU.mult)
        bias_mode = [slopes[2 * p + hp_] <= 2.0 ** -4 for hp_ in range(2)]
        SELT = H[9]
        P.memset(SELT[:, :], 0.0)
        nmap = 1 if kind == "moba" else 2
        GF = [SMB[i][:, :].bitcast(F32) for i in range(6)]

        def block_begin(n):
            cols = slice(n * 512, (n + 1) * 512)
            psq = PS[5]
            proj_cm(psq, wq, HT, cols)
            for m in range(nmask):
                P.stt(QZ[m][:, :], psq[:, :], float(scale), mask_tab[:, m:m + 1].to_broadcast([128, 512]),
                      ALU.mult, ALU.mult)
            if not (kind == "moba" and n >= 2):
                return
            q32 = F[1]
            P.cp(q32[:, :], psq[:, :], eng="act")
            W = 2 * n + 1
            psg = PS[7]
            for t in range(4):
                for hp in range(2):
                    P.mm(psg[:, (t * 2 + hp) * 8:(t * 2 + hp) * 8 + 8], q32[:, t * 128:(t + 1) * 128], KS[:, hp, :])
            g = GF[0]
            P.cp(g[:, 0:64], psg[:, 0:64])
            g3 = g[:, 0:64].rearrange("p (a b) -> p a b", b=8)
            P.memset(g3[:, 0:4, 2 * n:2 * n + 1], -1e30)
            cur = g3[:, :, 0:W]
            m_ = GF[1]
            for it in range(3):
                P.red(m_[:, 8 * it:8 * it + 8], cur, ALU.max)
                if it == 2:
                    break
                e_ = GF[2 + it]
                e3 = e_[:, 0:64].rearrange("p (a b) -> p a b", b=8)[:, :, 0:W]
                P.tt(e3, cur, m_[:, 8 * it:8 * it + 8].unsqueeze(2).to_broadcast([128, 8, W]), ALU.is_ge)
                P.stt(e3, e3, -1e30, cur, ALU.mult, ALU.add)
                cur = e3
            mv = GF[4]
            P.memset(mv[:, 0:64], 0.0)
            mv3 = mv[:, 0:64].rearrange("p (a b) -> p a b", b=8)
            P.tt(mv3[:, :, 0:W], g3[:, :, 0:W], m_[:, 16:24].unsqueeze(2).to_broadcast([128, 8, W]), ALU.is_ge)
            P.ts(mv3[:, :, 0:W], mv3[:, :, 0:W], BIG, -BIG, ALU.mult, ALU.add)
            P.memset(mv3[:, 0:4, 2 * n:2 * n + 1], 0.0)
            pst = PS[7]
            for t in range(4):
                P.tr(pst[0:16, t * 128:(t + 1) * 128], mv[:, t * 16:(t + 1) * 16], k32("ident"))
            P.cp(SELT[0:16, :], pst[0:16, :])

        jobs = [(n, hp, mp, jt) for n in range(NB) for hp in range(2) for mp in range(nmap) for jt in range(4 * n + 4)]
        deferred = []

        def emitA(job, gi):
            n, hp, mp, jt = job
            if hp == 0 and mp == 0 and jt == 0:
                block_begin(n)
            use_sel = (kind == "moba" and n >= 2)
            m = hp * nmap + mp if kind == "diff" else hp
            qa = QA[hp]
            c0 = max(0, jt - 4 * n) * 128
            sc = PS[gi % 3]
            pt = PT[gi % 3]
            blk = jt // 2
            masked = use_sel and blk <= 2 * n
            diag = jt >= 4 * n
            bm = bias_mode[hp]
            P.mm(sc[:, c0:512], KT[:, jt * 128:(jt + 1) * 128], QZ[m][:, c0:512], start=True,
                 stop=bm and not (masked or diag))
            if not bm:
                ka = k16("ka")[:, (jt - 4 * n + 12) * 128:(jt - 4 * n + 13) * 128]
                P.mm(sc[:, c0:512], ka, qa[:, c0:512], start=False, stop=not (masked or diag))
            if masked:
                oh = k16("oh")[:, (hp * 8 + blk) * 128:(hp * 8 + blk + 1) * 128]
                P.mm(sc[:, c0:512], oh, SELT[:, c0:512], start=False, stop=not diag)
            if diag:
                P.mm(sc[:, c0:c0 + 128], k16("ident"), k16("causneg"), start=False, stop=True)
            if bm:
                r_ = jt - 4 * n + 12
                P.act(pt[:, c0:512], sc[:, c0:512], AF.Exp, bias=ABI[:, hp, r_:r_ + 1])
            else:
                P.act(pt[:, c0:512], sc[:, c0:512], AF.Exp)

        def emitB(job, gi):
            n, hp, mp, jt = job
            njt = 4 * n + 4
            cols = slice(n * 512, (n + 1) * 512)
            c0 = max(0, jt - 4 * n) * 128
            pt = PT[gi % 3]
            acc = PS[3 + mp]
            P.mm(acc[0:65, c0:512], VA[:, jt, hp, :], pt[:, c0:512], start=(jt == 0), stop=(jt == njt - 1))
            if jt == njt - 1:
                oc = OC[mp]
                P.cp(oc[0:65, :], acc[0:65, :], eng="act")

                def f2(oc=oc, hp=hp, mp=mp, cols=cols):
                    P.mm(PS[7][0:64, :], k32("selden"), oc[:, :])
                    rc = RC[mp]
                    P.act(rc[0:64, :], PS[7][0:64, :], AF.Ln)
                    P.act(rc[0:64, :], rc[0:64, :], AF.Exp, scale=-1.0)
                    P.tt(oc[0:64, :], oc[0:64, :], rc[0:64, :], ALU.mult)
                    if mp == nmap - 1:
                        if kind == "diff":
                            P.stt(OC[0][0:64, :], OC[1][0:64, :], COL[0:64, 5:6], OC[0][0:64, :], ALU.mult, ALU.add)

                        def f5(hp=hp, cols=cols):
                            P.mm(PS[6][:, :], k32("place")[:, hp * 128:(hp + 1) * 128], OC[0][:, :],
                                 start=(hp == 0), stop=(hp == 1))
                            if hp == 1:
                                if kind == "moba":
                                    P.tt(Y[:, yblk, cols], PS[6][:, :], Y[:, yblk, cols], ALU.mult)
                                else:
                                    head_norm_gate(l, PS[6], COL[:, 6:7], Y[:, yblk, cols])
                        deferred.append([2, f5])
                deferred.append([2, f2])

        def run_deferred(flush=False):
            i = 0
            while i < len(deferred):
                deferred[i][0] -= 1
                if flush or deferred[i][0] <= 0:
                    fn = deferred.pop(i)[1]
                    fn()
                else:
                    i += 1

        LOOK = 3
        for i in range(min(LOOK, len(jobs))):
            emitA(jobs[i], i)
        for i in range(len(jobs)):
            emitB(jobs[i], i)
            if i + LOOK < len(jobs):
                emitA(jobs[i + LOOK], i + LOOK)
            run_deferred()
        while deferred:
            run_deferred(flush=True)

    def hgrn(l, p):
        yblk = 2 + p
        wq = load_w(l, ("hgrn", "q", p))
        wf = load_w(l, ("hgrn", "f", p))
        wz = load_w(l, ("hgrn", "z", p))
        wv = load_w(l, ("hgrn", "tm", p))
        z_gate(l, wz, yblk)
        VH = VB[:, 0:NT * 128].rearrange("p (t c) -> p t c", t=NT)
        for t in range(NT):
            ps = PS[5 + (t % 2)]
            for k in range(8):
                P.mm(ps[:, 0:128], HT[:, k, t * 128:(t + 1) * 128], wv[:, k, :], start=(k == 0), stop=(k == 7))
            P.cp(VH[:, t, :], ps[:, 0:128], eng="act")
        if l == 0:
            P.memset(COL[:, 8:9], 0.0)
        else:
            P.tt(COL[:, 8:9], par(l, "lbl", p), par(l, "lb0", p), ALU.subtract)
            P.act(COL[:, 8:9], COL[:, 8:9], AF.Sigmoid)
        P.ts(COL[:, 9:10], COL[:, 8:9], -1.0, 1.0, ALU.mult, ALU.add)
        P.memset(SS[:, :], 0.0)
        P.memset(SSB[:, :], 0.0)
        P.memset(SSB2[:, :], 0.0)
        SSBs = [SSB, SSB2]
        KTb = KT if KTs is None else KTs
        E = KTb[:, 0:1024].bitcast(F32)
        QS, FG, G, DG = F[0], F[1], F[2], F[3]
        HSET = [(H[0], H[1], H[2], H[3]), (H[4], H[5], H[6], H[7])]
        DLHs = [SMF[0], SMF[1]]
        TSETS = [SMB[5 * i:5 * i + 5] for i in range(3)]
        for st_ in TSETS:
            P.memset(st_[0][:, :], 0.0)
            P.memset(st_[1][:, :], 0.0)

        def block_prep(n):
            cols = slice(n * 512, (n + 1) * 512)
            QG, KG, QD, KDT = HSET[n % 2]
            DLH = DLHs[n % 2]
            proj_cm(PS[5], wq, HT, cols)
            P.act(QS[:, :], PS[5][:, :], AF.Silu)
            yield
            proj_cm(PS[6], wf, HT, cols)
            P.act(FG[:, :], PS[6][:, :], AF.Sigmoid)
            yield
            P.ts(FG[:, :], FG[:, :], COL[:, 9:10], COL[:, 8:9], ALU.mult, ALU.add)
            yield
            P.act(E, FG[:, :], AF.Ln)
            yield
            P.scan(G[:, :], k32("scanm"), E)
            P.ts(FG[:, :], FG[:, :], -1.0, 1.0, ALU.mult, ALU.add)
            yield
            G3 = G[:, :].rearrange("p (n c) -> p n c", c=64)
            DG3 = DG[:, :].rearrange("p (n c) -> p n c", c=64)
            P.tt(DG3, G3, G3[:, :, 32:33].to_broadcast([128, 8, 64]), ALU.subtract)
            yield
            P.act(E, DG[:, :], AF.Exp)
            yield
            P.tt(QG[:, :], QS[:, :], E, ALU.mult)
            yield
            P.act(E, DG[:, :], AF.Exp, scale=-1.0)
            yield
            P.tt(KG[:, :], FG[:, :], E, ALU.mult)
            yield
            P.act(E, G[:, :], AF.Exp)
            yield
            P.tt(QD[:, :], QS[:, :], E, ALU.mult)
            P.tt(DG3, G3[:, :, 63:64].to_broadcast([128, 8, 64]), G3, ALU.subtract)
            yield
            P.act(E, DG[:, :], AF.Exp)
            yield
            P.tt(KDT[:, :], FG[:, :], E, ALU.mult)
            P.act(DLH[:, 0:8], G3[:, :, 63], AF.Exp)
            yield

        def tile_pre(n, t):
            gt = 4 * n + t
            tc = slice(t * 128, (t + 1) * 128)
            ATM, VZ, KD0_, KD1_, QGZ = TSETS[gt % 3]
            KD = [KD0_, KD1_]
            QG, KG, QD, KDT = HSET[n % 2]
            pst = PS[7]
            psa = PS[gt % 3]
            pss = PS[gt % 3]
            P.tr(pst[:, 0:64].bitcast(BF16), KDT[:, tc], k16("ident"))
            P.tt(QGZ[:, :].rearrange("p (h c) -> p h c", h=2),
                 QG[:, tc].unsqueeze(1).to_broadcast([128, 2, 128]),
                 k16("hm2").unsqueeze(2).to_broadcast([128, 2, 128]), ALU.mult)
            for hp in range(2):
                P.cp(VZ[:, hp * 128 + hp * 64: hp * 128 + hp * 64 + 64], VH[:, gt, hp * 64:(hp + 1) * 64], eng="act")
            for hf in range(2):
                P.ts(KD[hf][:, 0:128], pst[:, 0:64].bitcast(BF16), k32("rm")[:, hf:hf + 1], None, ALU.mult)
            yield
            for hf in range(2):
                M = 64 if hf == 0 else 128
                for hp in range(2):
                    P.mm(psa[0:M, hp * 128 + hf * 64: hp * 128 + hf * 64 + 64],
                         KG[:, t * 128:t * 128 + M], QGZ[:, hp * 128 + hf * 64: hp * 128 + hf * 64 + 64])
            for hf in range(2):
                for hp in range(2):
                    P.mm(pss[:, 256 + hf * 128:256 + (hf + 1) * 128], KD[hf][:, 0:128], VZ[:, hp * 128:(hp + 1) * 128],
                         start=(hp == 0), stop=(hp == 1))
            yield
            A3 = ATM[:, :].rearrange("p (h c) -> p h c", h=2)
            for hf in range(2):
                ic = slice(hf * 64, hf * 64 + 64)
                rows = slice(hf * 64, hf * 64 + 64)
                P.tt(A3[rows, :, ic], psa[:, 0:256].rearrange("p (h c) -> p h c", h=2)[rows, :, ic],
                     k16("caus01")[rows, ic].unsqueeze(1).to_broadcast([64, 2, 64]), ALU.mult)
            yield

        def tile_chain(n, t, pso):
            gt = 4 * n + t
            ATM, VZ, KD0_, KD1_, QGZ = TSETS[gt % 3]
            QG, KG, QD, KDT = HSET[n % 2]
            DLH = DLHs[n % 2]
            pss = PS[gt % 3]
            A3 = ATM[:, :].rearrange("p (h c) -> p h c", h=2)
            for hf in range(2):
                ic = slice(hf * 64, hf * 64 + 64)
                oc = slice(t * 128 + hf * 64, t * 128 + hf * 64 + 64)
                cg = gt * 2 + hf
                P.mm(pso[:, oc], SSBs[cg % 2][:, :], QD[:, oc], start=True, stop=False)
                for hp in range(2):
                    P.mm(pso[:, oc], VZ[:, hp * 128:(hp + 1) * 128], A3[:, hp, ic], start=False, stop=(hp == 1))
                yield
                ch = t * 2 + hf
                for hp in range(2):
                    r = slice(hp * 64, hp * 64 + 64)
                    P.stt(SS[r, r], SS[r, r], DLH[r, ch:ch + 1], pss[r, 256 + hf * 128 + hp * 64:256 + hf * 128 + hp * 64 + 64], ALU.mult, ALU.add)
                yield
                P.cp(SSBs[(cg + 1) % 2][:, :], SS[:, :], eng="act")
                yield
            if t == 3:
                cols = slice(n * 512, (n + 1) * 512)
                head_norm_gate(l, pso, par(l, "hgrng"), Y[:, yblk, cols])

        def step(g_):
            try:
                next(g_)
                return True
            except StopIteration:
                return False

        pso = PS[4]
        for _ in block_prep(0):
            pass
        tiles = [(n, t) for n in range(NB) for t in range(4)]
        for _ in tile_pre(*tiles[0]):
            pass
        prev = None
        bg = block_prep(1) if NB > 1 else None
        bg_n = 1
        for i, (n, t) in enumerate(tiles):
            nxt = tiles[i + 1] if i + 1 < len(tiles) else None
            if nxt is not None and nxt[1] == 0:
                if bg is not None:
                    for _ in bg:
                        pass
                    bg = None
            cur = tile_pre(*nxt) if nxt is not None else None
            alive = cur is not None
            while alive or prev is not None:
                if alive:
                    alive = step(cur)
                if prev is not None and not step(prev):
                    prev = None
                if bg is not None:
                    if not step(bg):
                        bg = None
                    elif not step(bg):
                        bg = None
            if nxt is not None and nxt[1] == 0 and nxt[0] + 1 < NB:
                bg = block_prep(nxt[0] + 1)
            prev = tile_chain(n, t, pso)
        for _ in prev:
            pass

    def gdn(l, p):
        yblk = p
        wz = load_w(l, ("gdn", "z", p))
        wab = load_w(l, ("gdn", "tm", p))
        wq = load_w(l, ("gdn", "q", p))
        wk = load_w(l, ("gdn", "k", p))
        z_gate(l, wz, yblk)
        AB, GR, BETA, G, GL, EG, BEG, EGLG, NEGG, TMP = TOK
        psab = PS[7]
        for t in range(NT):
            for k in range(8):
                P.mm(psab[:, t * 4:t * 4 + 4], HT[:, k, t * 128:(t + 1) * 128], wab[:, k, 0:4], start=(k == 0), stop=(k == 7))
        P.cp(AB[:, :], psab[:, 0:NT * 4])
        wv = load_w(l, ("gdn", "v", p))
        AB3 = AB[:, :].rearrange("p (t c) -> p t c", c=4)
        GR3 = GR[:, 0:NT * 2].rearrange("p (t c) -> p t c", c=2)
        dtb = par(l, "dtb", 2 * p, 2).unsqueeze(1).to_broadcast([128, NT, 2])
        P.tt(GR3, AB3[:, :, 0:2], dtb, ALU.add)
        T3 = TMP[:, 0:NT * 2].rearrange("p (t c) -> p t c", c=2)
        P.ts(T3, GR3, -30.0, 0.0, ALU.add, ALU.max)
        P.ts(GR3, GR3, 30.0, None, ALU.min)
        P.act(GR[:, 0:NT * 2], GR[:, 0:NT * 2], AF.Exp)
        P.act(GR[:, 0:NT * 2], GR[:, 0:NT * 2], AF.Ln, bias=1.0)
        P.tt(GR3, GR3, T3, ALU.add)
        P.act(COL[:, 10:12], par(l, "alog", 2 * p, 2), AF.Exp)
        P.ts(COL[:, 10:12], COL[:, 10:12], -1.0, None, ALU.mult)
        P.tt(GR3, GR3, COL[:, 10:12].unsqueeze(1).to_broadcast([128, NT, 2]), ALU.mult)
        B3 = BETA[:, 0:NT * 2].rearrange("p (t c) -> p t c", c=2)
        P.act(B3, AB3[:, :, 2:4], AF.Sigmoid)
        n2 = NT * 2
        P.mm(PS[7][:, 0:n2], k32("blktri"), GR[:, 0:n2])
        P.cp(G[:, 0:n2], PS[7][:, 0:n2])
        P.mm(PS[6][:, 0:n2], k32("blk64"), GR[:, 0:n2])
        P.cp(GL[:, 0:n2], PS[6][:, 0:n2])
        P.act(EG[:, 0:n2], G[:, 0:n2], AF.Exp)
        P.tt(BEG[:, 0:n2], EG[:, 0:n2], BETA[:, 0:n2], ALU.mult)
        P.tt(EGLG[:, 0:n2], GL[:, 0:n2], G[:, 0:n2], ALU.subtract)
        P.act(EGLG[:, 0:n2], EGLG[:, 0:n2], AF.Exp)
        P.ts(NEGG[:, 0:n2], G[:, 0:n2], -1.0, None, ALU.mult)
        REP = SMF[0]
        psd = PS[6]
        for t in range(NT):
            P.cp(REP[:, 0:128].rearrange("p (h c) -> p h c", h=2),
                 GL[:, t * 2:t * 2 + 2].unsqueeze(2).to_broadcast([128, 2, 64]))
            P.mm(psd[:, 256 + t * 2:256 + t * 2 + 2], REP[:, 0:128], k32("e2"))
        P.act(DL[:, 0:n2], psd[:, 256:256 + n2], AF.Exp)
        P.memset(SS[:, :], 0.0)
        P.memset(SSB[:, :], 0.0)
        VNZ = SMB[0]
        P.memset(VNZ[:, :], 0.0)
        KBGZ = SMB[1]
        P.memset(KBGZ[:, :], 0.0)
        KD0 = SMB[2]
        KTb = KT if KTs is None else KTs
        PC = [VB[:, 0:515], VB[:, 520:1035], KTb[:, 0:1030].bitcast(F32)]
        for b_ in PC:
            P.memset(b_[:, 0:3], 0.0)
        DIAG = []
        for bi_ in range(2):
            for j_ in range(4):
                dst_ = wab[:, bi_ * 4 + j_, :]
                P.ts(dst_, k16("ident"), par(l, "conv", (bi_ * 2 + p) * 4 + j_), None, ALU.mult, eng="pool")
                DIAG.append(dst_)
        ACC, CQ, SQ, RS = F[0], F[1], F[2], F[3]
        QN, KN, VT = H[0], H[1], H[2]
        convw = lambda blk, j: par(l, "conv", (blk * 2 + p) * 4 + j)
        hmb = k16("hm2").unsqueeze(2).to_broadcast([128, 2, 128])
        idb = k16("ident").unsqueeze(1).to_broadcast([128, 2, 128])

        def v3(x):
            return x[:, :].rearrange("p (h c) -> p h c", h=2)

        def prepinv(n, t, si):
            gt = 4 * n + t
            tc = slice(t * 128, (t + 1) * 128)
            KNZ, QNZ, D, DT, A, AQT, QD, AL, IZ, TQ, EGB = SMB[3 + 11 * si: 3 + 11 * si + 11]
            Tm, Tt = TTS[si][:, 0:256], TTS[si][:, 256:512]
            REPg = SMF[si]
            psGZ, psK, psT = PS[0 + si], PS[2 + si], PS[5 + si]
            P.tt(v3(KNZ), KN[:, tc].unsqueeze(1).to_broadcast([128, 2, 128]), hmb, ALU.mult)
            P.tt(v3(QNZ), QN[:, tc].unsqueeze(1).to_broadcast([128, 2, 128]), hmb, ALU.mult)
            P.cp(v3(REPg), GR[:, gt * 2:gt * 2 + 2].unsqueeze(2).to_broadcast([128, 2, 128]))
            yield
            for hp in range(2):
                hs = slice(hp * 128, (hp + 1) * 128)
                P.mm(psGZ[:, hs], REPg[:, hs], k32("blktri"))
                P.mm(psK[:, hs], KN[:, tc], KNZ[:, hs])
                P.mm(psK[:, 256 + hp * 128:256 + (hp + 1) * 128], KN[:, tc], QNZ[:, hs])
            yield
            for hp in range(2):
                hs = slice(hp * 128, (hp + 1) * 128)
                P.act(D[:, hs], psGZ[:, hs], AF.Exp, bias=G[:, gt * 2 + hp:gt * 2 + hp + 1], scale=-1.0)
                P.act(DT[:, hs], psGZ[:, hs], AF.Exp, bias=NEGG[:, gt * 2 + hp:gt * 2 + hp + 1], scale=1.0)
            P.act(EGB[:, :], psGZ[:, 0:256], AF.Exp)
            yield
            P.stt(v3(D), v3(D), 1e30, k16("m01").unsqueeze(1).to_broadcast([128, 2, 128]), ALU.min, ALU.mult)
            P.stt(v3(DT), v3(DT), 1e30, k16("m01T").unsqueeze(1).to_broadcast([128, 2, 128]), ALU.min, ALU.mult)
            yield
            for hp in range(2):
                hs = slice(hp * 128, (hp + 1) * 128)
                P.stt(A[:, hs], psK[:, hs], BETA[:, gt * 2 + hp:gt * 2 + hp + 1], D[:, hs], ALU.mult, ALU.mult)
            P.tt(AQT[:, :], psK[:, 256:512], DT[:, :], ALU.mult)
            P.tt(TQ[:, :], QNZ[:, :], EGB[:, :], ALU.mult)
            P.tt(QD[:, 0:128], TQ[:, 0:128], TQ[:, 128:256], ALU.add)
            yield
            for lv in range(6):
                lvm = k16("lvl")[:, lv * 128:(lv + 1) * 128].unsqueeze(1).to_broadcast([128, 2, 128])
                P.tt(v3(AL), v3(A), lvm, ALU.mult)
                yield
                if lv == 0:
                    P.stt(v3(Tm), v3(AL), -1.0, idb, ALU.mult, ALU.add)
                    for hp in range(2):
                        hs = slice(hp * 128, (hp + 1) * 128)
                        P.mm(psGZ[:, 256 + hp * 128:256 + (hp + 1) * 128], AL[:, hs], k16("ident"))
                    yield
                    P.stt(v3(Tt), psGZ[:, 256:512].rearrange("p (h c) -> p h c", h=2), -1.0, idb, ALU.mult, ALU.add)
                    yield
                    continue
                for hp in range(2):
                    hs = slice(hp * 128, (hp + 1) * 128)
                    P.mm(psGZ[:, 256 + hp * 128:256 + (hp + 1) * 128], AL[:, hs], Tt[:, hs])
                yield
                P.stt(v3(IZ), psGZ[:, 256:512].rearrange("p (h c) -> p h c", h=2), -1.0, idb, ALU.mult, ALU.add)
                yield
                for hp in range(2):
                    hs = slice(hp * 128, (hp + 1) * 128)
                    if lv < 5:
                        P.mm(psT[:, hs], IZ[:, hs], Tm[:, hs])
                    P.mm(psT[:, 256 + hp * 128:256 + (hp + 1) * 128], Tm[:, hs], IZ[:, hs])
                yield
                if lv < 5:
                    P.cp(TTS[si][:, :], psT[:, :], eng="act")
                else:
                    P.cp(Tt[:, :], psT[:, 256:512], eng="act")
                yield

        WT2 = [H[6], H[7]]
        U2 = [F[4], F[5]]
        KDs = [[KD0, H[3]], [H[8], H[9]]]
        AQT2 = [SMB[25], SMB[26]]
        QD2 = [SMB[27], SMB[28]]

        def post(n, t, si):
            gt = 4 * n + t
            tc = slice(t * 128, (t + 1) * 128)
            KNZ, QNZ, D, DT, A, AQT, QD, AL, IZ, TQ, EGB = SMB[3 + 11 * si: 3 + 11 * si + 11]
            Tm, Tt = TTS[si][:, 0:256], TTS[si][:, 256:512]
            pst = PS[2 + si]
            P.tr(pst[:, 0:64].bitcast(BF16), KN[:, tc], k16("ident"))
            P.tr(pst[:, 64:128].bitcast(BF16), VT[:, tc], k16("ident"))
            kt_ = pst[:, 0:64].bitcast(BF16)
            vt_ = pst[:, 64:128].bitcast(BF16)
            KD = KDs[si]
            VBt = H[4]
            KDall = H[5]
            P.cp(AQT2[si][:, :], AQT[:, :], eng="pool")
            P.cp(QD2[si][:, 0:128], QD[:, 0:128], eng="pool")
            yield
            for hp in range(2):
                cs = slice(hp * 64, (hp + 1) * 64)
                P.ts(KBGZ[:, hp * 128 + hp * 64: hp * 128 + hp * 64 + 64], kt_[:, cs], BEG[:, gt * 2 + hp:gt * 2 + hp + 1], None, ALU.mult)
                P.ts(KDall[:, cs], kt_[:, cs], EGLG[:, gt * 2 + hp:gt * 2 + hp + 1], None, ALU.mult)
                P.ts(VBt[:, cs], vt_[:, cs], BETA[:, gt * 2 + hp:gt * 2 + hp + 1], None, ALU.mult)
            for hf in range(2):
                P.ts(KD[hf][:, 0:128], KDall[:, 0:128], k32("rm")[:, hf:hf + 1], None, ALU.mult, eng="pool")
            for hp in range(2):
                P.mm(pst[:, 128:256], KBGZ[:, hp * 128:(hp + 1) * 128], Tt[:, hp * 128:(hp + 1) * 128], start=(hp == 0), stop=(hp == 1))
            for hp in range(2):
                P.mm(pst[:, 256 + hp * 64:256 + (hp + 1) * 64], Tt[:, hp * 128:(hp + 1) * 128], VBt[:, hp * 64:(hp + 1) * 64])
            yield
            P.cp(WT2[si][:, 0:128], pst[:, 128:256], eng="act")
            P.cp(U2[si][:, 0:128], pst[:, 256:384], eng="act")
            yield

        def scan_pair(n, tp, pso):
            for si in range(2):
                t = 2 * tp + si
                gt = 4 * n + t
                WT, U, KD, AQT, QD = WT2[si], U2[si], KDs[si], AQT2[si], QD2[si]
                for hf in range(2):
                    rows = slice(hf * 64, hf * 64 + 64)
                    ic = slice(hf * 64, hf * 64 + 64)
                    M = 64 if hf == 0 else 128
                    psws = PS[7]
                    P.mm(psws[0:M, 0:128], WT[:, 0:M], SSB[:, :])
                    yield
                    for hp in range(2):
                        cs = slice(hp * 64, (hp + 1) * 64)
                        P.tt(VNZ[rows, hp * 128 + hp * 64: hp * 128 + hp * 64 + 64], U[rows, cs], psws[rows, cs], ALU.subtract)
                    yield
                    oc = slice(t * 128 + hf * 64, t * 128 + hf * 64 + 64)
                    P.mm(pso[:, oc], SSB[:, :], QD[:, ic], start=True, stop=False)
                    for hp in range(2):
                        P.mm(pso[:, oc], VNZ[:, hp * 128:(hp + 1) * 128], AQT[:, hp * 128 + hf * 64: hp * 128 + hf * 64 + 64],
                             start=False, stop=(hp == 1))
                    pss = PS[7]
                    for hp in range(2):
                        P.mm(pss[:, 128:256], KD[hf][:, 0:128], VNZ[:, hp * 128:(hp + 1) * 128], start=(hp == 0), stop=(hp == 1))
                    yield
                    ch = gt * 2 + hf
                    for hp in range(2):
                        r = slice(hp * 64, hp * 64 + 64)
                        P.stt(SS[r, r], SS[r, r], DL[r, ch:ch + 1], pss[r, 128 + hp * 64:128 + hp * 64 + 64], ALU.mult, ALU.add)
                    yield
                    P.cp(SSB[:, :], SS[:, :], eng="act")
                    yield
            if tp == 1:
                cols = slice(n * 512, (n + 1) * 512)
                head_norm_gate(l, pso, par(l, "gdng"), Y[:, yblk, cols])

        def chain(*gs):
            for g_ in gs:
                yield from g_

        pso = PS[4]
        prev = None
        for n in range(NB):
            cols = slice(n * 512, (n + 1) * 512)
            for bi, (w_, dst) in enumerate(((wq, QN), (wk, KN), (wv, VT))):
                ps = PS[5 + (bi % 2)]
                proj_cm(ps, w_, HT, cols)
                pc = PC[bi]
                P.cp(pc[:, 3:515], ps[:, :], eng="act")
                if bi == 2:
                    P.ts(ACC[:, :], pc[:, 0:512], convw(bi, 0), None, ALU.mult)
                    for j in range(1, 4):
                        P.stt(ACC[:, :], pc[:, j:j + 512], convw(bi, j), ACC[:, :], ALU.mult, ALU.add)
                    P.cp(pc[:, 0:3], pc[:, 512:515])
                    P.act(VT[:, :], ACC[:, :], AF.Silu)
                else:
                    psc = PS[bi % 2]
                    for j in range(4):
                        P.mm(psc[:, :], DIAG[bi * 4 + j], pc[:, j:j + 512], start=(j == 0), stop=(j == 3))
                    P.cp(pc[:, 0:3], pc[:, 512:515], eng="pool")
                    P.act(CQ[:, :], psc[:, :], AF.Silu)
                    P.act(SQ[:, :], CQ[:, :], AF.Square)
                    P.mm(PS[7][:, :], k32("blk64"), SQ[:, :])
                    rsqrt_from(RS[:, :], PS[7][:, :], 1.0, EPS)
                    P.stt(dst[:, :], CQ[:, :], float(HD ** -0.5) if bi == 0 else 1.0, RS[:, :], ALU.mult, ALU.mult)
            for tp in range(2):
                gens = [chain(prepinv(n, 2 * tp + si, si), post(n, 2 * tp + si, si)) for si in range(2)]
                alive = list(gens)
                while alive:
                    for g_ in list(alive):
                        try:
                            next(g_)
                        except StopIteration:
                            alive.remove(g_)
                    if prev is not None:
                        try:
                            next(prev)
                        except StopIteration:
                            prev = None
                if prev is not None:
                    for _ in prev:
                        pass
                prev = scan_pair(n, tp, pso)
        for _ in prev:
            pass

    ONESB = P.sb("ONESB", [128, 128], BF16)
    P.memset(ONESB[:, :], 1.0)

    def outproj(l):
        KTb = KT if KTs is None else KTs
        OB = [F[i][:, :] for i in range(7)] + [KTb[:, 0:1024].bitcast(F32)]
        wviews = []
        for d in range(8):
            if d < 4:
                w_ = load_w(l, ("out", "w", d))
                wviews.append([w_[:, k, :] for k in range(8)])
            else:
                ha, hb = H[2 + 2 * (d - 4)], H[3 + 2 * (d - 4)]
                src = wD[l, CMB[("out", "w", d)]]
                P.dma(ha[:, :], src[:, 0:512], q="pool")
                P.dma(hb[:, :], src[:, 512:1024], q="pool")
                wviews.append([(ha if k < 4 else hb)[:, (k % 4) * 128:(k % 4 + 1) * 128] for k in range(8)])
        for n in range(NB):
            cols = slice(n * 512, (n + 1) * 512)
            for d in range(8):
                ps = PS[5 + (d % 2)]
                for k in range(8):
                    P.mm(ps[:, :], wviews[d][k], Y[:, k, cols], start=(k == 0), stop=(k == 7))
                P.cp(OB[d], ps[:, :], eng="act")
                sq = H[d % 2]
                P.act(sq[:, :], ps[:, :], AF.Square)
                P.mm(PS[0][:, :], ONESB[:, :], sq[:, :], start=(d == 0), stop=(d == 7))
            rs = VB[:, 0:1024].bitcast(F32)
            rsqrt_from(rs, PS[0][:, :], 1.0 / D_MODEL, EPS)
            for d in range(8):
                P.stt(OB[d], OB[d], par(l, "postg", d), rs, ALU.mult, ALU.mult)
                P.tt(XT[:, d, cols], XT[:, d, cols], OB[d], ALU.add)

    for s in range(NSEQ):
        for c in range(8):
            P.dma(XT[:, c, :], xD[s, :, c * T:(c + 1) * T])
        for l in range(DEPTH):
            P.mark("prenorm s%d l%d" % (s, l))
            prenorm(l)
            P.memset(Y[:, :, :], 0.0) if (len(branches) < 4) else None
            for p in range(2):
                if "gdn" in branches:
                    P.mark("gdn s%d l%d p%d" % (s, l, p))
                    gdn(l, p)
                if "hgrn" in branches:
                    P.mark("hgrn s%d l%d p%d" % (s, l, p))
                    hgrn(l, p)
                if "moba" in branches:
                    P.mark("moba s%d l%d p%d" % (s, l, p))
                    attention(l, p, "moba")
                if "diff" in branches:
                    P.mark("diff s%d l%d p%d" % (s, l, p))
                    attention(l, p, "diff")
            if tap:
                TP = F
                for c in range(8):
                    for n in range(NB):
                        cols = slice(n * 512, (n + 1) * 512)
                        P.cp(TP[(c * NB + n) % 4][:, :], Y[:, c, cols])
                        P.dma(tapD[s, l, :, c * T + n * 512: c * T + (n + 1) * 512], TP[(c * NB + n) % 4][:, :])
            P.mark("outproj s%d l%d" % (s, l))
            outproj(l)
        for c in range(8):
            P.dma(yD[s, :, c * T:(c + 1) * T], XT[:, c, :])
    P.mark("end")
    P.final_wait("sp", [XT] + ([F[0], F[1], F[2], F[3]] if tap else []))
    P.emit()
    return nc, P


_CACHE = {}


def _prep_inputs(inp, T, nseq_total):
    x = np.asarray(inp["x"], np.float32)
    B = x.shape[0]
    xT = np.ascontiguousarray(x.reshape(B, T, 8, 128).transpose(0, 3, 2, 1)).reshape(B, 128, 8 * T)
    w = pack_weights(np.asarray(inp["w_in"], np.float32), np.asarray(inp["w_out"], np.float32))
    par = pack_params({k: np.asarray(v, np.float32) for k, v in inp.items()})
    c32, c16 = make_consts()
    return xT, w, par, c32, c16


def kernel(**inputs):
    x = np.asarray(inputs["x"])
    B, T, D = x.shape
    depth = inputs["w_in"].shape[0]
    nseq = B // N_CORES
    key = (T, nseq, depth)
    if key not in _CACHE:
        _CACHE[key] = build(T=T, NSEQ=nseq, DEPTH=depth)[0]
    nc = _CACHE[key]
    xT, w, par, c32, c16 = _prep_inputs(inputs, T, B)
    in_maps = []
    for c in range(N_CORES):
        in_maps.append({"x": xT[c * nseq:(c + 1) * nseq], "w": w, "c32": c32, "c16": c16, "par": par})
    res = run_bass_kernel_spmd(nc, in_maps, core_ids=list(range(N_CORES)))
    outs = []
    for c in range(N_CORES):
        yT = np.asarray(res.results[c]["y"]).reshape(nseq, 128, 8, T)
        outs.append(yT.transpose(0, 3, 2, 1).reshape(nseq, T, D))
    return np.concatenate(outs, axis=0).astype(np.float32)
```

```python
import math
import numpy as np
import concourse.bass as bass
import concourse.mybir as mybir
from concourse.bass_utils import run_bass_kernel_spmd

F32 = mybir.dt.float32
BF16 = mybir.dt.bfloat16
AF = mybir.ActivationFunctionType
ALU = mybir.AluOpType
AX = mybir.AxisListType

D_MODEL = 1024
HD = 64
EPS = 1e-6
BIG = 30000.0
N_CORES = 8


class View:
    def __init__(self, buf, ap):
        self.buf = buf
        self.ap = ap

    def __getitem__(self, k):
        return View(self.buf, self.ap[k])

    def rearrange(self, *a, **k):
        return View(self.buf, self.ap.rearrange(*a, **k))

    def to_broadcast(self, *a, **k):
        return View(self.buf, self.ap.to_broadcast(*a, **k))

    def unsqueeze(self, *a, **k):
        return View(self.buf, self.ap.unsqueeze(*a, **k))

    def bitcast(self, *a, **k):
        return View(self.buf, self.ap.bitcast(*a, **k))


class Buf:
    def __init__(self, t, name):
        self.t = t
        self.name = name
        self.last_w = None
        self.reads = {}
        self.dma_sem = None
        self.is_psum = False

    def __getitem__(self, k):
        return View(self, self.t[k])


class Prog:
    ENG = ("pe", "act", "dve", "pool", "sp")

    def __init__(self, nc):
        self.nc = nc
        self.streams = {e: [] for e in self.ENG}
        self.sems = {}
        self.cnt = {}
        self.waited = {e: {} for e in self.ENG}
        self.marks = []
        for e in ("pe", "act", "dve", "pool"):
            self.sems[e] = nc.alloc_semaphore("s_" + e)
            self.cnt[e] = 0

    def mark(self, label):
        self.marks.append((label, dict(self.cnt)))

    def sb(self, name, shape, dtype=F32):
        return Buf(self.nc.alloc_sbuf_tensor(name, list(shape), dtype), name)

    def ps(self, name, shape, dtype=F32):
        b = Buf(self.nc.alloc_psum_tensor(name, list(shape), dtype), name)
        b.is_psum = True
        return b

    def dram(self, ap, name):
        return Buf(ap, name)

    def _deps(self, eng, reads, writes):
        need = {}

        def add(k, c):
            if k == "pe" and eng == "pe":
                return
            if need.get(k, 0) < c:
                need[k] = c
        for b in reads:
            if b.last_w is not None:
                add(*b.last_w)
            if b.is_psum:
                for k, c in b.reads.items():
                    if k != eng:
                        add(k, c)
        for b in writes:
            if b.last_w is not None:
                add(*b.last_w)
            for k, c in b.reads.items():
                add(k, c)
        out = []
        w = self.waited[eng]
        for k, c in need.items():
            if w.get(k, 0) < c:
                w[k] = c
                out.append((k, c))
        return out

    def op(self, eng, fn, reads=(), writes=()):
        reads = list(dict.fromkeys(reads))
        writes = list(dict.fromkeys(writes))
        waits = self._deps(eng, reads, writes)
        self.cnt[eng] += 1
        c = self.cnt[eng]
        self.streams[eng].append((waits, fn, (eng, 1)))
        for b in reads:
            b.reads[eng] = c
        for b in writes:
            b.last_w = (eng, c)
            b.reads = {}

    def dma(self, out, in_, q="sp"):
        ob, ib = out.buf, in_.buf
        owner = ob if ob.dma_sem is not None else (ib if ib.dma_sem is not None else ob)
        if owner.dma_sem is None:
            key = "dma%d" % len(self.sems)
            self.sems[key] = self.nc.alloc_semaphore(key)
            self.cnt[key] = 0
            owner.dma_sem = key
        key = owner.dma_sem
        waits = self._deps(q, [ib], [ob])
        self.cnt[key] += 16
        c = self.cnt[key]
        oa, ia = out.ap, in_.ap
        self.streams[q].append((waits, lambda e: e.dma_start(out=oa, in_=ia), (key, 16)))
        ib.reads[key] = c
        ob.last_w = (key, c)
        ob.reads = {}

    def final_wait(self, eng, bufs):
        waits = self._deps(eng, bufs, bufs)
        self.streams[eng].append((waits, None, None))

    def emit(self):
        nc = self.nc
        with nc.Block() as block:
            def mk(ename):
                def body(e):
                    for waits, fn, inc in self.streams[ename]:
                        for k, c in waits:
                            e.wait_ge(self.sems[k], c)
                        if fn is None:
                            continue
                        ins = fn(e)
                        ins.then_inc(self.sems[inc[0]], inc[1])
                return body
            if self.streams["pe"]:
                block.tensor(mk("pe"))
            if self.streams["act"]:
                block.scalar(mk("act"))
            if self.streams["dve"]:
                block.vector(mk("dve"))
            if self.streams["pool"]:
                block.gpsimd(mk("pool"))
            if self.streams["sp"]:
                block.sync(mk("sp"))

    def mm(self, out, lhsT, rhs, start=True, stop=True):
        o, l, r = out.ap, lhsT.ap, rhs.ap
        self.op("pe", lambda e: e.matmul(o, l, r, start=start, stop=stop),
                reads=[lhsT.buf, rhs.buf], writes=[out.buf])

    def tr(self, out, in_, ident):
        o, i, d = out.ap, in_.ap, ident.ap
        self.op("pe", lambda e: e.transpose(o, i, d), reads=[in_.buf, ident.buf], writes=[out.buf])

    def act(self, out, in_, func, bias=None, scale=1.0, eng="act"):
        o, i = out.ap, in_.ap
        rd = [in_.buf]
        kw = {}
        if isinstance(bias, View):
            rd.append(bias.buf)
            kw["bias"] = bias.ap
        elif bias is not None:
            kw["bias"] = float(bias)
        if isinstance(scale, View):
            rd.append(scale.buf)
            kw["scale"] = scale.ap
        else:
            kw["scale"] = float(scale)
        self.op("act", lambda e: e.activation(o, i, func, **kw), reads=rd, writes=[out.buf])

    def tt(self, out, in0, in1, op, eng="dve"):
        o, a, b = out.ap, in0.ap, in1.ap
        self.op(eng, lambda e: e.tensor_tensor(o, a, b, op), reads=[in0.buf, in1.buf], writes=[out.buf])

    def ts(self, out, in0, s1, s2, op0, op1=None, eng="dve"):
        o, a = out.ap, in0.ap
        rd = [in0.buf]
        v1 = s1
        if isinstance(s1, View):
            rd.append(s1.buf)
            v1 = s1.ap
        v2 = s2
        if isinstance(s2, View):
            rd.append(s2.buf)
            v2 = s2.ap
        if op1 is None:
            self.op(eng, lambda e: e.tensor_scalar(o, a, v1, None, op0), reads=rd, writes=[out.buf])
        else:
            self.op(eng, lambda e: e.tensor_scalar(o, a, v1, v2, op0, op1), reads=rd, writes=[out.buf])

    def stt(self, out, in0, scalar, in1, op0, op1, eng="dve"):
        o, a, b = out.ap, in0.ap, in1.ap
        rd = [in0.buf, in1.buf]
        sv = scalar
        if isinstance(scalar, View):
            rd.append(scalar.buf)
            sv = scalar.ap
        self.op(eng, lambda e: e.scalar_tensor_tensor(o, a, sv, b, op0, op1), reads=rd, writes=[out.buf])

    def cp(self, out, in_, eng="dve"):
        o, i = out.ap, in_.ap
        if eng == "act":
            self.op("act", lambda e: e.copy(o, i), reads=[in_.buf], writes=[out.buf])
        else:
            self.op(eng, lambda e: e.tensor_copy(o, i), reads=[in_.buf], writes=[out.buf])

    def memset(self, out, val, eng="dve"):
        o = out.ap
        self.op(eng, lambda e: e.memset(o, val), writes=[out.buf])

    def red(self, out, in_, op, eng="dve"):
        o, i = out.ap, in_.ap
        self.op(eng, lambda e: e.tensor_reduce(o, i, AX.X, op), reads=[in_.buf], writes=[out.buf])

    def recip(self, out, in_):
        o, i = out.ap, in_.ap
        self.op("dve", lambda e: e.reciprocal(o, i), reads=[in_.buf], writes=[out.buf])

    def scan(self, out, d0, d1):
        o, a, b = out.ap, d0.ap, d1.ap
        self.op("dve", lambda e: e.tensor_tensor_scan(o, a, b, 0.0, ALU.mult, ALU.add),
                reads=[d0.buf, d1.buf], writes=[out.buf])


C32 = {}
_o = 0
for _n, _w in (("ident", 128), ("blk64", 128), ("blktri", 128), ("scanm", 512), ("selden", 64), ("place", 256), ("e2", 2), ("rm", 2), ("arel", 16)):
    C32[_n] = (_o, _w)
    _o += _w
NC32 = _o
C16 = {}
_o = 0
for _n, _w in (("ident", 128), ("lvl", 6 * 128), ("caus01", 128), ("causneg", 128), ("hm2", 2), ("hm4", 4),
               ("ka", 16 * 128), ("oh", 16 * 128), ("qab", 512), ("m01", 128), ("m01T", 128)):
    C16[_n] = (_o, _w)
    _o += _w
NC16 = _o


def make_consts():
    c32 = np.zeros((128, NC32), np.float32)
    c16 = np.zeros((128, NC16), np.float32)
    p = np.arange(128)[:, None]
    c = np.arange(128)[None, :]
    same = (p // 64) == (c // 64)

    def put(dst, tab, name, arr):
        o, w = tab[name]
        dst[:, o:o + w] = arr
    put(c32, C32, "ident", (p == c).astype(np.float32))
    put(c32, C32, "blk64", same.astype(np.float32))
    put(c32, C32, "blktri", (same & (p <= c)).astype(np.float32))
    sm = np.ones((128, 512), np.float32)
    sm[:, ::64] = 0.0
    put(c32, C32, "scanm", sm)
    sd = np.zeros((128, 64), np.float32)
    sd[64, :] = 1.0
    put(c32, C32, "selden", sd)
    pl = np.zeros((128, 256), np.float32)
    for r in range(64):
        pl[r, r] = 1.0
        pl[r, 128 + 64 + r] = 1.0
    put(c32, C32, "place", pl)
    e2 = np.zeros((128, 2), np.float32)
    e2[0, 0] = 1.0
    e2[64, 1] = 1.0
    put(c32, C32, "e2", e2)
    arel = np.zeros((128, 16), np.float32)
    for r in range(16):
        arel[:, r] = np.arange(128) + 128.0 * (r - 12) - 256.0
    put(c32, C32, "arel", arel)
    rm = np.zeros((128, 2), np.float32)
    rm[:64, 0] = 1.0
    rm[64:, 1] = 1.0
    put(c32, C32, "rm", rm)

    put(c16, C16, "ident", (p == c).astype(np.float32))
    lv = np.zeros((128, 6, 128), np.float32)
    for l in range(1, 7):
        s = 2 ** l
        lv[:, l - 1, :] = ((p // s) == (c // s)) & ((p % s) >= s // 2) & ((c % s) < s // 2)
    put(c16, C16, "lvl", lv.reshape(128, 768))
    put(c16, C16, "m01", (same & (p >= c)).astype(np.float32))
    put(c16, C16, "m01T", (same & (c >= p)).astype(np.float32))
    put(c16, C16, "caus01", (p <= c).astype(np.float32))
    put(c16, C16, "causneg", np.where(p > c, -BIG, 0.0))
    hm2 = np.zeros((128, 2), np.float32)
    hm2[:64, 0] = 1.0
    hm2[64:, 1] = 1.0
    put(c16, C16, "hm2", hm2)
    hm4 = np.zeros((128, 4), np.float32)
    for g in range(4):
        hm4[g * 32:(g + 1) * 32, g] = 1.0
    put(c16, C16, "hm4", hm4)
    ka = np.zeros((128, 16, 128), np.float32)
    for ri in range(16):
        ka[0, ri, :] = np.arange(128)
        ka[1, ri, :] = 1.0
        ka[2, ri, :] = 128.0 * (ri - 12)
        ka[3, ri, :] = 1.0
    put(c16, C16, "ka", ka.reshape(128, 2048))
    oh = np.zeros((128, 16, 128), np.float32)
    for r in range(16):
        oh[r, r, :] = 1.0
    put(c16, C16, "oh", oh.reshape(128, 2048))
    qab = np.zeros((128, 512), np.float32)
    il = np.arange(512)
    qab[0, :] = 1.0
    qab[1, :] = -(il % 128)
    qab[2, :] = 1.0
    qab[3, :] = -128.0 * (il // 128)
    put(c16, C16, "qab", qab)
    return c32, c16


PAR = {}
_o = 0
for _n, _w in (("preg", 8), ("postg", 8), ("conv", 24), ("gdng", 1), ("hgrng", 1), ("diffg", 1),
               ("lbl", 2), ("lb0", 2), ("alog", 4), ("dtb", 4), ("lq1", 32), ("lk1", 32), ("lq2", 32), ("lk2", 32)):
    PAR[_n] = (_o, _w)
    _o += _w
NPAR = _o

CMB = {}
_i = 0
for _br, _names in (("gdn", ("q", "k", "v", "z")), ("hgrn", ("q", "f", "z")), ("moba", ("q", "k", "z")),
                    ("diff", ("q", "k", "z"))):
    for _nm in _names:
        for _p in range(2):
            CMB[(_br, _nm, _p)] = _i
            _i += 1
for _br in ("gdn", "hgrn", "moba", "diff"):
    for _p in range(2):
        CMB[(_br, "tm", _p)] = _i
        _i += 1
for _d in range(8):
    CMB[("out", "w", _d)] = _i
    _i += 1
NWB = _i

GDN_BASE, HGRN_BASE, MOBA_BASE, DIFF_BASE = 0, 1032, 2056, 3080


def pack_weights(w_in, w_out):
    depth = w_in.shape[0]
    out = np.zeros((depth, NWB, 128, 1024), np.float32)

    def blockify(wcols):
        return wcols.reshape(8, 128, 128).transpose(1, 0, 2).reshape(128, 1024)
    for l in range(depth):
        W = w_in[l]
        def cols(base, off, p):
            return W[:, base + off + p * 128: base + off + (p + 1) * 128]
        for p in range(2):
            out[l, CMB[("gdn", "q", p)]] = blockify(cols(GDN_BASE, 0, p))
            out[l, CMB[("gdn", "k", p)]] = blockify(cols(GDN_BASE, 256, p))
            out[l, CMB[("gdn", "v", p)]] = blockify(cols(GDN_BASE, 512, p))
            out[l, CMB[("gdn", "z", p)]] = blockify(cols(GDN_BASE, 776, p))
            ab = np.zeros((1024, 128), np.float32)
            ab[:, 0:2] = W[:, GDN_BASE + 768 + 2 * p: GDN_BASE + 768 + 2 * p + 2]
            ab[:, 2:4] = W[:, GDN_BASE + 772 + 2 * p: GDN_BASE + 772 + 2 * p + 2]
            out[l, CMB[("gdn", "tm", p)]] = blockify(ab)
            out[l, CMB[("hgrn", "q", p)]] = blockify(cols(HGRN_BASE, 0, p))
            out[l, CMB[("hgrn", "f", p)]] = blockify(cols(HGRN_BASE, 256, p))
            out[l, CMB[("hgrn", "tm", p)]] = blockify(cols(HGRN_BASE, 512, p))
            out[l, CMB[("hgrn", "z", p)]] = blockify(cols(HGRN_BASE, 768, p))
            for br, base in (("moba", MOBA_BASE), ("diff", DIFF_BASE)):
                out[l, CMB[(br, "q", p)]] = blockify(cols(base, 0, p))
                out[l, CMB[(br, "k", p)]] = blockify(cols(base, 256, p))
                out[l, CMB[(br, "tm", p)]] = blockify(cols(base, 512, p))
                out[l, CMB[(br, "z", p)]] = blockify(cols(base, 768, p))
        for d in range(8):
            out[l, CMB[("out", "w", d)]] = blockify(w_out[l][:, d * 128:(d + 1) * 128])
    return out


def pack_params(inp):
    depth = inp["pre_norm_g"].shape[0]
    par = np.zeros((depth, 128, NPAR), np.float32)

    def put(l, name, arr):
        o, w = PAR[name]
        par[l, :, o:o + w] = arr
    for l in range(depth):
        put(l, "preg", inp["pre_norm_g"][l].reshape(8, 128).T)
        put(l, "postg", inp["post_norm_g"][l].reshape(8, 128).T)
        cw = inp["conv_w"][l]
        put(l, "conv", cw.reshape(4, 6, 128).transpose(2, 1, 0).reshape(128, 24))
        put(l, "gdng", np.tile(inp["gdn_norm_g"][l], 2)[:, None])
        put(l, "hgrng", np.tile(inp["hgrn_norm_g"][l], 2)[:, None])
        put(l, "diffg", np.tile(inp["diff_norm_g"][l], 2)[:, None])
        put(l, "lbl", inp["hgrn_lb"][l].reshape(2, 128).T)
        put(l, "lb0", inp["hgrn_lb"][0].reshape(2, 128).T)
        put(l, "alog", np.broadcast_to(inp["gdn_a_log"][l][None, :], (128, 4)))
        put(l, "dtb", np.broadcast_to(inp["gdn_dt_bias"][l][None, :], (128, 4)))
        put(l, "lq1", np.broadcast_to(inp["diff_lq1"][l][None, :], (128, 32)))
        put(l, "lk1", np.broadcast_to(inp["diff_lk1"][l][None, :], (128, 32)))
        put(l, "lq2", np.broadcast_to(inp["diff_lq2"][l][None, :], (128, 32)))
        put(l, "lk2", np.broadcast_to(inp["diff_lk2"][l][None, :], (128, 32)))
    return par


def build(T=2048, NSEQ=2, DEPTH=2, branches=("gdn", "hgrn", "moba", "diff"), tap=False):
    assert T % 512 == 0
    NT = T // 128
    NB = T // 512
    nc = bass.Bass("TRN2", target_bir_lowering=False)
    x_d = nc.dram_tensor("x", [NSEQ, 128, 8 * T], F32, kind="ExternalInput").ap()
    w_d = nc.dram_tensor("w", [DEPTH, NWB, 128, 1024], F32, kind="ExternalInput").ap()
    c32_d = nc.dram_tensor("c32", [128, NC32], F32, kind="ExternalInput").ap()
    c16_d = nc.dram_tensor("c16", [128, NC16], F32, kind="ExternalInput").ap()
    par_d = nc.dram_tensor("par", [DEPTH, 128, NPAR], F32, kind="ExternalInput").ap()
    y_d = nc.dram_tensor("y", [NSEQ, 128, 8 * T], F32, kind="ExternalOutput").ap()
    if tap:
        tap_d = nc.dram_tensor("tap", [NSEQ, DEPTH, 128, 8 * T], F32, kind="ExternalOutput").ap()

    P = Prog(nc)
    xD, wD, c32D, c16D, parD, yD = (P.dram(x_d, "x"), P.dram(w_d, "w"), P.dram(c32_d, "c32"),
                                    P.dram(c16_d, "c16"), P.dram(par_d, "par"), P.dram(y_d, "y"))
    tapD = P.dram(tap_d, "tap") if tap else None
    XT = P.sb("XT", [128, 8, T])
    HT = P.sb("HT", [128, 8, T], BF16)
    Y = P.sb("Y", [128, 8, T], BF16)
    K32 = P.sb("K32", [128, NC32])
    K16 = P.sb("K16", [128, NC16], BF16)
    PARS = [P.sb("PAR%d" % l, [128, NPAR]) for l in range(DEPTH)]
    NWS = 4
    WS = [P.sb("WS%d" % i, [128, 8, 128], BF16) for i in range(NWS)]
    NF, NH = 7, 10
    F = [P.sb("F%d" % i, [128, 512]) for i in range(NF)]
    H = [P.sb("H%d" % i, [128, 512], BF16) for i in range(NH)]
    SMB = [P.sb("SMB%d" % i, [128, 256], BF16) for i in range(29)]
    TTS = [P.sb("TT%d" % i, [128, 512], BF16) for i in range(2)]
    SMF = [P.sb("SMF%d" % i, [128, 256]) for i in range(2)]
    KT = P.sb("KT", [128, T], BF16)
    VB = P.sb("VB", [128, max(NT * 2 * 65, 2080)], BF16)
    KTs = P.sb("KTs", [128, max(T, 1040)], BF16) if T < 1040 else None
    TOK = [P.sb("TOK%d" % i, [128, NT * 4 if i == 0 else NT * 2]) for i in range(10)]
    SS = P.sb("SS", [128, 128])
    SSB = P.sb("SSB", [128, 128], BF16)
    SSB2 = P.sb("SSB2", [128, 128], BF16)
    COL = P.sb("COL", [128, 16])
    KS = P.sb("KS", [128, 2, 8])
    KSr = P.sb("KSr", [128, 8])
    DL = P.sb("DL", [128, NT * 2])
    ABI = P.sb("ABI", [128, 2, 16])
    PS = [P.ps("PS%d" % i, [128, 512]) for i in range(8)]

    def k32(name):
        o, w = C32[name]
        return K32[:, o:o + w]

    def k16(name):
        o, w = C16[name]
        return K16[:, o:o + w]

    P.dma(K32[:, :], c32D[:, :])
    P.dma(K16[:, :], c16D[:, :], q="pool")
    for l in range(DEPTH):
        P.dma(PARS[l][:, :], parD[l])

    wstate = {"next_slot": 0}

    def load_w(l, key):
        s = WS[wstate["next_slot"] % NWS]
        wstate["next_slot"] += 1
        P.dma(s[:, :, :], wD[l, CMB[key]].rearrange("p (k c) -> p k c", k=8), q="pool")
        return s

    def par(l, name, j=0, w=1):
        o, _ = PAR[name]
        return PARS[l][:, o + j:o + j + w]

    def rsqrt_from(out, in_, scale, eps):
        P.act(out, in_, AF.Ln, bias=eps, scale=scale)
        P.act(out, out, AF.Exp, scale=-0.5)

    def proj_cm(ps, wslot, src, cols):
        for k in range(8):
            P.mm(ps[:, :], wslot[:, k, :], src[:, k, cols], start=(k == 0), stop=(k == 7))

    def head_norm_gate(l, ops, gcol, yv, extra=1.0):
        osb, sq, rs = F[4], F[5], F[6]
        P.cp(osb[:, :], ops[:, :], eng="act")
        P.act(sq[:, :], ops[:, :], AF.Square)
        P.mm(PS[7][:, :], k32("blk64"), sq[:, :])
        rsqrt_from(rs[:, :], PS[7][:, :], 1.0 / HD, EPS)
        P.stt(osb[:, :], osb[:, :], gcol, rs[:, :], ALU.mult, ALU.mult)
        if extra != 1.0:
            P.stt(yv, osb[:, :], float(extra), yv, ALU.mult, ALU.mult)
        else:
            P.tt(yv, osb[:, :], yv, ALU.mult)

    def z_gate(l, wz, yblk):
        for n in range(NB):
            cols = slice(n * 512, (n + 1) * 512)
            ps = PS[5 + (n % 2)]
            proj_cm(ps, wz, HT, cols)
            P.act(Y[:, yblk, cols], ps[:, :], AF.Silu)

    def prenorm(l):
        for n in range(NB):
            cols = slice(n * 512, (n + 1) * 512)
            for k in range(8):
                sq = H[k % 2]
                P.act(sq[:, :], XT[:, k, cols], AF.Square)
                P.mm(PS[n % 2][:, :], ONESB[:, :], sq[:, :], start=(k == 0), stop=(k == 7))
            rs = F[2 + (n % 2)]
            rsqrt_from(rs[:, :], PS[n % 2][:, :], 1.0 / D_MODEL, EPS)
            for k in range(8):
                P.stt(HT[:, k, cols], XT[:, k, cols], par(l, "preg", k), rs[:, :], ALU.mult, ALU.mult)

    def attention(l, p, kind):
        yblk = (4 if kind == "moba" else 6) + p
        slopes = [2.0 ** -(2 * h + 2) for h in range(4)] if kind == "moba" else [2.0 ** -(2 * h + 1) for h in range(4)]
        dh = 64 if kind == "moba" else 32
        scale = dh ** -0.5
        wq = load_w(l, (kind, "q", p))
        wk = load_w(l, (kind, "k", p))
        wz = load_w(l, (kind, "z", p))
        wv = load_w(l, (kind, "tm", p))
        z_gate(l, wz, yblk)
        if kind == "moba":
            P.memset(KSr[:, :], 0.0)
        for n in range(NB):
            cols = slice(n * 512, (n + 1) * 512)
            ps = PS[5 + (n % 2)]
            proj_cm(ps, wk, HT, cols)
            P.cp(KT[:, cols], ps[:, :], eng="act")
            if kind == "moba":
                P.cp(F[3][:, :], ps[:, :])
                P.red(KSr[:, 2 * n:2 * n + 2], F[3][:, :].rearrange("p (b c) -> p b c", c=256), ALU.add)
        if kind == "moba":
            for hp in range(2):
                P.ts(KS[:, hp, :], KSr[:, :], k32("rm")[:, hp:hp + 1], None, ALU.mult)
        VA = VB[:, 0:NT * 2 * 65].rearrange("p (t h c) -> p t h c", t=NT, h=2)
        P.memset(VA[:, :, :, 64:65], 1.0)
        for t in range(NT):
            ps = PS[5 + (t % 2)]
            for k in range(8):
                P.mm(ps[:, 0:128], HT[:, k, t * 128:(t + 1) * 128], wv[:, k, :], start=(k == 0), stop=(k == 7))
            P.cp(VA[:, t, :, 0:64], ps[:, 0:128].rearrange("p (h c) -> p h c", h=2), eng="act")
        if kind == "diff":
            lam_init = 0.8 - 0.6 * math.exp(-0.3 * l)
            tmp = SMF[0]
            P.tt(tmp[:, 0:32], par(l, "lq1", 0, 32), par(l, "lk1", 0, 32), ALU.mult)
            P.red(COL[:, 0:1], tmp[:, 0:32], ALU.add)
            P.tt(tmp[:, 32:64], par(l, "lq2", 0, 32), par(l, "lk2", 0, 32), ALU.mult)
            P.red(COL[:, 1:2], tmp[:, 32:64], ALU.add)
            P.act(COL[:, 2:4], COL[:, 0:2], AF.Exp)
            P.tt(COL[:, 4:5], COL[:, 3:4], COL[:, 2:3], ALU.subtract)
            P.ts(COL[:, 5:6], COL[:, 4:5], -lam_init, None, ALU.add)
            P.ts(COL[:, 6:7], par(l, "diffg"), 1.0 - lam_init, None, ALU.mult)
        nmask = 2 if kind == "moba" else 4
        mask_tab = k16("hm2") if kind == "moba" else k16("hm4")
        QZ = [H[0], H[1], H[2], H[3]]
        QA = [H[4], H[5]]
        PT = [H[6], H[7], H[8]]
        OC = [F[0], F[1]]
        RC = [F[2], F[3]]
        for b_ in OC:
            P.memset(b_[:, :], 0.0)
        for hp_, b_ in enumerate(QA):
            P.memset(b_[:, :], 0.0)
            P.ts(b_[0:4, :], k16("qab")[0:4, :], float(slopes[2 * p + hp_]), None, ALU.mult)
            P.ts(ABI[:, hp_, :], k32("arel"), float(slopes[2 * p + hp_]), None, ALU.mult)
        bias_mode = [slopes[2 * p + hp_] <= 2.0 ** -4 for hp_ in range(2)]
        SELT = H[9]
        P.memset(SELT[:, :], 0.0)
        nmap = 1 if kind == "moba" else 2
        GF = [SMB[i][:, :].bitcast(F32) for i in range(6)]

        def block_begin(n):
            cols = slice(n * 512, (n + 1) * 512)
            psq = PS[5]
            proj_cm(psq, wq, HT, cols)
            for m in range(nmask):
                P.stt(QZ[m][:, :], psq[:, :], float(scale), mask_tab[:, m:m + 1].to_broadcast([128, 512]),
                      ALU.mult, ALU.mult)
            if not (kind == "moba" and n >= 2):
                return
            q32 = F[1]
            P.cp(q32[:, :], psq[:, :], eng="act")
            W = 2 * n + 1
            psg = PS[7]
            for t in range(4):
                for hp in range(2):
                    P.mm(psg[:, (t * 2 + hp) * 8:(t * 2 + hp) * 8 + 8], q32[:, t * 128:(t + 1) * 128], KS[:, hp, :])
            g = GF[0]
            P.cp(g[:, 0:64], psg[:, 0:64])
            g3 = g[:, 0:64].rearrange("p (a b) -> p a b", b=8)
            P.memset(g3[:, 0:4, 2 * n:2 * n + 1], -1e30)
            cur = g3[:, :, 0:W]
            m_ = GF[1]
            for it in range(3):
                P.red(m_[:, 8 * it:8 * it + 8], cur, ALU.max)
                if it == 2:
                    break
                e_ = GF[2 + it]
                e3 = e_[:, 0:64].rearrange("p (a b) -> p a b", b=8)[:, :, 0:W]
                P.tt(e3, cur, m_[:, 8 * it:8 * it + 8].unsqueeze(2).to_broadcast([128, 8, W]), ALU.is_ge)
                P.stt(e3, e3, -1e30, cur, ALU.mult, ALU.add)
                cur = e3
            mv = GF[4]
            P.memset(mv[:, 0:64], 0.0)
            mv3 = mv[:, 0:64].rearrange("p (a b) -> p a b", b=8)
            P.tt(mv3[:, :, 0:W], g3[:, :, 0:W], m_[:, 16:24].unsqueeze(2).to_broadcast([128, 8, W]), ALU.is_ge)
            P.ts(mv3[:, :, 0:W], mv3[:, :, 0:W], BIG, -BIG, ALU.mult, ALU.add)
            P.memset(mv3[:, 0:4, 2 * n:2 * n + 1], 0.0)
            pst = PS[7]
            for t in range(4):
                P.tr(pst[0:16, t * 128:(t + 1) * 128], mv[:, t * 16:(t + 1) * 16], k32("ident"))
            P.cp(SELT[0:16, :], pst[0:16, :])

        jobs = [(n, hp, mp, jt) for n in range(NB) for hp in range(2) for mp in range(nmap) for jt in range(4 * n + 4)]
        deferred = []

        def emitA(job, gi):
            n, hp, mp, jt = job
            if hp == 0 and mp == 0 and jt == 0:
                block_begin(n)
            use_sel = (kind == "moba" and n >= 2)
            m = hp * nmap + mp if kind == "diff" else hp
            qa = QA[hp]
            c0 = max(0, jt - 4 * n) * 128
            sc = PS[gi % 3]
            pt = PT[gi % 3]
            blk = jt // 2
            masked = use_sel and blk <= 2 * n
            diag = jt >= 4 * n
            bm = bias_mode[hp]
            P.mm(sc[:, c0:512], KT[:, jt * 128:(jt + 1) * 128], QZ[m][:, c0:512], start=True,
                 stop=bm and not (masked or diag))
            if not bm:
                ka = k16("ka")[:, (jt - 4 * n + 12) * 128:(jt - 4 * n + 13) * 128]
                P.mm(sc[:, c0:512], ka, qa[:, c0:512], start=False, stop=not (masked or diag))
            if masked:
                oh = k16("oh")[:, (hp * 8 + blk) * 128:(hp * 8 + blk + 1) * 128]
                P.mm(sc[:, c0:512], oh, SELT[:, c0:512], start=False, stop=not diag)
            if diag:
                P.mm(sc[:, c0:c0 + 128], k16("ident"), k16("causneg"), start=False, stop=True)
            if bm:
                r_ = jt - 4 * n + 12
                P.act(pt[:, c0:512], sc[:, c0:512], AF.Exp, bias=ABI[:, hp, r_:r_ + 1])
            else:
                P.act(pt[:, c0:512], sc[:, c0:512], AF.Exp)

        def emitB(job, gi):
            n, hp, mp, jt = job
            njt = 4 * n + 4
            cols = slice(n * 512, (n + 1) * 512)
            c0 = max(0, jt - 4 * n) * 128
            pt = PT[gi % 3]
            acc = PS[3 + mp]
            P.mm(acc[0:65, c0:512], VA[:, jt, hp, :], pt[:, c0:512], start=(jt == 0), stop=(jt == njt - 1))
            if jt == njt - 1:
                oc = OC[mp]
                P.cp(oc[0:65, :], acc[0:65, :], eng="act")

                def f2(oc=oc, hp=hp, mp=mp, cols=cols):
                    P.mm(PS[7][0:64, :], k32("selden"), oc[:, :])
                    rc = RC[mp]
                    P.act(rc[0:64, :], PS[7][0:64, :], AF.Ln)
                    P.act(rc[0:64, :], rc[0:64, :], AF.Exp, scale=-1.0)
                    P.tt(oc[0:64, :], oc[0:64, :], rc[0:64, :], ALU.mult)
                    if mp == nmap - 1:
                        if kind == "diff":
                            P.stt(OC[0][0:64, :], OC[1][0:64, :], COL[0:64, 5:6], OC[0][0:64, :], ALU.mult, ALU.add)

                        def f5(hp=hp, cols=cols):
                            P.mm(PS[6][:, :], k32("place")[:, hp * 128:(hp + 1) * 128], OC[0][:, :],
                                 start=(hp == 0), stop=(hp == 1))
                            if hp == 1:
                                if kind == "moba":
                                    P.tt(Y[:, yblk, cols], PS[6][:, :], Y[:, yblk, cols], ALU.mult)
                                else:
                                    head_norm_gate(l, PS[6], COL[:, 6:7], Y[:, yblk, cols])
                        deferred.append([2, f5])
                deferred.append([2, f2])

        def run_deferred(flush=False):
            i = 0
            while i < len(deferred):
                deferred[i][0] -= 1
                if flush or deferred[i][0] <= 0:
                    fn = deferred.pop(i)[1]
                    fn()
                else:
                    i += 1

        LOOK = 3
        for i in range(min(LOOK, len(jobs))):
            emitA(jobs[i], i)
        for i in range(len(jobs)):
            emitB(jobs[i], i)
            if i + LOOK < len(jobs):
                emitA(jobs[i + LOOK], i + LOOK)
            run_deferred()
        while deferred:
            run_deferred(flush=True)

    def hgrn(l, p):
        yblk = 2 + p
        wq = load_w(l, ("hgrn", "q", p))
        wf = load_w(l, ("hgrn", "f", p))
        wz = load_w(l, ("hgrn", "z", p))
        wv = load_w(l, ("hgrn", "tm", p))
        z_gate(l, wz, yblk)
        VH = VB[:, 0:NT * 128].rearrange("p (t c) -> p t c", t=NT)
        for t in range(NT):
            ps = PS[5 + (t % 2)]
            for k in range(8):
                P.mm(ps[:, 0:128], HT[:, k, t * 128:(t + 1) * 128], wv[:, k, :], start=(k == 0), stop=(k == 7))
            P.cp(VH[:, t, :], ps[:, 0:128], eng="act")
        if l == 0:
            P.memset(COL[:, 8:9], 0.0)
        else:
            P.tt(COL[:, 8:9], par(l, "lbl", p), par(l, "lb0", p), ALU.subtract)
            P.act(COL[:, 8:9], COL[:, 8:9], AF.Sigmoid)
        P.ts(COL[:, 9:10], COL[:, 8:9], -1.0, 1.0, ALU.mult, ALU.add)
        P.memset(SS[:, :], 0.0)
        P.memset(SSB[:, :], 0.0)
        P.memset(SSB2[:, :], 0.0)
        SSBs = [SSB, SSB2]
        KTb = KT if KTs is None else KTs
        E = KTb[:, 0:1024].bitcast(F32)
        QS, FG, G, DG = F[0], F[1], F[2], F[3]
        HSET = [(H[0], H[1], H[2], H[3]), (H[4], H[5], H[6], H[7])]
        DLHs = [SMF[0], SMF[1]]
        TSETS = [SMB[5 * i:5 * i + 5] for i in range(2)]
        for st_ in TSETS:
            P.memset(st_[0][:, :], 0.0)
            P.memset(st_[1][:, :], 0.0)

        def block_prep(n):
            cols = slice(n * 512, (n + 1) * 512)
            QG, KG, QD, KDT = HSET[n % 2]
            DLH = DLHs[n % 2]
            proj_cm(PS[5], wq, HT, cols)
            P.act(QS[:, :], PS[5][:, :], AF.Silu)
            yield
            proj_cm(PS[6], wf, HT, cols)
            P.act(FG[:, :], PS[6][:, :], AF.Sigmoid)
            yield
            P.ts(FG[:, :], FG[:, :], COL[:, 9:10], COL[:, 8:9], ALU.mult, ALU.add)
            yield
            P.act(E, FG[:, :], AF.Ln)
            yield
            P.scan(G[:, :], k32("scanm"), E)
            P.ts(FG[:, :], FG[:, :], -1.0, 1.0, ALU.mult, ALU.add)
            yield
            G3 = G[:, :].rearrange("p (n c) -> p n c", c=64)
            DG3 = DG[:, :].rearrange("p (n c) -> p n c", c=64)
            P.tt(DG3, G3, G3[:, :, 32:33].to_broadcast([128, 8, 64]), ALU.subtract)
            yield
            P.act(E, DG[:, :], AF.Exp)
            yield
            P.tt(QG[:, :], QS[:, :], E, ALU.mult)
            yield
            P.act(E, DG[:, :], AF.Exp, scale=-1.0)
            yield
            P.tt(KG[:, :], FG[:, :], E, ALU.mult)
            yield
            P.act(E, G[:, :], AF.Exp)
            yield
            P.tt(QD[:, :], QS[:, :], E, ALU.mult)
            P.tt(DG3, G3[:, :, 63:64].to_broadcast([128, 8, 64]), G3, ALU.subtract)
            yield
            P.act(E, DG[:, :], AF.Exp)
            yield
            P.tt(KDT[:, :], FG[:, :], E, ALU.mult)
            P.act(DLH[:, 0:8], G3[:, :, 63], AF.Exp)
            yield

        def tile_pre(n, t):
            gt = 4 * n + t
            tc = slice(t * 128, (t + 1) * 128)
            ATM, VZ, KD0_, KD1_, QGZ = TSETS[gt % 2]
            KD = [KD0_, KD1_]
            QG, KG, QD, KDT = HSET[n % 2]
            pst = PS[7]
            psa = PS[gt % 2]
            pss = PS[2 + gt % 2]
            P.tr(pst[:, 0:64].bitcast(BF16), KDT[:, tc], k16("ident"))
            P.tt(QGZ[:, :].rearrange("p (h c) -> p h c", h=2),
                 QG[:, tc].unsqueeze(1).to_broadcast([128, 2, 128]),
                 k16("hm2").unsqueeze(2).to_broadcast([128, 2, 128]), ALU.mult)
            for hp in range(2):
                P.cp(VZ[:, hp * 128 + hp * 64: hp * 128 + hp * 64 + 64], VH[:, gt, hp * 64:(hp + 1) * 64], eng="act")
            for hf in range(2):
                P.ts(KD[hf][:, 0:128], pst[:, 0:64].bitcast(BF16), k32("rm")[:, hf:hf + 1], None, ALU.mult)
            yield
            for hf in range(2):
                M = 64 if hf == 0 else 128
                for hp in range(2):
                    P.mm(psa[0:M, hp * 128 + hf * 64: hp * 128 + hf * 64 + 64],
                         KG[:, t * 128:t * 128 + M], QGZ[:, hp * 128 + hf * 64: hp * 128 + hf * 64 + 64])
            for hf in range(2):
                for hp in range(2):
                    P.mm(pss[:, hf * 128:(hf + 1) * 128], KD[hf][:, 0:128], VZ[:, hp * 128:(hp + 1) * 128],
                         start=(hp == 0), stop=(hp == 1))
            yield
            A3 = ATM[:, :].rearrange("p (h c) -> p h c", h=2)
            for hf in range(2):
                ic = slice(hf * 64, hf * 64 + 64)
                rows = slice(hf * 64, hf * 64 + 64)
                P.tt(A3[rows, :, ic], psa[:, 0:256].rearrange("p (h c) -> p h c", h=2)[rows, :, ic],
                     k16("caus01")[rows, ic].unsqueeze(1).to_broadcast([64, 2, 64]), ALU.mult)
            yield

        def tile_chain(n, t, pso):
            gt = 4 * n + t
            ATM, VZ, KD0_, KD1_, QGZ = TSETS[gt % 2]
            QG, KG, QD, KDT = HSET[n % 2]
            DLH = DLHs[n % 2]
            pss = PS[2 + gt % 2]
            A3 = ATM[:, :].rearrange("p (h c) -> p h c", h=2)
            for hf in range(2):
                ic = slice(hf * 64, hf * 64 + 64)
                oc = slice(t * 128 + hf * 64, t * 128 + hf * 64 + 64)
                cg = gt * 2 + hf
                P.mm(pso[:, oc], SSBs[cg % 2][:, :], QD[:, oc], start=True, stop=False)
                for hp in range(2):
                    P.mm(pso[:, oc], VZ[:, hp * 128:(hp + 1) * 128], A3[:, hp, ic], start=False, stop=(hp == 1))
                yield
                ch = t * 2 + hf
                for hp in range(2):
                    r = slice(hp * 64, hp * 64 + 64)
                    P.stt(SS[r, r], SS[r, r], DLH[r, ch:ch + 1], pss[r, hf * 128 + hp * 64:hf * 128 + hp * 64 + 64], ALU.mult, ALU.add)
                yield
                P.cp(SSBs[(cg + 1) % 2][:, :], SS[:, :], eng="act")
                yield
            if t == 3:
                cols = slice(n * 512, (n + 1) * 512)
                head_norm_gate(l, pso, par(l, "hgrng"), Y[:, yblk, cols])

        def step(g_):
            try:
                next(g_)
                return True
            except StopIteration:
                return False

        pso = PS[4]
        for _ in block_prep(0):
            pass
        prev = None
        bg = None
        for n in range(NB):
            for t in range(4):
                if t == 0:
                    if bg is not None:
                        for _ in bg:
                            pass
                    bg = block_prep(n + 1) if n + 1 < NB else None
                cur = tile_pre(n, t)
                alive = True
                while alive:
                    alive = step(cur)
                    if prev is not None and not step(prev):
                        prev = None
                    if t >= 1 and bg is not None and not step(bg):
                        bg = None
                if prev is not None:
                    for _ in prev:
                        pass
                prev = tile_chain(n, t, pso)
        for _ in prev:
            pass

    def gdn(l, p):
        yblk = p
        wz = load_w(l, ("gdn", "z", p))
        wab = load_w(l, ("gdn", "tm", p))
        wq = load_w(l, ("gdn", "q", p))
        wk = load_w(l, ("gdn", "k", p))
        z_gate(l, wz, yblk)
        AB, GR, BETA, G, GL, EG, BEG, EGLG, NEGG, TMP = TOK
        psab = PS[7]
        for t in range(NT):
            for k in range(8):
                P.mm(psab[:, t * 4:t * 4 + 4], HT[:, k, t * 128:(t + 1) * 128], wab[:, k, 0:4], start=(k == 0), stop=(k == 7))
        P.cp(AB[:, :], psab[:, 0:NT * 4])
        wv = load_w(l, ("gdn", "v", p))
        AB3 = AB[:, :].rearrange("p (t c) -> p t c", c=4)
        GR3 = GR[:, 0:NT * 2].rearrange("p (t c) -> p t c", c=2)
        dtb = par(l, "dtb", 2 * p, 2).unsqueeze(1).to_broadcast([128, NT, 2])
        P.tt(GR3, AB3[:, :, 0:2], dtb, ALU.add)
        T3 = TMP[:, 0:NT * 2].rearrange("p (t c) -> p t c", c=2)
        P.ts(T3, GR3, -30.0, 0.0, ALU.add, ALU.max)
        P.ts(GR3, GR3, 30.0, None, ALU.min)
        P.act(GR[:, 0:NT * 2], GR[:, 0:NT * 2], AF.Exp)
        P.act(GR[:, 0:NT * 2], GR[:, 0:NT * 2], AF.Ln, bias=1.0)
        P.tt(GR3, GR3, T3, ALU.add)
        P.act(COL[:, 10:12], par(l, "alog", 2 * p, 2), AF.Exp)
        P.ts(COL[:, 10:12], COL[:, 10:12], -1.0, None, ALU.mult)
        P.tt(GR3, GR3, COL[:, 10:12].unsqueeze(1).to_broadcast([128, NT, 2]), ALU.mult)
        B3 = BETA[:, 0:NT * 2].rearrange("p (t c) -> p t c", c=2)
        P.act(B3, AB3[:, :, 2:4], AF.Sigmoid)
        n2 = NT * 2
        P.mm(PS[7][:, 0:n2], k32("blktri"), GR[:, 0:n2])
        P.cp(G[:, 0:n2], PS[7][:, 0:n2])
        P.mm(PS[6][:, 0:n2], k32("blk64"), GR[:, 0:n2])
        P.cp(GL[:, 0:n2], PS[6][:, 0:n2])
        P.act(EG[:, 0:n2], G[:, 0:n2], AF.Exp)
        P.tt(BEG[:, 0:n2], EG[:, 0:n2], BETA[:, 0:n2], ALU.mult)
        P.tt(EGLG[:, 0:n2], GL[:, 0:n2], G[:, 0:n2], ALU.subtract)
        P.act(EGLG[:, 0:n2], EGLG[:, 0:n2], AF.Exp)
        P.ts(NEGG[:, 0:n2], G[:, 0:n2], -1.0, None, ALU.mult)
        REP = SMF[0]
        psd = PS[6]
        for t in range(NT):
            P.cp(REP[:, 0:128].rearrange("p (h c) -> p h c", h=2),
                 GL[:, t * 2:t * 2 + 2].unsqueeze(2).to_broadcast([128, 2, 64]))
            P.mm(psd[:, 256 + t * 2:256 + t * 2 + 2], REP[:, 0:128], k32("e2"))
        P.act(DL[:, 0:n2], psd[:, 256:256 + n2], AF.Exp)
        P.memset(SS[:, :], 0.0)
        P.memset(SSB[:, :], 0.0)
        VNZ = SMB[0]
        P.memset(VNZ[:, :], 0.0)
        KBGZ = SMB[1]
        P.memset(KBGZ[:, :], 0.0)
        KD0 = SMB[2]
        KTb = KT if KTs is None else KTs
        PC = [VB[:, 0:515], VB[:, 520:1035], KTb[:, 0:1030].bitcast(F32)]
        for b_ in PC:
            P.memset(b_[:, 0:3], 0.0)
        DIAG = []
        for bi_ in range(2):
            for j_ in range(4):
                dst_ = wab[:, bi_ * 4 + j_, :]
                P.ts(dst_, k16("ident"), par(l, "conv", (bi_ * 2 + p) * 4 + j_), None, ALU.mult, eng="pool")
                DIAG.append(dst_)
        ACC, CQ, SQ, RS = F[0], F[1], F[2], F[3]
        QN, KN, VT = H[0], H[1], H[2]
        convw = lambda blk, j: par(l, "conv", (blk * 2 + p) * 4 + j)
        hmb = k16("hm2").unsqueeze(2).to_broadcast([128, 2, 128])
        idb = k16("ident").unsqueeze(1).to_broadcast([128, 2, 128])

        def v3(x):
            return x[:, :].rearrange("p (h c) -> p h c", h=2)

        def prepinv(n, t, si):
            gt = 4 * n + t
            tc = slice(t * 128, (t + 1) * 128)
            KNZ, QNZ, D, DT, A, AQT, QD, AL, IZ, TQ, EGB = SMB[3 + 11 * si: 3 + 11 * si + 11]
            Tm, Tt = TTS[si][:, 0:256], TTS[si][:, 256:512]
            REPg = SMF[si]
            psGZ, psK, psT = PS[0 + si], PS[2 + si], PS[5 + si]
            P.tt(v3(KNZ), KN[:, tc].unsqueeze(1).to_broadcast([128, 2, 128]), hmb, ALU.mult)
            P.tt(v3(QNZ), QN[:, tc].unsqueeze(1).to_broadcast([128, 2, 128]), hmb, ALU.mult)
            P.cp(v3(REPg), GR[:, gt * 2:gt * 2 + 2].unsqueeze(2).to_broadcast([128, 2, 128]))
            yield
            for hp in range(2):
                hs = slice(hp * 128, (hp + 1) * 128)
                P.mm(psGZ[:, hs], REPg[:, hs], k32("blktri"))
                P.mm(psK[:, hs], KN[:, tc], KNZ[:, hs])
                P.mm(psK[:, 256 + hp * 128:256 + (hp + 1) * 128], KN[:, tc], QNZ[:, hs])
            yield
            for hp in range(2):
                hs = slice(hp * 128, (hp + 1) * 128)
                P.act(D[:, hs], psGZ[:, hs], AF.Exp, bias=G[:, gt * 2 + hp:gt * 2 + hp + 1], scale=-1.0)
                P.act(DT[:, hs], psGZ[:, hs], AF.Exp, bias=NEGG[:, gt * 2 + hp:gt * 2 + hp + 1], scale=1.0)
            P.act(EGB[:, :], psGZ[:, 0:256], AF.Exp)
            yield
            P.stt(v3(D), v3(D), 1e30, k16("m01").unsqueeze(1).to_broadcast([128, 2, 128]), ALU.min, ALU.mult)
            P.stt(v3(DT), v3(DT), 1e30, k16("m01T").unsqueeze(1).to_broadcast([128, 2, 128]), ALU.min, ALU.mult)
            yield
            for hp in range(2):
                hs = slice(hp * 128, (hp + 1) * 128)
                P.stt(A[:, hs], psK[:, hs], BETA[:, gt * 2 + hp:gt * 2 + hp + 1], D[:, hs], ALU.mult, ALU.mult)
            P.tt(AQT[:, :], psK[:, 256:512], DT[:, :], ALU.mult)
            P.tt(TQ[:, :], QNZ[:, :], EGB[:, :], ALU.mult)
            P.tt(QD[:, 0:128], TQ[:, 0:128], TQ[:, 128:256], ALU.add)
            yield
            for lv in range(6):
                lvm = k16("lvl")[:, lv * 128:(lv + 1) * 128].unsqueeze(1).to_broadcast([128, 2, 128])
                P.tt(v3(AL), v3(A), lvm, ALU.mult)
                yield
                if lv == 0:
                    P.stt(v3(Tm), v3(AL), -1.0, idb, ALU.mult, ALU.add)
                    for hp in range(2):
                        hs = slice(hp * 128, (hp + 1) * 128)
                        P.mm(psGZ[:, 256 + hp * 128:256 + (hp + 1) * 128], AL[:, hs], k16("ident"))
                    yield
                    P.stt(v3(Tt), psGZ[:, 256:512].rearrange("p (h c) -> p h c", h=2), -1.0, idb, ALU.mult, ALU.add)
                    yield
                    continue
                for hp in range(2):
                    hs = slice(hp * 128, (hp + 1) * 128)
                    P.mm(psGZ[:, 256 + hp * 128:256 + (hp + 1) * 128], AL[:, hs], Tt[:, hs])
                yield
                P.stt(v3(IZ), psGZ[:, 256:512].rearrange("p (h c) -> p h c", h=2), -1.0, idb, ALU.mult, ALU.add)
                yield
                for hp in range(2):
                    hs = slice(hp * 128, (hp + 1) * 128)
                    if lv < 5:
                        P.mm(psT[:, hs], IZ[:, hs], Tm[:, hs])
                    P.mm(psT[:, 256 + hp * 128:256 + (hp + 1) * 128], Tm[:, hs], IZ[:, hs])
                yield
                if lv < 5:
                    P.cp(TTS[si][:, :], psT[:, :], eng="act")
                else:
                    P.cp(Tt[:, :], psT[:, 256:512], eng="act")
                yield

        WT2 = [H[6], H[7]]
        U2 = [F[4], F[5]]
        KDs = [[KD0, H[3]], [H[8], H[9]]]
        AQT2 = [SMB[25], SMB[26]]
        QD2 = [SMB[27], SMB[28]]

        def post(n, t, si):
            gt = 4 * n + t
            tc = slice(t * 128, (t + 1) * 128)
            KNZ, QNZ, D, DT, A, AQT, QD, AL, IZ, TQ, EGB = SMB[3 + 11 * si: 3 + 11 * si + 11]
            Tm, Tt = TTS[si][:, 0:256], TTS[si][:, 256:512]
            pst = PS[2 + si]
            P.tr(pst[:, 0:64].bitcast(BF16), KN[:, tc], k16("ident"))
            P.tr(pst[:, 64:128].bitcast(BF16), VT[:, tc], k16("ident"))
            kt_ = pst[:, 0:64].bitcast(BF16)
            vt_ = pst[:, 64:128].bitcast(BF16)
            KD = KDs[si]
            VBt = H[4]
            KDall = H[5]
            P.cp(AQT2[si][:, :], AQT[:, :], eng="pool")
            P.cp(QD2[si][:, 0:128], QD[:, 0:128], eng="pool")
            yield
            for hp in range(2):
                cs = slice(hp * 64, (hp + 1) * 64)
                P.ts(KBGZ[:, hp * 128 + hp * 64: hp * 128 + hp * 64 + 64], kt_[:, cs], BEG[:, gt * 2 + hp:gt * 2 + hp + 1], None, ALU.mult)
                P.ts(KDall[:, cs], kt_[:, cs], EGLG[:, gt * 2 + hp:gt * 2 + hp + 1], None, ALU.mult)
                P.ts(VBt[:, cs], vt_[:, cs], BETA[:, gt * 2 + hp:gt * 2 + hp + 1], None, ALU.mult)
            for hf in range(2):
                P.ts(KD[hf][:, 0:128], KDall[:, 0:128], k32("rm")[:, hf:hf + 1], None, ALU.mult, eng="pool")
            for hp in range(2):
                P.mm(pst[:, 128:256], KBGZ[:, hp * 128:(hp + 1) * 128], Tt[:, hp * 128:(hp + 1) * 128], start=(hp == 0), stop=(hp == 1))
            for hp in range(2):
                P.mm(pst[:, 256 + hp * 64:256 + (hp + 1) * 64], Tt[:, hp * 128:(hp + 1) * 128], VBt[:, hp * 64:(hp + 1) * 64])
            yield
            P.cp(WT2[si][:, 0:128], pst[:, 128:256], eng="act")
            P.cp(U2[si][:, 0:128], pst[:, 256:384], eng="act")
            yield

        def scan_pair(n, tp, pso):
            for si in range(2):
                t = 2 * tp + si
                gt = 4 * n + t
                WT, U, KD, AQT, QD = WT2[si], U2[si], KDs[si], AQT2[si], QD2[si]
                for hf in range(2):
                    rows = slice(hf * 64, hf * 64 + 64)
                    ic = slice(hf * 64, hf * 64 + 64)
                    M = 64 if hf == 0 else 128
                    psws = PS[7]
                    P.mm(psws[0:M, 0:128], WT[:, 0:M], SSB[:, :])
                    yield
                    for hp in range(2):
                        cs = slice(hp * 64, (hp + 1) * 64)
                        P.tt(VNZ[rows, hp * 128 + hp * 64: hp * 128 + hp * 64 + 64], U[rows, cs], psws[rows, cs], ALU.subtract)
                    yield
                    oc = slice(t * 128 + hf * 64, t * 128 + hf * 64 + 64)
                    P.mm(pso[:, oc], SSB[:, :], QD[:, ic], start=True, stop=False)
                    for hp in range(2):
                        P.mm(pso[:, oc], VNZ[:, hp * 128:(hp + 1) * 128], AQT[:, hp * 128 + hf * 64: hp * 128 + hf * 64 + 64],
                             start=False, stop=(hp == 1))
                    pss = PS[7]
                    for hp in range(2):
                        P.mm(pss[:, 128:256], KD[hf][:, 0:128], VNZ[:, hp * 128:(hp + 1) * 128], start=(hp == 0), stop=(hp == 1))
                    yield
                    ch = gt * 2 + hf
                    for hp in range(2):
                        r = slice(hp * 64, hp * 64 + 64)
                        P.stt(SS[r, r], SS[r, r], DL[r, ch:ch + 1], pss[r, 128 + hp * 64:128 + hp * 64 + 64], ALU.mult, ALU.add)
                    yield
                    P.cp(SSB[:, :], SS[:, :], eng="act")
                    yield
            if tp == 1:
                cols = slice(n * 512, (n + 1) * 512)
                head_norm_gate(l, pso, par(l, "gdng"), Y[:, yblk, cols])

        def chain(*gs):
            for g_ in gs:
                yield from g_

        pso = PS[4]
        prev = None
        for n in range(NB):
            cols = slice(n * 512, (n + 1) * 512)
            for bi, (w_, dst) in enumerate(((wq, QN), (wk, KN), (wv, VT))):
                ps = PS[5 + (bi % 2)]
                proj_cm(ps, w_, HT, cols)
                pc = PC[bi]
                P.cp(pc[:, 3:515], ps[:, :], eng="act")
                if bi == 2:
                    P.ts(ACC[:, :], pc[:, 0:512], convw(bi, 0), None, ALU.mult)
                    for j in range(1, 4):
                        P.stt(ACC[:, :], pc[:, j:j + 512], convw(bi, j), ACC[:, :], ALU.mult, ALU.add)
                    P.cp(pc[:, 0:3], pc[:, 512:515])
                    P.act(VT[:, :], ACC[:, :], AF.Silu)
                else:
                    psc = PS[bi % 2]
                    for j in range(4):
                        P.mm(psc[:, :], DIAG[bi * 4 + j], pc[:, j:j + 512], start=(j == 0), stop=(j == 3))
                    P.cp(pc[:, 0:3], pc[:, 512:515], eng="pool")
                    P.act(CQ[:, :], psc[:, :], AF.Silu)
                    P.act(SQ[:, :], CQ[:, :], AF.Square)
                    P.mm(PS[7][:, :], k32("blk64"), SQ[:, :])
                    rsqrt_from(RS[:, :], PS[7][:, :], 1.0, EPS)
                    P.stt(dst[:, :], CQ[:, :], float(HD ** -0.5) if bi == 0 else 1.0, RS[:, :], ALU.mult, ALU.mult)
            for tp in range(2):
                gens = [chain(prepinv(n, 2 * tp + si, si), post(n, 2 * tp + si, si)) for si in range(2)]
                alive = list(gens)
                while alive:
                    for g_ in list(alive):
                        try:
                            next(g_)
                        except StopIteration:
                            alive.remove(g_)
                    if prev is not None:
                        try:
                            next(prev)
                        except StopIteration:
                            prev = None
                if prev is not None:
                    for _ in prev:
                        pass
                prev = scan_pair(n, tp, pso)
        for _ in prev:
            pass

    ONESB = P.sb("ONESB", [128, 128], BF16)
    P.memset(ONESB[:, :], 1.0)

    def outproj(l):
        KTb = KT if KTs is None else KTs
        OB = [F[i][:, :] for i in range(7)] + [KTb[:, 0:1024].bitcast(F32)]
        wviews = []
        for d in range(8):
            if d < 4:
                w_ = load_w(l, ("out", "w", d))
                wviews.append([w_[:, k, :] for k in range(8)])
            else:
                ha, hb = H[2 + 2 * (d - 4)], H[3 + 2 * (d - 4)]
                src = wD[l, CMB[("out", "w", d)]]
                P.dma(ha[:, :], src[:, 0:512], q="pool")
                P.dma(hb[:, :], src[:, 512:1024], q="pool")
                wviews.append([(ha if k < 4 else hb)[:, (k % 4) * 128:(k % 4 + 1) * 128] for k in range(8)])
        RSB = [VB[:, 0:1024].bitcast(F32), VB[:, 1024:2048].bitcast(F32)]

        def proj_d(n, d):
            cols = slice(n * 512, (n + 1) * 512)
            ps = PS[5 + (d % 2)]
            for k in range(8):
                P.mm(ps[:, :], wviews[d][k], Y[:, k, cols], start=(k == 0), stop=(k == 7))
            P.cp(OB[d], ps[:, :], eng="act")
            sq = H[d % 2]
            P.act(sq[:, :], ps[:, :], AF.Square)
            P.mm(PS[n % 2][:, :], ONESB[:, :], sq[:, :], start=(d == 0), stop=(d == 7))

        def post_d(n, d):
            cols = slice(n * 512, (n + 1) * 512)
            rs = RSB[n % 2]
            P.stt(OB[d], OB[d], par(l, "postg", d), rs, ALU.mult, ALU.mult)
            P.tt(XT[:, d, cols], XT[:, d, cols], OB[d], ALU.add)

        for d in range(8):
            proj_d(0, d)
        for n in range(NB):
            rsqrt_from(RSB[n % 2], PS[n % 2][:, :], 1.0 / D_MODEL, EPS)
            for d in range(8):
                post_d(n, d)
                if n + 1 < NB:
                    proj_d(n + 1, d)

    for s in range(NSEQ):
        for c in range(8):
            P.dma(XT[:, c, :], xD[s, :, c * T:(c + 1) * T])
        for l in range(DEPTH):
            P.mark("prenorm s%d l%d" % (s, l))
            prenorm(l)
            P.memset(Y[:, :, :], 0.0) if (len(branches) < 4) else None
            for p in range(2):
                if "gdn" in branches:
                    P.mark("gdn s%d l%d p%d" % (s, l, p))
                    gdn(l, p)
                if "hgrn" in branches:
                    P.mark("hgrn s%d l%d p%d" % (s, l, p))
                    hgrn(l, p)
                if "moba" in branches:
                    P.mark("moba s%d l%d p%d" % (s, l, p))
                    attention(l, p, "moba")
                if "diff" in branches:
                    P.mark("diff s%d l%d p%d" % (s, l, p))
                    attention(l, p, "diff")
            if tap:
                TP = F
                for c in range(8):
                    for n in range(NB):
                        cols = slice(n * 512, (n + 1) * 512)
                        P.cp(TP[(c * NB + n) % 4][:, :], Y[:, c, cols])
                        P.dma(tapD[s, l, :, c * T + n * 512: c * T + (n + 1) * 512], TP[(c * NB + n) % 4][:, :])
            P.mark("outproj s%d l%d" % (s, l))
            outproj(l)
        for c in range(8):
            P.dma(yD[s, :, c * T:(c + 1) * T], XT[:, c, :])
    P.mark("end")
    P.final_wait("sp", [XT] + ([F[0], F[1], F[2], F[3]] if tap else []))
    P.emit()
    return nc, P


_CACHE = {}


def _prep_inputs(inp, T, nseq_total):
    x = np.asarray(inp["x"], np.float32)
    B = x.shape[0]
    xT = np.ascontiguousarray(x.reshape(B, T, 8, 128).transpose(0, 3, 2, 1)).reshape(B, 128, 8 * T)
    w = pack_weights(np.asarray(inp["w_in"], np.float32), np.asarray(inp["w_out"], np.float32))
    par = pack_params({k: np.asarray(v, np.float32) for k, v in inp.items()})
    c32, c16 = make_consts()
    return xT, w, par, c32, c16


def kernel(**inputs):
    x = np.asarray(inputs["x"])
    B, T, D = x.shape
    depth = inputs["w_in"].shape[0]
    nseq = B // N_CORES
    key = (T, nseq, depth)
    if key not in _CACHE:
        _CACHE[key] = build(T=T, NSEQ=nseq, DEPTH=depth)[0]
    nc = _CACHE[key]
    xT, w, par, c32, c16 = _prep_inputs(inputs, T, B)
    in_maps = []
    for c in range(N_CORES):
        in_maps.append({"x": xT[c * nseq:(c + 1) * nseq], "w": w, "c32": c32, "c16": c16, "par": par})
    res = run_bass_kernel_spmd(nc, in_maps, core_ids=list(range(N_CORES)))
    outs = []
    for c in range(N_CORES):
        yT = np.asarray(res.results[c]["y"]).reshape(nseq, 128, 8, T)
        outs.append(yT.transpose(0, 3, 2, 1).reshape(nseq, T, D))
    return np.concatenate(outs, axis=0).astype(np.float32)
```

```python
import math
import numpy as np
import concourse.bass as bass
import concourse.mybir as mybir
from concourse.bass_utils import run_bass_kernel_spmd

F32 = mybir.dt.float32
BF16 = mybir.dt.bfloat16
AF = mybir.ActivationFunctionType
ALU = mybir.AluOpType
AX = mybir.AxisListType

D_MODEL = 1024
HD = 64
EPS = 1e-6
BIG = 30000.0
N_CORES = 8


class View:
    def __init__(self, buf, ap):
        self.buf = buf
        self.ap = ap

    def __getitem__(self, k):
        return View(self.buf, self.ap[k])

    def rearrange(self, *a, **k):
        return View(self.buf, self.ap.rearrange(*a, **k))

    def to_broadcast(self, *a, **k):
        return View(self.buf, self.ap.to_broadcast(*a, **k))

    def unsqueeze(self, *a, **k):
        return View(self.buf, self.ap.unsqueeze(*a, **k))

    def bitcast(self, *a, **k):
        return View(self.buf, self.ap.bitcast(*a, **k))


class Buf:
    def __init__(self, t, name):
        self.t = t
        self.name = name
        self.last_w = None
        self.reads = {}
        self.dma_sem = None
        self.is_psum = False

    def __getitem__(self, k):
        return View(self, self.t[k])


class Prog:
    ENG = ("pe", "act", "dve", "pool", "sp")

    def __init__(self, nc):
        self.nc = nc
        self.streams = {e: [] for e in self.ENG}
        self.sems = {}
        self.cnt = {}
        self.waited = {e: {} for e in self.ENG}
        self.marks = []
        for e in ("pe", "act", "dve", "pool"):
            self.sems[e] = nc.alloc_semaphore("s_" + e)
            self.cnt[e] = 0

    def mark(self, label):
        self.marks.append((label, dict(self.cnt)))

    def sb(self, name, shape, dtype=F32):
        return Buf(self.nc.alloc_sbuf_tensor(name, list(shape), dtype), name)

    def ps(self, name, shape, dtype=F32):
        b = Buf(self.nc.alloc_psum_tensor(name, list(shape), dtype), name)
        b.is_psum = True
        return b

    def dram(self, ap, name):
        return Buf(ap, name)

    def _deps(self, eng, reads, writes):
        need = {}

        def add(k, c):
            if k == "pe" and eng == "pe":
                return
            if need.get(k, 0) < c:
                need[k] = c
        for b in reads:
            if b.last_w is not None:
                add(*b.last_w)
            if b.is_psum:
                for k, c in b.reads.items():
                    if k != eng:
                        add(k, c)
        for b in writes:
            if b.last_w is not None:
                add(*b.last_w)
            for k, c in b.reads.items():
                add(k, c)
        out = []
        w = self.waited[eng]
        for k, c in need.items():
            if w.get(k, 0) < c:
                w[k] = c
                out.append((k, c))
        return out

    def op(self, eng, fn, reads=(), writes=()):
        reads = list(dict.fromkeys(reads))
        writes = list(dict.fromkeys(writes))
        waits = self._deps(eng, reads, writes)
        self.cnt[eng] += 1
        c = self.cnt[eng]
        self.streams[eng].append((waits, fn, (eng, 1)))
        for b in reads:
            b.reads[eng] = c
        for b in writes:
            b.last_w = (eng, c)
            b.reads = {}

    def dma(self, out, in_, q="sp"):
        ob, ib = out.buf, in_.buf
        owner = ob if ob.dma_sem is not None else (ib if ib.dma_sem is not None else ob)
        if owner.dma_sem is None:
            key = "dma%d" % len(self.sems)
            self.sems[key] = self.nc.alloc_semaphore(key)
            self.cnt[key] = 0
            owner.dma_sem = key
        key = owner.dma_sem
        waits = self._deps(q, [ib], [ob])
        self.cnt[key] += 16
        c = self.cnt[key]
        oa, ia = out.ap, in_.ap
        self.streams[q].append((waits, lambda e: e.dma_start(out=oa, in_=ia), (key, 16)))
        ib.reads[key] = c
        ob.last_w = (key, c)
        ob.reads = {}

    def final_wait(self, eng, bufs):
        waits = self._deps(eng, bufs, bufs)
        self.streams[eng].append((waits, None, None))

    def emit(self):
        nc = self.nc
        with nc.Block() as block:
            def mk(ename):
                def body(e):
                    for waits, fn, inc in self.streams[ename]:
                        for k, c in waits:
                            e.wait_ge(self.sems[k], c)
                        if fn is None:
                            continue
                        ins = fn(e)
                        ins.then_inc(self.sems[inc[0]], inc[1])
                return body
            if self.streams["pe"]:
                block.tensor(mk("pe"))
            if self.streams["act"]:
                block.scalar(mk("act"))
            if self.streams["dve"]:
                block.vector(mk("dve"))
            if self.streams["pool"]:
                block.gpsimd(mk("pool"))
            if self.streams["sp"]:
                block.sync(mk("sp"))

    def mm(self, out, lhsT, rhs, start=True, stop=True):
        o, l, r = out.ap, lhsT.ap, rhs.ap
        self.op("pe", lambda e: e.matmul(o, l, r, start=start, stop=stop),
                reads=[lhsT.buf, rhs.buf], writes=[out.buf])

    def tr(self, out, in_, ident):
        o, i, d = out.ap, in_.ap, ident.ap
        self.op("pe", lambda e: e.transpose(o, i, d), reads=[in_.buf, ident.buf], writes=[out.buf])

    def act(self, out, in_, func, bias=None, scale=1.0, eng="act"):
        o, i = out.ap, in_.ap
        rd = [in_.buf]
        kw = {}
        if isinstance(bias, View):
            rd.append(bias.buf)
            kw["bias"] = bias.ap
        elif bias is not None:
            kw["bias"] = float(bias)
        if isinstance(scale, View):
            rd.append(scale.buf)
            kw["scale"] = scale.ap
        else:
            kw["scale"] = float(scale)
        self.op("act", lambda e: e.activation(o, i, func, **kw), reads=rd, writes=[out.buf])

    def tt(self, out, in0, in1, op, eng="dve"):
        o, a, b = out.ap, in0.ap, in1.ap
        self.op(eng, lambda e: e.tensor_tensor(o, a, b, op), reads=[in0.buf, in1.buf], writes=[out.buf])

    def ts(self, out, in0, s1, s2, op0, op1=None, eng="dve"):
        o, a = out.ap, in0.ap
        rd = [in0.buf]
        v1 = s1
        if isinstance(s1, View):
            rd.append(s1.buf)
            v1 = s1.ap
        v2 = s2
        if isinstance(s2, View):
            rd.append(s2.buf)
            v2 = s2.ap
        if op1 is None:
            self.op(eng, lambda e: e.tensor_scalar(o, a, v1, None, op0), reads=rd, writes=[out.buf])
        else:
            self.op(eng, lambda e: e.tensor_scalar(o, a, v1, v2, op0, op1), reads=rd, writes=[out.buf])

    def stt(self, out, in0, scalar, in1, op0, op1, eng="dve"):
        o, a, b = out.ap, in0.ap, in1.ap
        rd = [in0.buf, in1.buf]
        sv = scalar
        if isinstance(scalar, View):
            rd.append(scalar.buf)
            sv = scalar.ap
        self.op(eng, lambda e: e.scalar_tensor_tensor(o, a, sv, b, op0, op1), reads=rd, writes=[out.buf])

    def cp(self, out, in_, eng="dve"):
        o, i = out.ap, in_.ap
        if eng == "act":
            self.op("act", lambda e: e.copy(o, i), reads=[in_.buf], writes=[out.buf])
        else:
            self.op(eng, lambda e: e.tensor_copy(o, i), reads=[in_.buf], writes=[out.buf])

    def memset(self, out, val, eng="dve"):
        o = out.ap
        self.op(eng, lambda e: e.memset(o, val), writes=[out.buf])

    def red(self, out, in_, op, eng="dve"):
        o, i = out.ap, in_.ap
        self.op(eng, lambda e: e.tensor_reduce(o, i, AX.X, op), reads=[in_.buf], writes=[out.buf])

    def recip(self, out, in_):
        o, i = out.ap, in_.ap
        self.op("dve", lambda e: e.reciprocal(o, i), reads=[in_.buf], writes=[out.buf])

    def scan(self, out, d0, d1):
        o, a, b = out.ap, d0.ap, d1.ap
        self.op("dve", lambda e: e.tensor_tensor_scan(o, a, b, 0.0, ALU.mult, ALU.add),
                reads=[d0.buf, d1.buf], writes=[out.buf])


C32 = {}
_o = 0
for _n, _w in (("ident", 128), ("blk64", 128), ("blktri", 128), ("scanm", 512), ("selden", 64), ("place", 256), ("e2", 2), ("rm", 2), ("arel", 16)):
    C32[_n] = (_o, _w)
    _o += _w
NC32 = _o
C16 = {}
_o = 0
for _n, _w in (("ident", 128), ("lvl", 6 * 128), ("caus01", 128), ("causneg", 128), ("hm2", 2), ("hm4", 4),
               ("ka", 16 * 128), ("oh", 16 * 128), ("qab", 512), ("m01", 128), ("m01T", 128)):
    C16[_n] = (_o, _w)
    _o += _w
NC16 = _o


def make_consts():
    c32 = np.zeros((128, NC32), np.float32)
    c16 = np.zeros((128, NC16), np.float32)
    p = np.arange(128)[:, None]
    c = np.arange(128)[None, :]
    same = (p // 64) == (c // 64)

    def put(dst, tab, name, arr):
        o, w = tab[name]
        dst[:, o:o + w] = arr
    put(c32, C32, "ident", (p == c).astype(np.float32))
    put(c32, C32, "blk64", same.astype(np.float32))
    put(c32, C32, "blktri", (same & (p <= c)).astype(np.float32))
    sm = np.ones((128, 512), np.float32)
    sm[:, ::64] = 0.0
    put(c32, C32, "scanm", sm)
    sd = np.zeros((128, 64), np.float32)
    sd[64, :] = 1.0
    put(c32, C32, "selden", sd)
    pl = np.zeros((128, 256), np.float32)
    for r in range(64):
        pl[r, r] = 1.0
        pl[r, 128 + 64 + r] = 1.0
    put(c32, C32, "place", pl)
    e2 = np.zeros((128, 2), np.float32)
    e2[0, 0] = 1.0
    e2[64, 1] = 1.0
    put(c32, C32, "e2", e2)
    arel = np.zeros((128, 16), np.float32)
    for r in range(16):
        arel[:, r] = np.arange(128) + 128.0 * (r - 12) - 256.0
    put(c32, C32, "arel", arel)
    rm = np.zeros((128, 2), np.float32)
    rm[:64, 0] = 1.0
    rm[64:, 1] = 1.0
    put(c32, C32, "rm", rm)

    put(c16, C16, "ident", (p == c).astype(np.float32))
    lv = np.zeros((128, 6, 128), np.float32)
    for l in range(1, 7):
        s = 2 ** l
        lv[:, l - 1, :] = ((p // s) == (c // s)) & ((p % s) >= s // 2) & ((c % s) < s // 2)
    put(c16, C16, "lvl", lv.reshape(128, 768))
    put(c16, C16, "m01", (same & (p >= c)).astype(np.float32))
    put(c16, C16, "m01T", (same & (c >= p)).astype(np.float32))
    put(c16, C16, "caus01", (p <= c).astype(np.float32))
    put(c16, C16, "causneg", np.where(p > c, -BIG, 0.0))
    hm2 = np.zeros((128, 2), np.float32)
    hm2[:64, 0] = 1.0
    hm2[64:, 1] = 1.0
    put(c16, C16, "hm2", hm2)
    hm4 = np.zeros((128, 4), np.float32)
    for g in range(4):
        hm4[g * 32:(g + 1) * 32, g] = 1.0
    put(c16, C16, "hm4", hm4)
    ka = np.zeros((128, 16, 128), np.float32)
    for ri in range(16):
        ka[0, ri, :] = np.arange(128)
        ka[1, ri, :] = 1.0
        ka[2, ri, :] = 128.0 * (ri - 12)
        ka[3, ri, :] = 1.0
    put(c16, C16, "ka", ka.reshape(128, 2048))
    oh = np.zeros((128, 16, 128), np.float32)
    for r in range(16):
        oh[r, r, :] = 1.0
    put(c16, C16, "oh", oh.reshape(128, 2048))
    qab = np.zeros((128, 512), np.float32)
    il = np.arange(512)
    qab[0, :] = 1.0
    qab[1, :] = -(il % 128)
    qab[2, :] = 1.0
    qab[3, :] = -128.0 * (il // 128)
    put(c16, C16, "qab", qab)
    return c32, c16


PAR = {}
_o = 0
for _n, _w in (("preg", 8), ("postg", 8), ("conv", 24), ("gdng", 1), ("hgrng", 1), ("diffg", 1),
               ("lbl", 2), ("lb0", 2), ("alog", 4), ("dtb", 4), ("lq1", 32), ("lk1", 32), ("lq2", 32), ("lk2", 32)):
    PAR[_n] = (_o, _w)
    _o += _w
NPAR = _o

CMB = {}
_i = 0
for _br, _names in (("gdn", ("q", "k", "v", "z")), ("hgrn", ("q", "f", "z")), ("moba", ("q", "k", "z")),
                    ("diff", ("q", "k", "z"))):
    for _nm in _names:
        for _p in range(2):
            CMB[(_br, _nm, _p)] = _i
            _i += 1
for _br in ("gdn", "hgrn", "moba", "diff"):
    for _p in range(2):
        CMB[(_br, "tm", _p)] = _i
        _i += 1
for _d in range(8):
    CMB[("out", "w", _d)] = _i
    _i += 1
NWB = _i

GDN_BASE, HGRN_BASE, MOBA_BASE, DIFF_BASE = 0, 1032, 2056, 3080


def pack_weights(w_in, w_out):
    depth = w_in.shape[0]
    out = np.zeros((depth, NWB, 128, 1024), np.float32)

    def blockify(wcols):
        return wcols.reshape(8, 128, 128).transpose(1, 0, 2).reshape(128, 1024)
    for l in range(depth):
        W = w_in[l]
        def cols(base, off, p):
            return W[:, base + off + p * 128: base + off + (p + 1) * 128]
        for p in range(2):
            out[l, CMB[("gdn", "q", p)]] = blockify(cols(GDN_BASE, 0, p))
            out[l, CMB[("gdn", "k", p)]] = blockify(cols(GDN_BASE, 256, p))
            out[l, CMB[("gdn", "v", p)]] = blockify(cols(GDN_BASE, 512, p))
            out[l, CMB[("gdn", "z", p)]] = blockify(cols(GDN_BASE, 776, p))
            ab = np.zeros((1024, 128), np.float32)
            ab[:, 0:2] = W[:, GDN_BASE + 768 + 2 * p: GDN_BASE + 768 + 2 * p + 2]
            ab[:, 2:4] = W[:, GDN_BASE + 772 + 2 * p: GDN_BASE + 772 + 2 * p + 2]
            out[l, CMB[("gdn", "tm", p)]] = blockify(ab)
            out[l, CMB[("hgrn", "q", p)]] = blockify(cols(HGRN_BASE, 0, p))
            out[l, CMB[("hgrn", "f", p)]] = blockify(cols(HGRN_BASE, 256, p))
            out[l, CMB[("hgrn", "tm", p)]] = blockify(cols(HGRN_BASE, 512, p))
            out[l, CMB[("hgrn", "z", p)]] = blockify(cols(HGRN_BASE, 768, p))
            for br, base in (("moba", MOBA_BASE), ("diff", DIFF_BASE)):
                out[l, CMB[(br, "q", p)]] = blockify(cols(base, 0, p))
                out[l, CMB[(br, "k", p)]] = blockify(cols(base, 256, p))
                out[l, CMB[(br, "tm", p)]] = blockify(cols(base, 512, p))
                out[l, CMB[(br, "z", p)]] = blockify(cols(base, 768, p))
        for d in range(8):
            out[l, CMB[("out", "w", d)]] = blockify(w_out[l][:, d * 128:(d + 1) * 128])
    return out


def pack_params(inp):
    depth = inp["pre_norm_g"].shape[0]
    par = np.zeros((depth, 128, NPAR), np.float32)

    def put(l, name, arr):
        o, w = PAR[name]
        par[l, :, o:o + w] = arr
    for l in range(depth):
        put(l, "preg", inp["pre_norm_g"][l].reshape(8, 128).T)
        put(l, "postg", inp["post_norm_g"][l].reshape(8, 128).T)
        cw = inp["conv_w"][l]
        put(l, "conv", cw.reshape(4, 6, 128).transpose(2, 1, 0).reshape(128, 24))
        put(l, "gdng", np.tile(inp["gdn_norm_g"][l], 2)[:, None])
        put(l, "hgrng", np.tile(inp["hgrn_norm_g"][l], 2)[:, None])
        put(l, "diffg", np.tile(inp["diff_norm_g"][l], 2)[:, None])
        put(l, "lbl", inp["hgrn_lb"][l].reshape(2, 128).T)
        put(l, "lb0", inp["hgrn_lb"][0].reshape(2, 128).T)
        put(l, "alog", np.broadcast_to(inp["gdn_a_log"][l][None, :], (128, 4)))
        put(l, "dtb", np.broadcast_to(inp["gdn_dt_bias"][l][None, :], (128, 4)))
        put(l, "lq1", np.broadcast_to(inp["diff_lq1"][l][None, :], (128, 32)))
        put(l, "lk1", np.broadcast_to(inp["diff_lk1"][l][None, :], (128, 32)))
        put(l, "lq2", np.broadcast_to(inp["diff_lq2"][l][None, :], (128, 32)))
        put(l, "lk2", np.broadcast_to(inp["diff_lk2"][l][None, :], (128, 32)))
    return par


def build(T=2048, NSEQ=2, DEPTH=2, branches=("gdn", "hgrn", "moba", "diff"), tap=False):
    assert T % 512 == 0
    NT = T // 128
    NB = T // 512
    nc = bass.Bass("TRN2", target_bir_lowering=False)
    x_d = nc.dram_tensor("x", [NSEQ, 128, 8 * T], F32, kind="ExternalInput").ap()
    w_d = nc.dram_tensor("w", [DEPTH, NWB, 128, 1024], F32, kind="ExternalInput").ap()
    c32_d = nc.dram_tensor("c32", [128, NC32], F32, kind="ExternalInput").ap()
    c16_d = nc.dram_tensor("c16", [128, NC16], F32, kind="ExternalInput").ap()
    par_d = nc.dram_tensor("par", [DEPTH, 128, NPAR], F32, kind="ExternalInput").ap()
    y_d = nc.dram_tensor("y", [NSEQ, 128, 8 * T], F32, kind="ExternalOutput").ap()
    if tap:
        tap_d = nc.dram_tensor("tap", [NSEQ, DEPTH, 128, 8 * T], F32, kind="ExternalOutput").ap()

    P = Prog(nc)
    xD, wD, c32D, c16D, parD, yD = (P.dram(x_d, "x"), P.dram(w_d, "w"), P.dram(c32_d, "c32"),
                                    P.dram(c16_d, "c16"), P.dram(par_d, "par"), P.dram(y_d, "y"))
    tapD = P.dram(tap_d, "tap") if tap else None
    XT = P.sb("XT", [128, 8, T])
    HT = P.sb("HT", [128, 8, T], BF16)
    Y = P.sb("Y", [128, 8, T], BF16)
    K32 = P.sb("K32", [128, NC32])
    K16 = P.sb("K16", [128, NC16], BF16)
    PARS = [P.sb("PAR%d" % l, [128, NPAR]) for l in range(DEPTH)]
    NWS = 4
    WS = [P.sb("WS%d" % i, [128, 8, 128], BF16) for i in range(NWS)]
    NF, NH = 7, 10
    F = [P.sb("F%d" % i, [128, 512]) for i in range(NF)]
    H = [P.sb("H%d" % i, [128, 512], BF16) for i in range(NH)]
    SMB = [P.sb("SMB%d" % i, [128, 256], BF16) for i in range(29)]
    TTS = [P.sb("TT%d" % i, [128, 512], BF16) for i in range(2)]
    SMF = [P.sb("SMF%d" % i, [128, 256]) for i in range(2)]
    KT = P.sb("KT", [128, T], BF16)
    VB = P.sb("VB", [128, max(NT * 2 * 65, 2080)], BF16)
    KTs = P.sb("KTs", [128, max(T, 1040)], BF16) if T < 1040 else None
    TOK = [P.sb("TOK%d" % i, [128, NT * 4 if i == 0 else NT * 2]) for i in range(10)]
    SS = P.sb("SS", [128, 128])
    SSB = P.sb("SSB", [128, 128], BF16)
    SSB2 = P.sb("SSB2", [128, 128], BF16)
    COL = P.sb("COL", [128, 16])
    KS = P.sb("KS", [128, 2, 8])
    KSr = P.sb("KSr", [128, 8])
    DL = P.sb("DL", [128, NT * 2])
    ABI = P.sb("ABI", [128, 2, 16])
    PS = [P.ps("PS%d" % i, [128, 512]) for i in range(8)]

    def k32(name):
        o, w = C32[name]
        return K32[:, o:o + w]

    def k16(name):
        o, w = C16[name]
        return K16[:, o:o + w]

    P.dma(K32[:, :], c32D[:, :])
    P.dma(K16[:, :], c16D[:, :], q="pool")
    for l in range(DEPTH):
        P.dma(PARS[l][:, :], parD[l])

    wstate = {"next_slot": 0}

    def load_w(l, key):
        s = WS[wstate["next_slot"] % NWS]
        wstate["next_slot"] += 1
        P.dma(s[:, :, :], wD[l, CMB[key]].rearrange("p (k c) -> p k c", k=8), q="pool")
        return s

    def par(l, name, j=0, w=1):
        o, _ = PAR[name]
        return PARS[l][:, o + j:o + j + w]

    def rsqrt_from(out, in_, scale, eps):
        P.act(out, in_, AF.Ln, bias=eps, scale=scale)
        P.act(out, out, AF.Exp, scale=-0.5)

    def proj_cm(ps, wslot, src, cols):
        for k in range(8):
            P.mm(ps[:, :], wslot[:, k, :], src[:, k, cols], start=(k == 0), stop=(k == 7))

    def head_norm_gate(l, ops, gcol, yv, extra=1.0):
        osb, sq, rs = F[4], F[5], F[6]
        P.cp(osb[:, :], ops[:, :], eng="act")
        P.act(sq[:, :], ops[:, :], AF.Square)
        P.mm(PS[7][:, :], k32("blk64"), sq[:, :])
        rsqrt_from(rs[:, :], PS[7][:, :], 1.0 / HD, EPS)
        P.stt(osb[:, :], osb[:, :], gcol, rs[:, :], ALU.mult, ALU.mult)
        if extra != 1.0:
            P.stt(yv, osb[:, :], float(extra), yv, ALU.mult, ALU.mult)
        else:
            P.tt(yv, osb[:, :], yv, ALU.mult)

    def z_gate(l, wz, yblk):
        for n in range(NB):
            cols = slice(n * 512, (n + 1) * 512)
            ps = PS[5 + (n % 2)]
            proj_cm(ps, wz, HT, cols)
            P.act(Y[:, yblk, cols], ps[:, :], AF.Silu)

    def prenorm(l):
        for n in range(NB):
            cols = slice(n * 512, (n + 1) * 512)
            for k in range(8):
                sq = H[k % 2]
                P.act(sq[:, :], XT[:, k, cols], AF.Square)
                P.mm(PS[n % 2][:, :], ONESB[:, :], sq[:, :], start=(k == 0), stop=(k == 7))
            rs = F[2 + (n % 2)]
            rsqrt_from(rs[:, :], PS[n % 2][:, :], 1.0 / D_MODEL, EPS)
            for k in range(8):
                P.stt(HT[:, k, cols], XT[:, k, cols], par(l, "preg", k), rs[:, :], ALU.mult, ALU.mult)

    def attention(l, p, kind):
        yblk = (4 if kind == "moba" else 6) + p
        slopes = [2.0 ** -(2 * h + 2) for h in range(4)] if kind == "moba" else [2.0 ** -(2 * h + 1) for h in range(4)]
        dh = 64 if kind == "moba" else 32
        scale = dh ** -0.5
        wq = load_w(l, (kind, "q", p))
        wk = load_w(l, (kind, "k", p))
        wz = load_w(l, (kind, "z", p))
        wv = load_w(l, (kind, "tm", p))
        z_gate(l, wz, yblk)
        if kind == "moba":
            P.memset(KSr[:, :], 0.0)
        for n in range(NB):
            cols = slice(n * 512, (n + 1) * 512)
            ps = PS[5 + (n % 2)]
            proj_cm(ps, wk, HT, cols)
            P.cp(KT[:, cols], ps[:, :], eng="act")
            if kind == "moba":
                P.cp(F[3][:, :], ps[:, :])
                P.red(KSr[:, 2 * n:2 * n + 2], F[3][:, :].rearrange("p (b c) -> p b c", c=256), ALU.add)
        if kind == "moba":
            for hp in range(2):
                P.ts(KS[:, hp, :], KSr[:, :], k32("rm")[:, hp:hp + 1], None, ALU.mult)
        VA = VB[:, 0:NT * 2 * 65].rearrange("p (t h c) -> p t h c", t=NT, h=2)
        P.memset(VA[:, :, :, 64:65], 1.0)
        for t in range(NT):
            ps = PS[5 + (t % 2)]
            for k in range(8):
                P.mm(ps[:, 0:128], HT[:, k, t * 128:(t + 1) * 128], wv[:, k, :], start=(k == 0), stop=(k == 7))
            P.cp(VA[:, t, :, 0:64], ps[:, 0:128].rearrange("p (h c) -> p h c", h=2), eng="act")
        if kind == "diff":
            lam_init = 0.8 - 0.6 * math.exp(-0.3 * l)
            tmp = SMF[0]
            P.tt(tmp[:, 0:32], par(l, "lq1", 0, 32), par(l, "lk1", 0, 32), ALU.mult)
            P.red(COL[:, 0:1], tmp[:, 0:32], ALU.add)
            P.tt(tmp[:, 32:64], par(l, "lq2", 0, 32), par(l, "lk2", 0, 32), ALU.mult)
            P.red(COL[:, 1:2], tmp[:, 32:64], ALU.add)
            P.act(COL[:, 2:4], COL[:, 0:2], AF.Exp)
            P.tt(COL[:, 4:5], COL[:, 3:4], COL[:, 2:3], ALU.subtract)
            P.ts(COL[:, 5:6], COL[:, 4:5], -lam_init, None, ALU.add)
            P.ts(COL[:, 6:7], par(l, "diffg"), 1.0 - lam_init, None, ALU.mult)
        nmask = 2 if kind == "moba" else 4
        mask_tab = k16("hm2") if kind == "moba" else k16("hm4")
        QZ = [H[0], H[1], H[2], H[3]]
        QA = [H[4], H[5]]
        PT = [H[6], H[7], H[8]]
        OC = [F[0], F[1]]
        RC = [F[2], F[3]]
        for b_ in OC:
            P.memset(b_[:, :], 0.0)
        for hp_, b_ in enumerate(QA):
            P.memset(b_[:, :], 0.0)
            P.ts(b_[0:4, :], k16("qab")[0:4, :], float(slopes[2 * p + hp_]), None, ALU.mult)
            P.ts(ABI[:, hp_, :], k32("arel"), float(slopes[2 * p + hp_]), None, ALU.mult)
        bias_mode = [slopes[2 * p + hp_] <= 2.0 ** -4 for hp_ in range(2)]
        SELT = H[9]
        P.memset(SELT[:, :], 0.0)
        nmap = 1 if kind == "moba" else 2
        GF = [SMB[i][:, :].bitcast(F32) for i in range(6)]

        def block_begin(n):
            cols = slice(n * 512, (n + 1) * 512)
            psq = PS[5]
            proj_cm(psq, wq, HT, cols)
            for m in range(nmask):
                P.stt(QZ[m][:, :], psq[:, :], float(scale), mask_tab[:, m:m + 1].to_broadcast([128, 512]),
                      ALU.mult, ALU.mult)
            if not (kind == "moba" and n >= 2):
                return
            q32 = F[1]
            P.cp(q32[:, :], psq[:, :], eng="act")
            W = 2 * n + 1
            psg = PS[7]
            for t in range(4):
                for hp in range(2):
                    P.mm(psg[:, (t * 2 + hp) * 8:(t * 2 + hp) * 8 + 8], q32[:, t * 128:(t + 1) * 128], KS[:, hp, :])
            g = GF[0]
            P.cp(g[:, 0:64], psg[:, 0:64])
            g3 = g[:, 0:64].rearrange("p (a b) -> p a b", b=8)
            P.memset(g3[:, 0:4, 2 * n:2 * n + 1], -1e30)
            cur = g3[:, :, 0:W]
            m_ = GF[1]
            for it in range(3):
                P.red(m_[:, 8 * it:8 * it + 8], cur, ALU.max)
                if it == 2:
                    break
                e_ = GF[2 + it]
                e3 = e_[:, 0:64].rearrange("p (a b) -> p a b", b=8)[:, :, 0:W]
                P.tt(e3, cur, m_[:, 8 * it:8 * it + 8].unsqueeze(2).to_broadcast([128, 8, W]), ALU.is_ge)
                P.stt(e3, e3, -1e30, cur, ALU.mult, ALU.add)
                cur = e3
            mv = GF[4]
            P.memset(mv[:, 0:64], 0.0)
            mv3 = mv[:, 0:64].rearrange("p (a b) -> p a b", b=8)
            P.tt(mv3[:, :, 0:W], g3[:, :, 0:W], m_[:, 16:24].unsqueeze(2).to_broadcast([128, 8, W]), ALU.is_ge)
            P.ts(mv3[:, :, 0:W], mv3[:, :, 0:W], BIG, -BIG, ALU.mult, ALU.add)
            P.memset(mv3[:, 0:4, 2 * n:2 * n + 1], 0.0)
            pst = PS[7]
            for t in range(4):
                P.tr(pst[0:16, t * 128:(t + 1) * 128], mv[:, t * 16:(t + 1) * 16], k32("ident"))
            P.cp(SELT[0:16, :], pst[0:16, :])

        jobs = [(n, hp, mp, jt) for n in range(NB) for hp in range(2) for mp in range(nmap) for jt in range(4 * n + 4)]
        deferred = []

        def emitA(job, gi):
            n, hp, mp, jt = job
            if hp == 0 and mp == 0 and jt == 0:
                block_begin(n)
            use_sel = (kind == "moba" and n >= 2)
            m = hp * nmap + mp if kind == "diff" else hp
            qa = QA[hp]
            c0 = max(0, jt - 4 * n) * 128
            sc = PS[gi % 3]
            pt = PT[gi % 3]
            blk = jt // 2
            masked = use_sel and blk <= 2 * n
            diag = jt >= 4 * n
            bm = bias_mode[hp]
            P.mm(sc[:, c0:512], KT[:, jt * 128:(jt + 1) * 128], QZ[m][:, c0:512], start=True,
                 stop=bm and not (masked or diag))
            if not bm:
                ka = k16("ka")[:, (jt - 4 * n + 12) * 128:(jt - 4 * n + 13) * 128]
                P.mm(sc[:, c0:512], ka, qa[:, c0:512], start=False, stop=not (masked or diag))
            if masked:
                oh = k16("oh")[:, (hp * 8 + blk) * 128:(hp * 8 + blk + 1) * 128]
                P.mm(sc[:, c0:512], oh, SELT[:, c0:512], start=False, stop=not diag)
            if diag:
                P.mm(sc[:, c0:c0 + 128], k16("ident"), k16("causneg"), start=False, stop=True)
            if bm:
                r_ = jt - 4 * n + 12
                P.act(pt[:, c0:512], sc[:, c0:512], AF.Exp, bias=ABI[:, hp, r_:r_ + 1])
            else:
                P.act(pt[:, c0:512], sc[:, c0:512], AF.Exp)

        def emitB(job, gi):
            n, hp, mp, jt = job
            njt = 4 * n + 4
            cols = slice(n * 512, (n + 1) * 512)
            c0 = max(0, jt - 4 * n) * 128
            pt = PT[gi % 3]
            acc = PS[3 + mp]
            P.mm(acc[0:65, c0:512], VA[:, jt, hp, :], pt[:, c0:512], start=(jt == 0), stop=(jt == njt - 1))
            if jt == njt - 1:
                oc = OC[mp]
                P.cp(oc[0:65, :], acc[0:65, :], eng="act")

                def f2(oc=oc, hp=hp, mp=mp, cols=cols):
                    P.mm(PS[7][0:64, :], k32("selden"), oc[:, :])
                    rc = RC[mp]
                    P.act(rc[0:64, :], PS[7][0:64, :], AF.Ln)
                    P.act(rc[0:64, :], rc[0:64, :], AF.Exp, scale=-1.0)
                    P.tt(oc[0:64, :], oc[0:64, :], rc[0:64, :], ALU.mult)
                    if mp == nmap - 1:
                        if kind == "diff":
                            P.stt(OC[0][0:64, :], OC[1][0:64, :], COL[0:64, 5:6], OC[0][0:64, :], ALU.mult, ALU.add)

                        def f5(hp=hp, cols=cols):
                            P.mm(PS[6][:, :], k32("place")[:, hp * 128:(hp + 1) * 128], OC[0][:, :],
                                 start=(hp == 0), stop=(hp == 1))
                            if hp == 1:
                                if kind == "moba":
                                    P.tt(Y[:, yblk, cols], PS[6][:, :], Y[:, yblk, cols], ALU.mult)
                                else:
                                    head_norm_gate(l, PS[6], COL[:, 6:7], Y[:, yblk, cols])
                        deferred.append([2, f5])
                deferred.append([2, f2])

        def run_deferred(flush=False):
            i = 0
            while i < len(deferred):
                deferred[i][0] -= 1
                if flush or deferred[i][0] <= 0:
                    fn = deferred.pop(i)[1]
                    fn()
                else:
                    i += 1

        LOOK = 3
        for i in range(min(LOOK, len(jobs))):
            emitA(jobs[i], i)
        for i in range(len(jobs)):
            emitB(jobs[i], i)
            if i + LOOK < len(jobs):
                emitA(jobs[i + LOOK], i + LOOK)
            run_deferred()
        while deferred:
            run_deferred(flush=True)

    def hgrn(l, p):
        yblk = 2 + p
        wq = load_w(l, ("hgrn", "q", p))
        wf = load_w(l, ("hgrn", "f", p))
        wz = load_w(l, ("hgrn", "z", p))
        wv = load_w(l, ("hgrn", "tm", p))
        z_gate(l, wz, yblk)
        VH = VB[:, 0:NT * 128].rearrange("p (t c) -> p t c", t=NT)
        for t in range(NT):
            ps = PS[5 + (t % 2)]
            for k in range(8):
                P.mm(ps[:, 0:128], HT[:, k, t * 128:(t + 1) * 128], wv[:, k, :], start=(k == 0), stop=(k == 7))
            P.cp(VH[:, t, :], ps[:, 0:128], eng="act")
        if l == 0:
            P.memset(COL[:, 8:9], 0.0)
        else:
            P.tt(COL[:, 8:9], par(l, "lbl", p), par(l, "lb0", p), ALU.subtract)
            P.act(COL[:, 8:9], COL[:, 8:9], AF.Sigmoid)
        P.ts(COL[:, 9:10], COL[:, 8:9], -1.0, 1.0, ALU.mult, ALU.add)
        P.memset(SS[:, :], 0.0)
        P.memset(SSB[:, :], 0.0)
        P.memset(SSB2[:, :], 0.0)
        SSBs = [SSB, SSB2]
        KTb = KT if KTs is None else KTs
        E = KTb[:, 0:1024].bitcast(F32)
        QS, FG, G, DG = F[0], F[1], F[2], F[3]
        HSET = [(H[0], H[1], H[2], H[3]), (H[4], H[5], H[6], H[7])]
        DLHs = [SMF[0], SMF[1]]
        TSETS = [SMB[5 * i:5 * i + 5] for i in range(2)]
        for st_ in TSETS:
            P.memset(st_[0][:, :], 0.0)
            P.memset(st_[1][:, :], 0.0)

        def block_prep(n):
            cols = slice(n * 512, (n + 1) * 512)
            QG, KG, QD, KDT = HSET[n % 2]
            DLH = DLHs[n % 2]
            proj_cm(PS[5], wq, HT, cols)
            P.act(QS[:, :], PS[5][:, :], AF.Silu)
            yield
            proj_cm(PS[6], wf, HT, cols)
            P.act(FG[:, :], PS[6][:, :], AF.Sigmoid)
            yield
            P.ts(FG[:, :], FG[:, :], COL[:, 9:10], COL[:, 8:9], ALU.mult, ALU.add)
            yield
            P.act(E, FG[:, :], AF.Ln)
            yield
            P.scan(G[:, :], k32("scanm"), E)
            P.ts(FG[:, :], FG[:, :], -1.0, 1.0, ALU.mult, ALU.add)
            yield
            G3 = G[:, :].rearrange("p (n c) -> p n c", c=64)
            DG3 = DG[:, :].rearrange("p (n c) -> p n c", c=64)
            P.tt(DG3, G3, G3[:, :, 32:33].to_broadcast([128, 8, 64]), ALU.subtract)
            yield
            P.act(E, DG[:, :], AF.Exp)
            yield
            P.tt(QG[:, :], QS[:, :], E, ALU.mult)
            yield
            P.act(E, DG[:, :], AF.Exp, scale=-1.0)
            yield
            P.tt(KG[:, :], FG[:, :], E, ALU.mult)
            yield
            P.act(E, G[:, :], AF.Exp)
            yield
            P.tt(QD[:, :], QS[:, :], E, ALU.mult)
            P.tt(DG3, G3[:, :, 63:64].to_broadcast([128, 8, 64]), G3, ALU.subtract)
            yield
            P.act(E, DG[:, :], AF.Exp)
            yield
            P.tt(KDT[:, :], FG[:, :], E, ALU.mult)
            P.act(DLH[:, 0:8], G3[:, :, 63], AF.Exp)
            yield

        def tile_pre(n, t):
            gt = 4 * n + t
            tc = slice(t * 128, (t + 1) * 128)
            ATM, VZ, KD0_, KD1_, QGZ = TSETS[gt % 2]
            KD = [KD0_, KD1_]
            QG, KG, QD, KDT = HSET[n % 2]
            pst = PS[7]
            psa = PS[gt % 2]
            pss = PS[2 + gt % 2]
            P.tr(pst[:, 0:64].bitcast(BF16), KDT[:, tc], k16("ident"))
            P.tt(QGZ[:, :].rearrange("p (h c) -> p h c", h=2),
                 QG[:, tc].unsqueeze(1).to_broadcast([128, 2, 128]),
                 k16("hm2").unsqueeze(2).to_broadcast([128, 2, 128]), ALU.mult)
            for hp in range(2):
                P.cp(VZ[:, hp * 128 + hp * 64: hp * 128 + hp * 64 + 64], VH[:, gt, hp * 64:(hp + 1) * 64], eng="act")
            for hf in range(2):
                P.ts(KD[hf][:, 0:128], pst[:, 0:64].bitcast(BF16), k32("rm")[:, hf:hf + 1], None, ALU.mult)
            yield
            for hf in range(2):
                M = 64 if hf == 0 else 128
                for hp in range(2):
                    P.mm(psa[0:M, hp * 128 + hf * 64: hp * 128 + hf * 64 + 64],
                         KG[:, t * 128:t * 128 + M], QGZ[:, hp * 128 + hf * 64: hp * 128 + hf * 64 + 64])
            for hf in range(2):
                for hp in range(2):
                    P.mm(pss[:, hf * 128:(hf + 1) * 128], KD[hf][:, 0:128], VZ[:, hp * 128:(hp + 1) * 128],
                         start=(hp == 0), stop=(hp == 1))
            yield
            A3 = ATM[:, :].rearrange("p (h c) -> p h c", h=2)
            for hf in range(2):
                ic = slice(hf * 64, hf * 64 + 64)
                rows = slice(hf * 64, hf * 64 + 64)
                P.tt(A3[rows, :, ic], psa[:, 0:256].rearrange("p (h c) -> p h c", h=2)[rows, :, ic],
                     k16("caus01")[rows, ic].unsqueeze(1).to_broadcast([64, 2, 64]), ALU.mult)
            yield

        def tile_chain(n, t, pso):
            gt = 4 * n + t
            ATM, VZ, KD0_, KD1_, QGZ = TSETS[gt % 2]
            QG, KG, QD, KDT = HSET[n % 2]
            DLH = DLHs[n % 2]
            pss = PS[2 + gt % 2]
            A3 = ATM[:, :].rearrange("p (h c) -> p h c", h=2)
            for hf in range(2):
                ic = slice(hf * 64, hf * 64 + 64)
                oc = slice(t * 128 + hf * 64, t * 128 + hf * 64 + 64)
                cg = gt * 2 + hf
                P.mm(pso[:, oc], SSBs[cg % 2][:, :], QD[:, oc], start=True, stop=False)
                for hp in range(2):
                    P.mm(pso[:, oc], VZ[:, hp * 128:(hp + 1) * 128], A3[:, hp, ic], start=False, stop=(hp == 1))
                yield
                ch = t * 2 + hf
                for hp in range(2):
                    r = slice(hp * 64, hp * 64 + 64)
                    P.stt(SS[r, r], SS[r, r], DLH[r, ch:ch + 1], pss[r, hf * 128 + hp * 64:hf * 128 + hp * 64 + 64], ALU.mult, ALU.add)
                yield
                P.cp(SSBs[(cg + 1) % 2][:, :], SS[:, :], eng="act")
                yield
            if t == 3:
                cols = slice(n * 512, (n + 1) * 512)
                head_norm_gate(l, pso, par(l, "hgrng"), Y[:, yblk, cols])

        def step(g_):
            try:
                next(g_)
                return True
            except StopIteration:
                return False

        pso = PS[4]
        for _ in block_prep(0):
            pass
        prev = None
        bg = None
        for n in range(NB):
            for t in range(4):
                if t == 0:
                    if bg is not None:
                        for _ in bg:
                            pass
                    bg = block_prep(n + 1) if n + 1 < NB else None
                cur = tile_pre(n, t)
                alive = True
                while alive:
                    alive = step(cur)
                    if prev is not None and not step(prev):
                        prev = None
                    if t >= 1 and bg is not None and not step(bg):
                        bg = None
                if prev is not None:
                    for _ in prev:
                        pass
                prev = tile_chain(n, t, pso)
        for _ in prev:
            pass

    def gdn(l, p):
        yblk = p
        wz = load_w(l, ("gdn", "z", p))
        wab = load_w(l, ("gdn", "tm", p))
        wq = load_w(l, ("gdn", "q", p))
        wk = load_w(l, ("gdn", "k", p))
        z_gate(l, wz, yblk)
        AB, GR, BETA, G, GL, EG, BEG, EGLG, NEGG, TMP = TOK
        psab = PS[7]
        for t in range(NT):
            for k in range(8):
                P.mm(psab[:, t * 4:t * 4 + 4], HT[:, k, t * 128:(t + 1) * 128], wab[:, k, 0:4], start=(k == 0), stop=(k == 7))
        P.cp(AB[:, :], psab[:, 0:NT * 4])
        wv = load_w(l, ("gdn", "v", p))
        AB3 = AB[:, :].rearrange("p (t c) -> p t c", c=4)
        GR3 = GR[:, 0:NT * 2].rearrange("p (t c) -> p t c", c=2)
        dtb = par(l, "dtb", 2 * p, 2).unsqueeze(1).to_broadcast([128, NT, 2])
        P.tt(GR3, AB3[:, :, 0:2], dtb, ALU.add)
        T3 = TMP[:, 0:NT * 2].rearrange("p (t c) -> p t c", c=2)
        P.ts(T3, GR3, -30.0, 0.0, ALU.add, ALU.max)
        P.ts(GR3, GR3, 30.0, None, ALU.min)
        P.act(GR[:, 0:NT * 2], GR[:, 0:NT * 2], AF.Exp)
        P.act(GR[:, 0:NT * 2], GR[:, 0:NT * 2], AF.Ln, bias=1.0)
        P.tt(GR3, GR3, T3, ALU.add)
        P.act(COL[:, 10:12], par(l, "alog", 2 * p, 2), AF.Exp)
        P.ts(COL[:, 10:12], COL[:, 10:12], -1.0, None, ALU.mult)
        P.tt(GR3, GR3, COL[:, 10:12].unsqueeze(1).to_broadcast([128, NT, 2]), ALU.mult)
        B3 = BETA[:, 0:NT * 2].rearrange("p (t c) -> p t c", c=2)
        P.act(B3, AB3[:, :, 2:4], AF.Sigmoid)
        n2 = NT * 2
        P.mm(PS[7][:, 0:n2], k32("blktri"), GR[:, 0:n2])
        P.cp(G[:, 0:n2], PS[7][:, 0:n2])
        P.mm(PS[6][:, 0:n2], k32("blk64"), GR[:, 0:n2])
        P.cp(GL[:, 0:n2], PS[6][:, 0:n2])
        P.act(EG[:, 0:n2], G[:, 0:n2], AF.Exp)
        P.tt(BEG[:, 0:n2], EG[:, 0:n2], BETA[:, 0:n2], ALU.mult)
        P.tt(EGLG[:, 0:n2], GL[:, 0:n2], G[:, 0:n2], ALU.subtract)
        P.act(EGLG[:, 0:n2], EGLG[:, 0:n2], AF.Exp)
        P.ts(NEGG[:, 0:n2], G[:, 0:n2], -1.0, None, ALU.mult)
        REP = SMF[0]
        psd = PS[6]
        for t in range(NT):
            P.cp(REP[:, 0:128].rearrange("p (h c) -> p h c", h=2),
                 GL[:, t * 2:t * 2 + 2].unsqueeze(2).to_broadcast([128, 2, 64]))
            P.mm(psd[:, 256 + t * 2:256 + t * 2 + 2], REP[:, 0:128], k32("e2"))
        P.act(DL[:, 0:n2], psd[:, 256:256 + n2], AF.Exp)
        P.memset(SS[:, :], 0.0)
        P.memset(SSB[:, :], 0.0)
        VNZ = SMB[0]
        P.memset(VNZ[:, :], 0.0)
        KBGZ = SMB[1]
        P.memset(KBGZ[:, :], 0.0)
        KD0 = SMB[2]
        KTb = KT if KTs is None else KTs
        PC = [VB[:, 0:515], VB[:, 520:1035], KTb[:, 0:1030].bitcast(F32)]
        for b_ in PC:
            P.memset(b_[:, 0:3], 0.0)
        DIAG = []
        for bi_ in range(2):
            for j_ in range(4):
                dst_ = wab[:, bi_ * 4 + j_, :]
                P.ts(dst_, k16("ident"), par(l, "conv", (bi_ * 2 + p) * 4 + j_), None, ALU.mult, eng="pool")
                DIAG.append(dst_)
        ACC, CQ, SQ, RS = F[0], F[1], F[2], F[3]
        QN, KN, VT = H[0], H[1], H[2]
        convw = lambda blk, j: par(l, "conv", (blk * 2 + p) * 4 + j)
        hmb = k16("hm2").unsqueeze(2).to_broadcast([128, 2, 128])
        idb = k16("ident").unsqueeze(1).to_broadcast([128, 2, 128])

        def v3(x):
            return x[:, :].rearrange("p (h c) -> p h c", h=2)

        def prepinv(n, t, si):
            gt = 4 * n + t
            tc = slice(t * 128, (t + 1) * 128)
            KNZ, QNZ, D, DT, A, AQT, QD, AL, IZ, TQ, EGB = SMB[3 + 11 * si: 3 + 11 * si + 11]
            Tm, Tt = TTS[si][:, 0:256], TTS[si][:, 256:512]
            REPg = SMF[si]
            psGZ, psK, psT = PS[0 + si], PS[2 + si], PS[5 + si]
            P.tt(v3(KNZ), KN[:, tc].unsqueeze(1).to_broadcast([128, 2, 128]), hmb, ALU.mult)
            P.tt(v3(QNZ), QN[:, tc].unsqueeze(1).to_broadcast([128, 2, 128]), hmb, ALU.mult)
            P.cp(v3(REPg), GR[:, gt * 2:gt * 2 + 2].unsqueeze(2).to_broadcast([128, 2, 128]))
            yield
            for hp in range(2):
                hs = slice(hp * 128, (hp + 1) * 128)
                P.mm(psGZ[:, hs], REPg[:, hs], k32("blktri"))
                P.mm(psK[:, hs], KN[:, tc], KNZ[:, hs])
                P.mm(psK[:, 256 + hp * 128:256 + (hp + 1) * 128], KN[:, tc], QNZ[:, hs])
            yield
            for hp in range(2):
                hs = slice(hp * 128, (hp + 1) * 128)
                P.act(D[:, hs], psGZ[:, hs], AF.Exp, bias=G[:, gt * 2 + hp:gt * 2 + hp + 1], scale=-1.0)
                P.act(DT[:, hs], psGZ[:, hs], AF.Exp, bias=NEGG[:, gt * 2 + hp:gt * 2 + hp + 1], scale=1.0)
            P.act(EGB[:, :], psGZ[:, 0:256], AF.Exp)
            yield
            P.stt(v3(D), v3(D), 1e30, k16("m01").unsqueeze(1).to_broadcast([128, 2, 128]), ALU.min, ALU.mult)
            P.stt(v3(DT), v3(DT), 1e30, k16("m01T").unsqueeze(1).to_broadcast([128, 2, 128]), ALU.min, ALU.mult)
            yield
            for hp in range(2):
                hs = slice(hp * 128, (hp + 1) * 128)
                P.stt(A[:, hs], psK[:, hs], BETA[:, gt * 2 + hp:gt * 2 + hp + 1], D[:, hs], ALU.mult, ALU.mult)
            P.tt(AQT[:, :], psK[:, 256:512], DT[:, :], ALU.mult)
            P.tt(TQ[:, :], QNZ[:, :], EGB[:, :], ALU.mult)
            P.tt(QD[:, 0:128], TQ[:, 0:128], TQ[:, 128:256], ALU.add)
            yield
            for lv in range(6):
                lvm = k16("lvl")[:, lv * 128:(lv + 1) * 128].unsqueeze(1).to_broadcast([128, 2, 128])
                P.tt(v3(AL), v3(A), lvm, ALU.mult)
                yield
                if lv == 0:
                    P.stt(v3(Tm), v3(AL), -1.0, idb, ALU.mult, ALU.add)
                    for hp in range(2):
                        hs = slice(hp * 128, (hp + 1) * 128)
                        P.mm(psGZ[:, 256 + hp * 128:256 + (hp + 1) * 128], AL[:, hs], k16("ident"))
                    yield
                    P.stt(v3(Tt), psGZ[:, 256:512].rearrange("p (h c) -> p h c", h=2), -1.0, idb, ALU.mult, ALU.add)
                    yield
                    continue
                for hp in range(2):
                    hs = slice(hp * 128, (hp + 1) * 128)
                    P.mm(psGZ[:, 256 + hp * 128:256 + (hp + 1) * 128], AL[:, hs], Tt[:, hs])
                yield
                P.stt(v3(IZ), psGZ[:, 256:512].rearrange("p (h c) -> p h c", h=2), -1.0, idb, ALU.mult, ALU.add)
                yield
                for hp in range(2):
                    hs = slice(hp * 128, (hp + 1) * 128)
                    if lv < 5:
                        P.mm(psT[:, hs], IZ[:, hs], Tm[:, hs])
                    P.mm(psT[:, 256 + hp * 128:256 + (hp + 1) * 128], Tm[:, hs], IZ[:, hs])
                yield
                if lv < 5:
                    P.cp(TTS[si][:, :], psT[:, :], eng="act")
                else:
                    P.cp(Tt[:, :], psT[:, 256:512], eng="act")
                yield

        WT2 = [H[6], H[7]]
        U2 = [F[4], F[5]]
        KDs = [[KD0, H[3]], [H[8], H[9]]]
        AQT2 = [SMB[25], SMB[26]]
        QD2 = [SMB[27], SMB[28]]

        def post(n, t, si):
            gt = 4 * n + t
            tc = slice(t * 128, (t + 1) * 128)
            KNZ, QNZ, D, DT, A, AQT, QD, AL, IZ, TQ, EGB = SMB[3 + 11 * si: 3 + 11 * si + 11]
            Tm, Tt = TTS[si][:, 0:256], TTS[si][:, 256:512]
            pst = PS[2 + si]
            P.tr(pst[:, 0:64].bitcast(BF16), KN[:, tc], k16("ident"))
            P.tr(pst[:, 64:128].bitcast(BF16), VT[:, tc], k16("ident"))
            kt_ = pst[:, 0:64].bitcast(BF16)
            vt_ = pst[:, 64:128].bitcast(BF16)
            KD = KDs[si]
            VBt = H[4]
            KDall = H[5]
            P.cp(AQT2[si][:, :], AQT[:, :], eng="pool")
            P.cp(QD2[si][:, 0:128], QD[:, 0:128], eng="pool")
            yield
            for hp in range(2):
                cs = slice(hp * 64, (hp + 1) * 64)
                P.ts(KBGZ[:, hp * 128 + hp * 64: hp * 128 + hp * 64 + 64], kt_[:, cs], BEG[:, gt * 2 + hp:gt * 2 + hp + 1], None, ALU.mult)
                P.ts(KDall[:, cs], kt_[:, cs], EGLG[:, gt * 2 + hp:gt * 2 + hp + 1], None, ALU.mult)
                P.ts(VBt[:, cs], vt_[:, cs], BETA[:, gt * 2 + hp:gt * 2 + hp + 1], None, ALU.mult)
            for hf in range(2):
                P.ts(KD[hf][:, 0:128], KDall[:, 0:128], k32("rm")[:, hf:hf + 1], None, ALU.mult, eng="pool")
            for hp in range(2):
                P.mm(pst[:, 128:256], KBGZ[:, hp * 128:(hp + 1) * 128], Tt[:, hp * 128:(hp + 1) * 128], start=(hp == 0), stop=(hp == 1))
            for hp in range(2):
                P.mm(pst[:, 256 + hp * 64:256 + (hp + 1) * 64], Tt[:, hp * 128:(hp + 1) * 128], VBt[:, hp * 64:(hp + 1) * 64])
            yield
            P.cp(WT2[si][:, 0:128], pst[:, 128:256], eng="act")
            P.cp(U2[si][:, 0:128], pst[:, 256:384], eng="act")
            yield

        def scan_pair(n, tp, pso):
            for si in range(2):
                t = 2 * tp + si
                gt = 4 * n + t
                WT, U, KD, AQT, QD = WT2[si], U2[si], KDs[si], AQT2[si], QD2[si]
                for hf in range(2):
                    rows = slice(hf * 64, hf * 64 + 64)
                    ic = slice(hf * 64, hf * 64 + 64)
                    M = 64 if hf == 0 else 128
                    psws = PS[7]
                    P.mm(psws[0:M, 0:128], WT[:, 0:M], SSB[:, :])
                    yield
                    for hp in range(2):
                        cs = slice(hp * 64, (hp + 1) * 64)
                        P.tt(VNZ[rows, hp * 128 + hp * 64: hp * 128 + hp * 64 + 64], U[rows, cs], psws[rows, cs], ALU.subtract)
                    yield
                    oc = slice(t * 128 + hf * 64, t * 128 + hf * 64 + 64)
                    P.mm(pso[:, oc], SSB[:, :], QD[:, ic], start=True, stop=False)
                    for hp in range(2):
                        P.mm(pso[:, oc], VNZ[:, hp * 128:(hp + 1) * 128], AQT[:, hp * 128 + hf * 64: hp * 128 + hf * 64 + 64],
                             start=False, stop=(hp == 1))
                    pss = PS[7]
                    for hp in range(2):
                        P.mm(pss[:, 128:256], KD[hf][:, 0:128], VNZ[:, hp * 128:(hp + 1) * 128], start=(hp == 0), stop=(hp == 1))
                    yield
                    ch = gt * 2 + hf
                    for hp in range(2):
                        r = slice(hp * 64, hp * 64 + 64)
                        P.stt(SS[r, r], SS[r, r], DL[r, ch:ch + 1], pss[r, 128 + hp * 64:128 + hp * 64 + 64], ALU.mult, ALU.add)
                    yield
                    P.cp(SSB[:, :], SS[:, :], eng="act")
                    yield
            if tp == 1:
                cols = slice(n * 512, (n + 1) * 512)
                head_norm_gate(l, pso, par(l, "gdng"), Y[:, yblk, cols])

        def chain(*gs):
            for g_ in gs:
                yield from g_

        pso = PS[4]
        prev = None
        for n in range(NB):
            cols = slice(n * 512, (n + 1) * 512)
            for bi, (w_, dst) in enumerate(((wq, QN), (wk, KN), (wv, VT))):
                ps = PS[5 + (bi % 2)]
                proj_cm(ps, w_, HT, cols)
                pc = PC[bi]
                P.cp(pc[:, 3:515], ps[:, :], eng="act")
                if bi == 2:
                    P.ts(ACC[:, :], pc[:, 0:512], convw(bi, 0), None, ALU.mult)
                    for j in range(1, 4):
                        P.stt(ACC[:, :], pc[:, j:j + 512], convw(bi, j), ACC[:, :], ALU.mult, ALU.add)
                    P.cp(pc[:, 0:3], pc[:, 512:515])
                    P.act(VT[:, :], ACC[:, :], AF.Silu)
                else:
                    psc = PS[bi % 2]
                    for j in range(4):
                        P.mm(psc[:, :], DIAG[bi * 4 + j], pc[:, j:j + 512], start=(j == 0), stop=(j == 3))
                    P.cp(pc[:, 0:3], pc[:, 512:515], eng="pool")
                    P.act(CQ[:, :], psc[:, :], AF.Silu)
                    P.act(SQ[:, :], CQ[:, :], AF.Square)
                    P.mm(PS[7][:, :], k32("blk64"), SQ[:, :])
                    rsqrt_from(RS[:, :], PS[7][:, :], 1.0, EPS)
                    P.stt(dst[:, :], CQ[:, :], float(HD ** -0.5) if bi == 0 else 1.0, RS[:, :], ALU.mult, ALU.mult)
            for tp in range(2):
                gens = [chain(prepinv(n, 2 * tp + si, si), post(n, 2 * tp + si, si)) for si in range(2)]
                alive = list(gens)
                while alive:
                    for g_ in list(alive):
                        try:
                            next(g_)
                        except StopIteration:
                            alive.remove(g_)
                    if prev is not None:
                        try:
                            next(prev)
                        except StopIteration:
                            prev = None
                if prev is not None:
                    for _ in prev:
                        pass
                prev = scan_pair(n, tp, pso)
        for _ in prev:
            pass

    ONESB = P.sb("ONESB", [128, 128], BF16)
    P.memset(ONESB[:, :], 1.0)

    def outproj(l):
        KTb = KT if KTs is None else KTs
        OB = [F[i][:, :] for i in range(7)] + [KTb[:, 0:1024].bitcast(F32)]
        wviews = []
        for d in range(8):
            if d < 4:
                w_ = load_w(l, ("out", "w", d))
                wviews.append([w_[:, k, :] for k in range(8)])
            else:
                ha, hb = H[2 + 2 * (d - 4)], H[3 + 2 * (d - 4)]
                src = wD[l, CMB[("out", "w", d)]]
                P.dma(ha[:, :], src[:, 0:512], q="pool")
                P.dma(hb[:, :], src[:, 512:1024], q="pool")
                wviews.append([(ha if k < 4 else hb)[:, (k % 4) * 128:(k % 4 + 1) * 128] for k in range(8)])
        RSB = [VB[:, 0:1024].bitcast(F32), VB[:, 1024:2048].bitcast(F32)]

        def proj_d(n, d):
            cols = slice(n * 512, (n + 1) * 512)
            ps = PS[5 + (d % 2)]
            for k in range(8):
                P.mm(ps[:, :], wviews[d][k], Y[:, k, cols], start=(k == 0), stop=(k == 7))
            P.cp(OB[d], ps[:, :], eng="act")
            sq = H[d % 2]
            P.act(sq[:, :], ps[:, :], AF.Square)
            while pending:
                pending.pop(0)()
            pending.append(lambda n=n, d=d, sq=sq: P.mm(PS[n % 2][:, :], ONESB[:, :], sq[:, :],
                                                       start=(d == 0), stop=(d == 7)))

        pending = []

        def post_d(n, d):
            cols = slice(n * 512, (n + 1) * 512)
            rs = RSB[n % 2]
            P.stt(OB[d], OB[d], par(l, "postg", d), rs, ALU.mult, ALU.mult)
            P.tt(XT[:, d, cols], XT[:, d, cols], OB[d], ALU.add)

        for d in range(8):
            proj_d(0, d)
        for n in range(NB):
            while pending:
                pending.pop(0)()
            rsqrt_from(RSB[n % 2], PS[n % 2][:, :], 1.0 / D_MODEL, EPS)
            for d in range(8):
                post_d(n, d)
                if n + 1 < NB:
                    proj_d(n + 1, d)

    for s in range(NSEQ):
        for c in range(8):
            P.dma(XT[:, c, :], xD[s, :, c * T:(c + 1) * T])
        for l in range(DEPTH):
            P.mark("prenorm s%d l%d" % (s, l))
            prenorm(l)
            P.memset(Y[:, :, :], 0.0) if (len(branches) < 4) else None
            for p in range(2):
                if "gdn" in branches:
                    P.mark("gdn s%d l%d p%d" % (s, l, p))
                    gdn(l, p)
                if "hgrn" in branches:
                    P.mark("hgrn s%d l%d p%d" % (s, l, p))
                    hgrn(l, p)
                if "moba" in branches:
                    P.mark("moba s%d l%d p%d" % (s, l, p))
                    attention(l, p, "moba")
                if "diff" in branches:
                    P.mark("diff s%d l%d p%d" % (s, l, p))
                    attention(l, p, "diff")
            if tap:
                TP = F
                for c in range(8):
                    for n in range(NB):
                        cols = slice(n * 512, (n + 1) * 512)
                        P.cp(TP[(c * NB + n) % 4][:, :], Y[:, c, cols])
                        P.dma(tapD[s, l, :, c * T + n * 512: c * T + (n + 1) * 512], TP[(c * NB + n) % 4][:, :])
            P.mark("outproj s%d l%d" % (s, l))
            outproj(l)
        for c in range(8):
            P.dma(yD[s, :, c * T:(c + 1) * T], XT[:, c, :])
    P.mark("end")
    P.final_wait("sp", [XT] + ([F[0], F[1], F[2], F[3]] if tap else []))
    P.emit()
    return nc, P


_CACHE = {}


def _prep_inputs(inp, T, nseq_total):
    x = np.asarray(inp["x"], np.float32)
    B = x.shape[0]
    xT = np.ascontiguousarray(x.reshape(B, T, 8, 128).transpose(0, 3, 2, 1)).reshape(B, 128, 8 * T)
    w = pack_weights(np.asarray(inp["w_in"], np.float32), np.asarray(inp["w_out"], np.float32))
    par = pack_params({k: np.asarray(v, np.float32) for k, v in inp.items()})
    c32, c16 = make_consts()
    return xT, w, par, c32, c16


def kernel(**inputs):
    x = np.asarray(inputs["x"])
    B, T, D = x.shape
    depth = inputs["w_in"].shape[0]
    nseq = B // N_CORES
    key = (T, nseq, depth)
    if key not in _CACHE:
        _CACHE[key] = build(T=T, NSEQ=nseq, DEPTH=depth)[0]
    nc = _CACHE[key]
    xT, w, par, c32, c16 = _prep_inputs(inputs, T, B)
    in_maps = []
    for c in range(N_CORES):
        in_maps.append({"x": xT[c * nseq:(c + 1) * nseq], "w": w, "c32": c32, "c16": c16, "par": par})
    res = run_bass_kernel_spmd(nc, in_maps, core_ids=list(range(N_CORES)))
    outs = []
    for c in range(N_CORES):
        yT = np.asarray(res.results[c]["y"]).reshape(nseq, 128, 8, T)
        outs.append(yT.transpose(0, 3, 2, 1).reshape(nseq, T, D))
    return np.concatenate(outs, axis=0).astype(np.float32)
```

```python
import math
import numpy as np
import concourse.bass as bass
import concourse.mybir as mybir
from concourse.bass_utils import run_bass_kernel_spmd

F32 = mybir.dt.float32
BF16 = mybir.dt.bfloat16
AF = mybir.ActivationFunctionType
ALU = mybir.AluOpType
AX = mybir.AxisListType

D_MODEL = 1024
HD = 64
EPS = 1e-6
BIG = 30000.0
N_CORES = 8


class View:
    def __init__(self, buf, ap):
        self.buf = buf
        self.ap = ap

    def __getitem__(self, k):
        return View(self.buf, self.ap[k])

    def rearrange(self, *a, **k):
        return View(self.buf, self.ap.rearrange(*a, **k))

    def to_broadcast(self, *a, **k):
        return View(self.buf, self.ap.to_broadcast(*a, **k))

    def unsqueeze(self, *a, **k):
        return View(self.buf, self.ap.unsqueeze(*a, **k))

    def bitcast(self, *a, **k):
        return View(self.buf, self.ap.bitcast(*a, **k))


class Buf:
    def __init__(self, t, name):
        self.t = t
        self.name = name
        self.last_w = None
        self.reads = {}
        self.dma_sem = None
        self.is_psum = False

    def __getitem__(self, k):
        return View(self, self.t[k])


class Prog:
    ENG = ("pe", "act", "dve", "pool", "sp")

    def __init__(self, nc):
        self.nc = nc
        self.streams = {e: [] for e in self.ENG}
        self.sems = {}
        self.cnt = {}
        self.waited = {e: {} for e in self.ENG}
        self.marks = []
        for e in ("pe", "act", "dve", "pool"):
            self.sems[e] = nc.alloc_semaphore("s_" + e)
            self.cnt[e] = 0

    def mark(self, label):
        self.marks.append((label, dict(self.cnt)))

    def sb(self, name, shape, dtype=F32):
        return Buf(self.nc.alloc_sbuf_tensor(name, list(shape), dtype), name)

    def ps(self, name, shape, dtype=F32):
        b = Buf(self.nc.alloc_psum_tensor(name, list(shape), dtype), name)
        b.is_psum = True
        return b

    def dram(self, ap, name):
        return Buf(ap, name)

    def _deps(self, eng, reads, writes):
        need = {}

        def add(k, c):
            if k == "pe" and eng == "pe":
                return
            if need.get(k, 0) < c:
                need[k] = c
        for b in reads:
            if b.last_w is not None:
                add(*b.last_w)
            if b.is_psum:
                for k, c in b.reads.items():
                    if k != eng:
                        add(k, c)
        for b in writes:
            if b.last_w is not None:
                add(*b.last_w)
            for k, c in b.reads.items():
                add(k, c)
        out = []
        w = self.waited[eng]
        for k, c in need.items():
            if w.get(k, 0) < c:
                w[k] = c
                out.append((k, c))
        return out

    def op(self, eng, fn, reads=(), writes=()):
        reads = list(dict.fromkeys(reads))
        writes = list(dict.fromkeys(writes))
        waits = self._deps(eng, reads, writes)
        self.cnt[eng] += 1
        c = self.cnt[eng]
        self.streams[eng].append((waits, fn, (eng, 1)))
        for b in reads:
            b.reads[eng] = c
        for b in writes:
            b.last_w = (eng, c)
            b.reads = {}

    def dma(self, out, in_, q="sp"):
        ob, ib = out.buf, in_.buf
        owner = ob if ob.dma_sem is not None else (ib if ib.dma_sem is not None else ob)
        if owner.dma_sem is None:
            key = "dma%d" % len(self.sems)
            self.sems[key] = self.nc.alloc_semaphore(key)
            self.cnt[key] = 0
            owner.dma_sem = key
        key = owner.dma_sem
        waits = self._deps(q, [ib], [ob])
        self.cnt[key] += 16
        c = self.cnt[key]
        oa, ia = out.ap, in_.ap
        self.streams[q].append((waits, lambda e: e.dma_start(out=oa, in_=ia), (key, 16)))
        ib.reads[key] = c
        ob.last_w = (key, c)
        ob.reads = {}

    def final_wait(self, eng, bufs):
        waits = self._deps(eng, bufs, bufs)
        self.streams[eng].append((waits, None, None))

    def emit(self):
        nc = self.nc
        with nc.Block() as block:
            def mk(ename):
                def body(e):
                    for waits, fn, inc in self.streams[ename]:
                        for k, c in waits:
                            e.wait_ge(self.sems[k], c)
                        if fn is None:
                            continue
                        ins = fn(e)
                        ins.then_inc(self.sems[inc[0]], inc[1])
                return body
            if self.streams["pe"]:
                block.tensor(mk("pe"))
            if self.streams["act"]:
                block.scalar(mk("act"))
            if self.streams["dve"]:
                block.vector(mk("dve"))
            if self.streams["pool"]:
                block.gpsimd(mk("pool"))
            if self.streams["sp"]:
                block.sync(mk("sp"))

    def mm(self, out, lhsT, rhs, start=True, stop=True):
        o, l, r = out.ap, lhsT.ap, rhs.ap
        self.op("pe", lambda e: e.matmul(o, l, r, start=start, stop=stop),
                reads=[lhsT.buf, rhs.buf], writes=[out.buf])

    def tr(self, out, in_, ident):
        o, i, d = out.ap, in_.ap, ident.ap
        self.op("pe", lambda e: e.transpose(o, i, d), reads=[in_.buf, ident.buf], writes=[out.buf])

    def act(self, out, in_, func, bias=None, scale=1.0, eng="act"):
        o, i = out.ap, in_.ap
        rd = [in_.buf]
        kw = {}
        if isinstance(bias, View):
            rd.append(bias.buf)
            kw["bias"] = bias.ap
        elif bias is not None:
            kw["bias"] = float(bias)
        if isinstance(scale, View):
            rd.append(scale.buf)
            kw["scale"] = scale.ap
        else:
            kw["scale"] = float(scale)
        self.op("act", lambda e: e.activation(o, i, func, **kw), reads=rd, writes=[out.buf])

    def tt(self, out, in0, in1, op, eng="dve"):
        o, a, b = out.ap, in0.ap, in1.ap
        self.op(eng, lambda e: e.tensor_tensor(o, a, b, op), reads=[in0.buf, in1.buf], writes=[out.buf])

    def ts(self, out, in0, s1, s2, op0, op1=None, eng="dve"):
        o, a = out.ap, in0.ap
        rd = [in0.buf]
        v1 = s1
        if isinstance(s1, View):
            rd.append(s1.buf)
            v1 = s1.ap
        v2 = s2
        if isinstance(s2, View):
            rd.append(s2.buf)
            v2 = s2.ap
        if op1 is None:
            self.op(eng, lambda e: e.tensor_scalar(o, a, v1, None, op0), reads=rd, writes=[out.buf])
        else:
            self.op(eng, lambda e: e.tensor_scalar(o, a, v1, v2, op0, op1), reads=rd, writes=[out.buf])

    def stt(self, out, in0, scalar, in1, op0, op1, eng="dve"):
        o, a, b = out.ap, in0.ap, in1.ap
        rd = [in0.buf, in1.buf]
        sv = scalar
        if isinstance(scalar, View):
            rd.append(scalar.buf)
            sv = scalar.ap
        self.op(eng, lambda e: e.scalar_tensor_tensor(o, a, sv, b, op0, op1), reads=rd, writes=[out.buf])

    def cp(self, out, in_, eng="dve"):
        o, i = out.ap, in_.ap
        if eng == "act":
            self.op("act", lambda e: e.copy(o, i), reads=[in_.buf], writes=[out.buf])
        else:
            self.op(eng, lambda e: e.tensor_copy(o, i), reads=[in_.buf], writes=[out.buf])

    def memset(self, out, val, eng="dve"):
        o = out.ap
        self.op(eng, lambda e: e.memset(o, val), writes=[out.buf])

    def red(self, out, in_, op, eng="dve"):
        o, i = out.ap, in_.ap
        self.op(eng, lambda e: e.tensor_reduce(o, i, AX.X, op), reads=[in_.buf], writes=[out.buf])

    def recip(self, out, in_):
        o, i = out.ap, in_.ap
        self.op("dve", lambda e: e.reciprocal(o, i), reads=[in_.buf], writes=[out.buf])

    def scan(self, out, d0, d1):
        o, a, b = out.ap, d0.ap, d1.ap
        self.op("dve", lambda e: e.tensor_tensor_scan(o, a, b, 0.0, ALU.mult, ALU.add),
                reads=[d0.buf, d1.buf], writes=[out.buf])


C32 = {}
_o = 0
for _n, _w in (("ident", 128), ("blk64", 128), ("blktri", 128), ("scanm", 512), ("selden", 64), ("place", 256), ("e2", 2), ("rm", 2), ("arel", 16)):
    C32[_n] = (_o, _w)
    _o += _w
NC32 = _o
C16 = {}
_o = 0
for _n, _w in (("ident", 128), ("lvl", 6 * 128), ("caus01", 128), ("causneg", 128), ("hm2", 2), ("hm4", 4),
               ("ka", 16 * 128), ("oh", 16 * 128), ("qab", 512), ("m01", 128), ("m01T", 128)):
    C16[_n] = (_o, _w)
    _o += _w
NC16 = _o


def make_consts():
    c32 = np.zeros((128, NC32), np.float32)
    c16 = np.zeros((128, NC16), np.float32)
    p = np.arange(128)[:, None]
    c = np.arange(128)[None, :]
    same = (p // 64) == (c // 64)

    def put(dst, tab, name, arr):
        o, w = tab[name]
        dst[:, o:o + w] = arr
    put(c32, C32, "ident", (p == c).astype(np.float32))
    put(c32, C32, "blk64", same.astype(np.float32))
    put(c32, C32, "blktri", (same & (p <= c)).astype(np.float32))
    sm = np.ones((128, 512), np.float32)
    sm[:, ::64] = 0.0
    put(c32, C32, "scanm", sm)
    sd = np.zeros((128, 64), np.float32)
    sd[64, :] = 1.0
    put(c32, C32, "selden", sd)
    pl = np.zeros((128, 256), np.float32)
    for r in range(64):
        pl[r, r] = 1.0
        pl[r, 128 + 64 + r] = 1.0
    put(c32, C32, "place", pl)
    e2 = np.zeros((128, 2), np.float32)
    e2[0, 0] = 1.0
    e2[64, 1] = 1.0
    put(c32, C32, "e2", e2)
    arel = np.zeros((128, 16), np.float32)
    for r in range(16):
        arel[:, r] = np.arange(128) + 128.0 * (r - 12) - 256.0
    put(c32, C32, "arel", arel)
    rm = np.zeros((128, 2), np.float32)
    rm[:64, 0] = 1.0
    rm[64:, 1] = 1.0
    put(c32, C32, "rm", rm)

    put(c16, C16, "ident", (p == c).astype(np.float32))
    lv = np.zeros((128, 6, 128), np.float32)
    for l in range(1, 7):
        s = 2 ** l
        lv[:, l - 1, :] = ((p // s) == (c // s)) & ((p % s) >= s // 2) & ((c % s) < s // 2)
    put(c16, C16, "lvl", lv.reshape(128, 768))
    put(c16, C16, "m01", (same & (p >= c)).astype(np.float32))
    put(c16, C16, "m01T", (same & (c >= p)).astype(np.float32))
    put(c16, C16, "caus01", (p <= c).astype(np.float32))
    put(c16, C16, "causneg", np.where(p > c, -BIG, 0.0))
    hm2 = np.zeros((128, 2), np.float32)
    hm2[:64, 0] = 1.0
    hm2[64:, 1] = 1.0
    put(c16, C16, "hm2", hm2)
    hm4 = np.zeros((128, 4), np.float32)
    for g in range(4):
        hm4[g * 32:(g + 1) * 32, g] = 1.0
    put(c16, C16, "hm4", hm4)
    ka = np.zeros((128, 16, 128), np.float32)
    for ri in range(16):
        ka[0, ri, :] = np.arange(128)
        ka[1, ri, :] = 1.0
        ka[2, ri, :] = 128.0 * (ri - 12)
        ka[3, ri, :] = 1.0
    put(c16, C16, "ka", ka.reshape(128, 2048))
    oh = np.zeros((128, 16, 128), np.float32)
    for r in range(16):
        oh[r, r, :] = 1.0
    put(c16, C16, "oh", oh.reshape(128, 2048))
    qab = np.zeros((128, 512), np.float32)
    il = np.arange(512)
    qab[0, :] = 1.0
    qab[1, :] = -(il % 128)
    qab[2, :] = 1.0
    qab[3, :] = -128.0 * (il // 128)
    put(c16, C16, "qab", qab)
    return c32, c16


PAR = {}
_o = 0
for _n, _w in (("preg", 8), ("postg", 8), ("conv", 24), ("gdng", 1), ("hgrng", 1), ("diffg", 1),
               ("lbl", 2), ("lb0", 2), ("alog", 4), ("dtb", 4), ("lq1", 32), ("lk1", 32), ("lq2", 32), ("lk2", 32)):
    PAR[_n] = (_o, _w)
    _o += _w
NPAR = _o

CMB = {}
_i = 0
for _br, _names in (("gdn", ("q", "k", "v", "z")), ("hgrn", ("q", "f", "z")), ("moba", ("q", "k", "z")),
                    ("diff", ("q", "k", "z"))):
    for _nm in _names:
        for _p in range(2):
            CMB[(_br, _nm, _p)] = _i
            _i += 1
for _br in ("gdn", "hgrn", "moba", "diff"):
    for _p in range(2):
        CMB[(_br, "tm", _p)] = _i
        _i += 1
for _d in range(8):
    CMB[("out", "w", _d)] = _i
    _i += 1
NWB = _i

GDN_BASE, HGRN_BASE, MOBA_BASE, DIFF_BASE = 0, 1032, 2056, 3080


def pack_weights(w_in, w_out):
    depth = w_in.shape[0]
    out = np.zeros((depth, NWB, 128, 1024), np.float32)

    def blockify(wcols):
        return wcols.reshape(8, 128, 128).transpose(1, 0, 2).reshape(128, 1024)
    for l in range(depth):
        W = w_in[l]
        def cols(base, off, p):
            return W[:, base + off + p * 128: base + off + (p + 1) * 128]
        for p in range(2):
            out[l, CMB[("gdn", "q", p)]] = blockify(cols(GDN_BASE, 0, p))
            out[l, CMB[("gdn", "k", p)]] = blockify(cols(GDN_BASE, 256, p))
            out[l, CMB[("gdn", "v", p)]] = blockify(cols(GDN_BASE, 512, p))
            out[l, CMB[("gdn", "z", p)]] = blockify(cols(GDN_BASE, 776, p))
            ab = np.zeros((1024, 128), np.float32)
            ab[:, 0:2] = W[:, GDN_BASE + 768 + 2 * p: GDN_BASE + 768 + 2 * p + 2]
            ab[:, 2:4] = W[:, GDN_BASE + 772 + 2 * p: GDN_BASE + 772 + 2 * p + 2]
            out[l, CMB[("gdn", "tm", p)]] = blockify(ab)
            out[l, CMB[("hgrn", "q", p)]] = blockify(cols(HGRN_BASE, 0, p))
            out[l, CMB[("hgrn", "f", p)]] = blockify(cols(HGRN_BASE, 256, p))
            out[l, CMB[("hgrn", "tm", p)]] = blockify(cols(HGRN_BASE, 512, p))
            out[l, CMB[("hgrn", "z", p)]] = blockify(cols(HGRN_BASE, 768, p))
            for br, base in (("moba", MOBA_BASE), ("diff", DIFF_BASE)):
                out[l, CMB[(br, "q", p)]] = blockify(cols(base, 0, p))
                out[l, CMB[(br, "k", p)]] = blockify(cols(base, 256, p))
                out[l, CMB[(br, "tm", p)]] = blockify(cols(base, 512, p))
                out[l, CMB[(br, "z", p)]] = blockify(cols(base, 768, p))
        for d in range(8):
            out[l, CMB[("out", "w", d)]] = blockify(w_out[l][:, d * 128:(d + 1) * 128])
    return out


def pack_params(inp):
    depth = inp["pre_norm_g"].shape[0]
    par = np.zeros((depth, 128, NPAR), np.float32)

    def put(l, name, arr):
        o, w = PAR[name]
        par[l, :, o:o + w] = arr
    for l in range(depth):
        put(l, "preg", inp["pre_norm_g"][l].reshape(8, 128).T)
        put(l, "postg", inp["post_norm_g"][l].reshape(8, 128).T)
        cw = inp["conv_w"][l]
        put(l, "conv", cw.reshape(4, 6, 128).transpose(2, 1, 0).reshape(128, 24))
        put(l, "gdng", np.tile(inp["gdn_norm_g"][l], 2)[:, None])
        put(l, "hgrng", np.tile(inp["hgrn_norm_g"][l], 2)[:, None])
        put(l, "diffg", np.tile(inp["diff_norm_g"][l], 2)[:, None])
        put(l, "lbl", inp["hgrn_lb"][l].reshape(2, 128).T)
        put(l, "lb0", inp["hgrn_lb"][0].reshape(2, 128).T)
        put(l, "alog", np.broadcast_to(inp["gdn_a_log"][l][None, :], (128, 4)))
        put(l, "dtb", np.broadcast_to(inp["gdn_dt_bias"][l][None, :], (128, 4)))
        put(l, "lq1", np.broadcast_to(inp["diff_lq1"][l][None, :], (128, 32)))
        put(l, "lk1", np.broadcast_to(inp["diff_lk1"][l][None, :], (128, 32)))
        put(l, "lq2", np.broadcast_to(inp["diff_lq2"][l][None, :], (128, 32)))
        put(l, "lk2", np.broadcast_to(inp["diff_lk2"][l][None, :], (128, 32)))
    return par


def build(T=2048, NSEQ=2, DEPTH=2, branches=("gdn", "hgrn", "moba", "diff"), tap=False):
    assert T % 512 == 0
    NT = T // 128
    NB = T // 512
    nc = bass.Bass("TRN2", target_bir_lowering=False)
    x_d = nc.dram_tensor("x", [NSEQ, 128, 8 * T], F32, kind="ExternalInput").ap()
    w_d = nc.dram_tensor("w", [DEPTH, NWB, 128, 1024], F32, kind="ExternalInput").ap()
    c32_d = nc.dram_tensor("c32", [128, NC32], F32, kind="ExternalInput").ap()
    c16_d = nc.dram_tensor("c16", [128, NC16], F32, kind="ExternalInput").ap()
    par_d = nc.dram_tensor("par", [DEPTH, 128, NPAR], F32, kind="ExternalInput").ap()
    y_d = nc.dram_tensor("y", [NSEQ, 128, 8 * T], F32, kind="ExternalOutput").ap()
    if tap:
        tap_d = nc.dram_tensor("tap", [NSEQ, DEPTH, 128, 8 * T], F32, kind="ExternalOutput").ap()

    P = Prog(nc)
    xD, wD, c32D, c16D, parD, yD = (P.dram(x_d, "x"), P.dram(w_d, "w"), P.dram(c32_d, "c32"),
                                    P.dram(c16_d, "c16"), P.dram(par_d, "par"), P.dram(y_d, "y"))
    tapD = P.dram(tap_d, "tap") if tap else None
    XT = P.sb("XT", [128, 8, T])
    HT = P.sb("HT", [128, 8, T], BF16)
    Y = P.sb("Y", [128, 8, T], BF16)
    K32 = P.sb("K32", [128, NC32])
    K16 = P.sb("K16", [128, NC16], BF16)
    PARS = [P.sb("PAR%d" % l, [128, NPAR]) for l in range(DEPTH)]
    NWS = 4
    WS = [P.sb("WS%d" % i, [128, 8, 128], BF16) for i in range(NWS)]
    NF, NH = 7, 10
    F = [P.sb("F%d" % i, [128, 512]) for i in range(NF)]
    H = [P.sb("H%d" % i, [128, 512], BF16) for i in range(NH)]
    SMB = [P.sb("SMB%d" % i, [128, 256], BF16) for i in range(29)]
    TTS = [P.sb("TT%d" % i, [128, 512], BF16) for i in range(2)]
    SMF = [P.sb("SMF%d" % i, [128, 256]) for i in range(2)]
    KT = P.sb("KT", [128, T], BF16)
    VB = P.sb("VB", [128, max(NT * 2 * 65, 2080)], BF16)
    KTs = P.sb("KTs", [128, max(T, 1040)], BF16) if T < 1040 else None
    TOK = [P.sb("TOK%d" % i, [128, NT * 4 if i == 0 else NT * 2]) for i in range(10)]
    SS = P.sb("SS", [128, 128])
    SSB = P.sb("SSB", [128, 128], BF16)
    SSB2 = P.sb("SSB2", [128, 128], BF16)
    COL = P.sb("COL", [128, 16])
    KS = P.sb("KS", [128, 2, 8])
    KSr = P.sb("KSr", [128, 8])
    DL = P.sb("DL", [128, NT * 2])
    ABI = P.sb("ABI", [128, 2, 16])
    PS = [P.ps("PS%d" % i, [128, 512]) for i in range(8)]

    def k32(name):
        o, w = C32[name]
        return K32[:, o:o + w]

    def k16(name):
        o, w = C16[name]
        return K16[:, o:o + w]

    P.dma(K32[:, :], c32D[:, :])
    P.dma(K16[:, :], c16D[:, :], q="pool")
    for l in range(DEPTH):
        P.dma(PARS[l][:, :], parD[l])

    wstate = {"next_slot": 0}

    def load_w(l, key):
        s = WS[wstate["next_slot"] % NWS]
        wstate["next_slot"] += 1
        P.dma(s[:, :, :], wD[l, CMB[key]].rearrange("p (k c) -> p k c", k=8), q="pool")
        return s

    def par(l, name, j=0, w=1):
        o, _ = PAR[name]
        return PARS[l][:, o + j:o + j + w]

    def rsqrt_from(out, in_, scale, eps):
        P.act(out, in_, AF.Ln, bias=eps, scale=scale)
        P.act(out, out, AF.Exp, scale=-0.5)

    def proj_cm(ps, wslot, src, cols):
        for k in range(8):
            P.mm(ps[:, :], wslot[:, k, :], src[:, k, cols], start=(k == 0), stop=(k == 7))

    def head_norm_gate(l, ops, gcol, yv, extra=1.0):
        osb, sq, rs = F[4], F[5], F[6]
        P.cp(osb[:, :], ops[:, :], eng="act")
        P.act(sq[:, :], ops[:, :], AF.Square)
        P.mm(PS[7][:, :], k32("blk64"), sq[:, :])
        rsqrt_from(rs[:, :], PS[7][:, :], 1.0 / HD, EPS)
        P.stt(osb[:, :], osb[:, :], gcol, rs[:, :], ALU.mult, ALU.mult)
        if extra != 1.0:
            P.stt(yv, osb[:, :], float(extra), yv, ALU.mult, ALU.mult)
        else:
            P.tt(yv, osb[:, :], yv, ALU.mult)

    def z_gate(l, wz, yblk):
        for n in range(NB):
            cols = slice(n * 512, (n + 1) * 512)
            ps = PS[5 + (n % 2)]
            proj_cm(ps, wz, HT, cols)
            P.act(Y[:, yblk, cols], ps[:, :], AF.Silu)

    def prenorm(l):
        for n in range(NB):
            cols = slice(n * 512, (n + 1) * 512)
            pend = None
            for k in range(8):
                sq = H[k % 2]
                P.act(sq[:, :], XT[:, k, cols], AF.Square)
                if pend is not None:
                    pend()
                pend = (lambda k=k, sq=sq: P.mm(PS[n % 2][:, :], ONESB[:, :], sq[:, :], start=(k == 0), stop=(k == 7)))
            pend()
            rs = F[2 + (n % 2)]
            rsqrt_from(rs[:, :], PS[n % 2][:, :], 1.0 / D_MODEL, EPS)
            for k in range(8):
                P.stt(HT[:, k, cols], XT[:, k, cols], par(l, "preg", k), rs[:, :], ALU.mult, ALU.mult)

    def attention(l, p, kind):
        yblk = (4 if kind == "moba" else 6) + p
        slopes = [2.0 ** -(2 * h + 2) for h in range(4)] if kind == "moba" else [2.0 ** -(2 * h + 1) for h in range(4)]
        dh = 64 if kind == "moba" else 32
        scale = dh ** -0.5
        wq = load_w(l, (kind, "q", p))
        wk = load_w(l, (kind, "k", p))
        wz = load_w(l, (kind, "z", p))
        wv = load_w(l, (kind, "tm", p))
        z_gate(l, wz, yblk)
        if kind == "moba":
            P.memset(KSr[:, :], 0.0)
        for n in range(NB):
            cols = slice(n * 512, (n + 1) * 512)
            ps = PS[5 + (n % 2)]
            proj_cm(ps, wk, HT, cols)
            P.cp(KT[:, cols], ps[:, :], eng="act")
            if kind == "moba":
                P.cp(F[3][:, :], ps[:, :])
                P.red(KSr[:, 2 * n:2 * n + 2], F[3][:, :].rearrange("p (b c) -> p b c", c=256), ALU.add)
        if kind == "moba":
            for hp in range(2):
                P.ts(KS[:, hp, :], KSr[:, :], k32("rm")[:, hp:hp + 1], None, ALU.mult)
        VA = VB[:, 0:NT * 2 * 65].rearrange("p (t h c) -> p t h c", t=NT, h=2)
        P.memset(VA[:, :, :, 64:65], 1.0)
        for t in range(NT):
            ps = PS[5 + (t % 2)]
            for k in range(8):
                P.mm(ps[:, 0:128], HT[:, k, t * 128:(t + 1) * 128], wv[:, k, :], start=(k == 0), stop=(k == 7))
            P.cp(VA[:, t, :, 0:64], ps[:, 0:128].rearrange("p (h c) -> p h c", h=2), eng="act")
        if kind == "diff":
            lam_init = 0.8 - 0.6 * math.exp(-0.3 * l)
            tmp = SMF[0]
            P.tt(tmp[:, 0:32], par(l, "lq1", 0, 32), par(l, "lk1", 0, 32), ALU.mult)
            P.red(COL[:, 0:1], tmp[:, 0:32], ALU.add)
            P.tt(tmp[:, 32:64], par(l, "lq2", 0, 32), par(l, "lk2", 0, 32), ALU.mult)
            P.red(COL[:, 1:2], tmp[:, 32:64], ALU.add)
            P.act(COL[:, 2:4], COL[:, 0:2], AF.Exp)
            P.tt(COL[:, 4:5], COL[:, 3:4], COL[:, 2:3], ALU.subtract)
            P.ts(COL[:, 5:6], COL[:, 4:5], -lam_init, None, ALU.add)
            P.ts(COL[:, 6:7], par(l, "diffg"), 1.0 - lam_init, None, ALU.mult)
        nmask = 2 if kind == "moba" else 4
        mask_tab = k16("hm2") if kind == "moba" else k16("hm4")
        QZ = [H[0], H[1], H[2], H[3]]
        QA = [H[4], H[5]]
        PT = [H[6], H[7], H[8]]
        OC = [F[0], F[1]]
        RC = [F[2], F[3]]
        for b_ in OC:
            P.memset(b_[:, :], 0.0)
        for hp_, b_ in enumerate(QA):
            P.memset(b_[:, :], 0.0)
            P.ts(b_[0:4, :], k16("qab")[0:4, :], float(slopes[2 * p + hp_]), None, ALU.mult)
            P.ts(ABI[:, hp_, :], k32("arel"), float(slopes[2 * p + hp_]), None, ALU.mult)
        bias_mode = [slopes[2 * p + hp_] <= 2.0 ** -4 for hp_ in range(2)]
        SELT = H[9]
        P.memset(SELT[:, :], 0.0)
        nmap = 1 if kind == "moba" else 2
        GF = [SMB[i][:, :].bitcast(F32) for i in range(6)]

        def block_begin(n):
            cols = slice(n * 512, (n + 1) * 512)
            psq = PS[5]
            proj_cm(psq, wq, HT, cols)
            for m in range(nmask):
                P.stt(QZ[m][:, :], psq[:, :], float(scale), mask_tab[:, m:m + 1].to_broadcast([128, 512]),
                      ALU.mult, ALU.mult)
            if not (kind == "moba" and n >= 2):
                return
            q32 = F[1]
            P.cp(q32[:, :], psq[:, :], eng="act")
            W = 2 * n + 1
            psg = PS[7]
            for t in range(4):
                for hp in range(2):
                    P.mm(psg[:, (t * 2 + hp) * 8:(t * 2 + hp) * 8 + 8], q32[:, t * 128:(t + 1) * 128], KS[:, hp, :])
            g = GF[0]
            P.cp(g[:, 0:64], psg[:, 0:64])
            g3 = g[:, 0:64].rearrange("p (a b) -> p a b", b=8)
            P.memset(g3[:, 0:4, 2 * n:2 * n + 1], -1e30)
            cur = g3[:, :, 0:W]
            m_ = GF[1]
            for it in range(3):
                P.red(m_[:, 8 * it:8 * it + 8], cur, ALU.max)
                if it == 2:
                    break
                e_ = GF[2 + it]
                e3 = e_[:, 0:64].rearrange("p (a b) -> p a b", b=8)[:, :, 0:W]
                P.tt(e3, cur, m_[:, 8 * it:8 * it + 8].unsqueeze(2).to_broadcast([128, 8, W]), ALU.is_ge)
                P.stt(e3, e3, -1e30, cur, ALU.mult, ALU.add)
                cur = e3
            mv = GF[4]
            P.memset(mv[:, 0:64], 0.0)
            mv3 = mv[:, 0:64].rearrange("p (a b) -> p a b", b=8)
            P.tt(mv3[:, :, 0:W], g3[:, :, 0:W], m_[:, 16:24].unsqueeze(2).to_broadcast([128, 8, W]), ALU.is_ge)
            P.ts(mv3[:, :, 0:W], mv3[:, :, 0:W], BIG, -BIG, ALU.mult, ALU.add)
            P.memset(mv3[:, 0:4, 2 * n:2 * n + 1], 0.0)
            pst = PS[7]
            for t in range(4):
                P.tr(pst[0:16, t * 128:(t + 1) * 128], mv[:, t * 16:(t + 1) * 16], k32("ident"))
            P.cp(SELT[0:16, :], pst[0:16, :])

        jobs = [(n, hp, mp, jt) for n in range(NB) for hp in range(2) for mp in range(nmap) for jt in range(4 * n + 4)]
        deferred = []

        def emitA(job, gi):
            n, hp, mp, jt = job
            if hp == 0 and mp == 0 and jt == 0:
                block_begin(n)
            use_sel = (kind == "moba" and n >= 2)
            m = hp * nmap + mp if kind == "diff" else hp
            qa = QA[hp]
            c0 = max(0, jt - 4 * n) * 128
            sc = PS[gi % 3]
            pt = PT[gi % 3]
            blk = jt // 2
            masked = use_sel and blk <= 2 * n
            diag = jt >= 4 * n
            bm = bias_mode[hp]
            P.mm(sc[:, c0:512], KT[:, jt * 128:(jt + 1) * 128], QZ[m][:, c0:512], start=True,
                 stop=bm and not (masked or diag))
            if not bm:
                ka = k16("ka")[:, (jt - 4 * n + 12) * 128:(jt - 4 * n + 13) * 128]
                P.mm(sc[:, c0:512], ka, qa[:, c0:512], start=False, stop=not (masked or diag))
            if masked:
                oh = k16("oh")[:, (hp * 8 + blk) * 128:(hp * 8 + blk + 1) * 128]
                P.mm(sc[:, c0:512], oh, SELT[:, c0:512], start=False, stop=not diag)
            if diag:
                P.mm(sc[:, c0:c0 + 128], k16("ident"), k16("causneg"), start=False, stop=True)
            if bm:
                r_ = jt - 4 * n + 12
                P.act(pt[:, c0:512], sc[:, c0:512], AF.Exp, bias=ABI[:, hp, r_:r_ + 1])
            else:
                P.act(pt[:, c0:512], sc[:, c0:512], AF.Exp)

        def emitB(job, gi):
            n, hp, mp, jt = job
            njt = 4 * n + 4
            cols = slice(n * 512, (n + 1) * 512)
            c0 = max(0, jt - 4 * n) * 128
            pt = PT[gi % 3]
            acc = PS[3 + mp]
            P.mm(acc[0:65, c0:512], VA[:, jt, hp, :], pt[:, c0:512], start=(jt == 0), stop=(jt == njt - 1))
            if jt == njt - 1:
                oc = OC[mp]
                P.cp(oc[0:65, :], acc[0:65, :], eng="act")

                def f2(oc=oc, hp=hp, mp=mp, cols=cols):
                    P.mm(PS[7][0:64, :], k32("selden"), oc[:, :])
                    rc = RC[mp]
                    P.act(rc[0:64, :], PS[7][0:64, :], AF.Ln)
                    P.act(rc[0:64, :], rc[0:64, :], AF.Exp, scale=-1.0)
                    P.tt(oc[0:64, :], oc[0:64, :], rc[0:64, :], ALU.mult)
                    if mp == nmap - 1:
                        if kind == "diff":
                            P.stt(OC[0][0:64, :], OC[1][0:64, :], COL[0:64, 5:6], OC[0][0:64, :], ALU.mult, ALU.add)

                        def f5(hp=hp, cols=cols):
                            P.mm(PS[6][:, :], k32("place")[:, hp * 128:(hp + 1) * 128], OC[0][:, :],
                                 start=(hp == 0), stop=(hp == 1))
                            if hp == 1:
                                if kind == "moba":
                                    P.tt(Y[:, yblk, cols], PS[6][:, :], Y[:, yblk, cols], ALU.mult)
                                else:
                                    head_norm_gate(l, PS[6], COL[:, 6:7], Y[:, yblk, cols])
                        deferred.append([2, f5])
                deferred.append([2, f2])

        def run_deferred(flush=False):
            i = 0
            while i < len(deferred):
                deferred[i][0] -= 1
                if flush or deferred[i][0] <= 0:
                    fn = deferred.pop(i)[1]
                    fn()
                else:
                    i += 1

        LOOK = 3
        for i in range(min(LOOK, len(jobs))):
            emitA(jobs[i], i)
        for i in range(len(jobs)):
            emitB(jobs[i], i)
            if i + LOOK < len(jobs):
                emitA(jobs[i + LOOK], i + LOOK)
            run_deferred()
        while deferred:
            run_deferred(flush=True)

    def hgrn(l, p):
        yblk = 2 + p
        wq = load_w(l, ("hgrn", "q", p))
        wf = load_w(l, ("hgrn", "f", p))
        wz = load_w(l, ("hgrn", "z", p))
        wv = load_w(l, ("hgrn", "tm", p))
        z_gate(l, wz, yblk)
        VH = VB[:, 0:NT * 128].rearrange("p (t c) -> p t c", t=NT)
        for t in range(NT):
            ps = PS[5 + (t % 2)]
            for k in range(8):
                P.mm(ps[:, 0:128], HT[:, k, t * 128:(t + 1) * 128], wv[:, k, :], start=(k == 0), stop=(k == 7))
            P.cp(VH[:, t, :], ps[:, 0:128], eng="act")
        if l == 0:
            P.memset(COL[:, 8:9], 0.0)
        else:
            P.tt(COL[:, 8:9], par(l, "lbl", p), par(l, "lb0", p), ALU.subtract)
            P.act(COL[:, 8:9], COL[:, 8:9], AF.Sigmoid)
        P.ts(COL[:, 9:10], COL[:, 8:9], -1.0, 1.0, ALU.mult, ALU.add)
        P.memset(SS[:, :], 0.0)
        P.memset(SSB[:, :], 0.0)
        P.memset(SSB2[:, :], 0.0)
        SSBs = [SSB, SSB2]
        KTb = KT if KTs is None else KTs
        E = KTb[:, 0:1024].bitcast(F32)
        QS, FG, G, DG = F[0], F[1], F[2], F[3]
        HSET = [(H[0], H[1], H[2], H[3]), (H[4], H[5], H[6], H[7])]
        DLHs = [SMF[0], SMF[1]]
        TSETS = [SMB[5 * i:5 * i + 5] for i in range(2)]
        for st_ in TSETS:
            P.memset(st_[0][:, :], 0.0)
            P.memset(st_[1][:, :], 0.0)

        def block_prep(n):
            cols = slice(n * 512, (n + 1) * 512)
            QG, KG, QD, KDT = HSET[n % 2]
            DLH = DLHs[n % 2]
            proj_cm(PS[5], wq, HT, cols)
            P.act(QS[:, :], PS[5][:, :], AF.Silu)
            yield
            proj_cm(PS[6], wf, HT, cols)
            P.act(FG[:, :], PS[6][:, :], AF.Sigmoid)
            yield
            P.ts(FG[:, :], FG[:, :], COL[:, 9:10], COL[:, 8:9], ALU.mult, ALU.add)
            yield
            P.act(E, FG[:, :], AF.Ln)
            yield
            P.scan(G[:, :], k32("scanm"), E)
            P.ts(FG[:, :], FG[:, :], -1.0, 1.0, ALU.mult, ALU.add)
            yield
            G3 = G[:, :].rearrange("p (n c) -> p n c", c=64)
            DG3 = DG[:, :].rearrange("p (n c) -> p n c", c=64)
            P.tt(DG3, G3, G3[:, :, 32:33].to_broadcast([128, 8, 64]), ALU.subtract)
            yield
            P.act(E, DG[:, :], AF.Exp)
            yield
            P.tt(QG[:, :], QS[:, :], E, ALU.mult)
            yield
            P.act(E, DG[:, :], AF.Exp, scale=-1.0)
            yield
            P.tt(KG[:, :], FG[:, :], E, ALU.mult)
            yield
            P.act(E, G[:, :], AF.Exp)
            yield
            P.tt(QD[:, :], QS[:, :], E, ALU.mult)
            P.tt(DG3, G3[:, :, 63:64].to_broadcast([128, 8, 64]), G3, ALU.subtract)
            yield
            P.act(E, DG[:, :], AF.Exp)
            yield
            P.tt(KDT[:, :], FG[:, :], E, ALU.mult)
            P.act(DLH[:, 0:8], G3[:, :, 63], AF.Exp)
            yield

        def tile_pre(n, t):
            gt = 4 * n + t
            tc = slice(t * 128, (t + 1) * 128)
            ATM, VZ, KD0_, KD1_, QGZ = TSETS[gt % 2]
            KD = [KD0_, KD1_]
            QG, KG, QD, KDT = HSET[n % 2]
            pst = PS[7]
            psa = PS[gt % 2]
            pss = PS[2 + gt % 2]
            P.tr(pst[:, 0:64].bitcast(BF16), KDT[:, tc], k16("ident"))
            P.tt(QGZ[:, :].rearrange("p (h c) -> p h c", h=2),
                 QG[:, tc].unsqueeze(1).to_broadcast([128, 2, 128]),
                 k16("hm2").unsqueeze(2).to_broadcast([128, 2, 128]), ALU.mult)
            for hp in range(2):
                P.cp(VZ[:, hp * 128 + hp * 64: hp * 128 + hp * 64 + 64], VH[:, gt, hp * 64:(hp + 1) * 64], eng="act")
            for hf in range(2):
                P.ts(KD[hf][:, 0:128], pst[:, 0:64].bitcast(BF16), k32("rm")[:, hf:hf + 1], None, ALU.mult)
            yield
            for hf in range(2):
                M = 64 if hf == 0 else 128
                for hp in range(2):
                    P.mm(psa[0:M, hp * 128 + hf * 64: hp * 128 + hf * 64 + 64],
                         KG[:, t * 128:t * 128 + M], QGZ[:, hp * 128 + hf * 64: hp * 128 + hf * 64 + 64])
            for hf in range(2):
                for hp in range(2):
                    P.mm(pss[:, hf * 128:(hf + 1) * 128], KD[hf][:, 0:128], VZ[:, hp * 128:(hp + 1) * 128],
                         start=(hp == 0), stop=(hp == 1))
            yield
            A3 = ATM[:, :].rearrange("p (h c) -> p h c", h=2)
            for hf in range(2):
                ic = slice(hf * 64, hf * 64 + 64)
                rows = slice(hf * 64, hf * 64 + 64)
                P.tt(A3[rows, :, ic], psa[:, 0:256].rearrange("p (h c) -> p h c", h=2)[rows, :, ic],
                     k16("caus01")[rows, ic].unsqueeze(1).to_broadcast([64, 2, 64]), ALU.mult)
            yield

        def tile_chain(n, t, pso):
            gt = 4 * n + t
            ATM, VZ, KD0_, KD1_, QGZ = TSETS[gt % 2]
            QG, KG, QD, KDT = HSET[n % 2]
            DLH = DLHs[n % 2]
            pss = PS[2 + gt % 2]
            A3 = ATM[:, :].rearrange("p (h c) -> p h c", h=2)
            for hf in range(2):
                ic = slice(hf * 64, hf * 64 + 64)
                oc = slice(t * 128 + hf * 64, t * 128 + hf * 64 + 64)
                cg = gt * 2 + hf
                P.mm(pso[:, oc], SSBs[cg % 2][:, :], QD[:, oc], start=True, stop=False)
                for hp in range(2):
                    P.mm(pso[:, oc], VZ[:, hp * 128:(hp + 1) * 128], A3[:, hp, ic], start=False, stop=(hp == 1))
                yield
                ch = t * 2 + hf
                for hp in range(2):
                    r = slice(hp * 64, hp * 64 + 64)
                    P.stt(SS[r, r], SS[r, r], DLH[r, ch:ch + 1], pss[r, hf * 128 + hp * 64:hf * 128 + hp * 64 + 64], ALU.mult, ALU.add)
                yield
                P.cp(SSBs[(cg + 1) % 2][:, :], SS[:, :], eng="act")
                yield
            if t == 3:
                cols = slice(n * 512, (n + 1) * 512)
                head_norm_gate(l, pso, par(l, "hgrng"), Y[:, yblk, cols])

        def step(g_):
            try:
                next(g_)
                return True
            except StopIteration:
                return False

        pso = PS[4]
        for _ in block_prep(0):
            pass
        prev = None
        bg = None
        for n in range(NB):
            for t in range(4):
                if t == 0:
                    if bg is not None:
                        for _ in bg:
                            pass
                    bg = block_prep(n + 1) if n + 1 < NB else None
                cur = tile_pre(n, t)
                alive = True
                while alive:
                    alive = step(cur)
                    if prev is not None and not step(prev):
                        prev = None
                    if t >= 1 and bg is not None and not step(bg):
                        bg = None
                if prev is not None:
                    for _ in prev:
                        pass
                prev = tile_chain(n, t, pso)
        for _ in prev:
            pass

    def gdn(l, p):
        yblk = p
        wz = load_w(l, ("gdn", "z", p))
        wab = load_w(l, ("gdn", "tm", p))
        wq = load_w(l, ("gdn", "q", p))
        wk = load_w(l, ("gdn", "k", p))
        z_gate(l, wz, yblk)
        AB, GR, BETA, G, GL, EG, BEG, EGLG, NEGG, TMP = TOK
        psab = PS[7]
        for t in range(NT):
            for k in range(8):
                P.mm(psab[:, t * 4:t * 4 + 4], HT[:, k, t * 128:(t + 1) * 128], wab[:, k, 0:4], start=(k == 0), stop=(k == 7))
        P.cp(AB[:, :], psab[:, 0:NT * 4])
        wv = load_w(l, ("gdn", "v", p))
        AB3 = AB[:, :].rearrange("p (t c) -> p t c", c=4)
        GR3 = GR[:, 0:NT * 2].rearrange("p (t c) -> p t c", c=2)
        dtb = par(l, "dtb", 2 * p, 2).unsqueeze(1).to_broadcast([128, NT, 2])
        P.tt(GR3, AB3[:, :, 0:2], dtb, ALU.add)
        T3 = TMP[:, 0:NT * 2].rearrange("p (t c) -> p t c", c=2)
        P.ts(T3, GR3, -30.0, 0.0, ALU.add, ALU.max)
        P.ts(GR3, GR3, 30.0, None, ALU.min)
        P.act(GR[:, 0:NT * 2], GR[:, 0:NT * 2], AF.Exp)
        P.act(GR[:, 0:NT * 2], GR[:, 0:NT * 2], AF.Ln, bias=1.0)
        P.tt(GR3, GR3, T3, ALU.add)
        P.act(COL[:, 10:12], par(l, "alog", 2 * p, 2), AF.Exp)
        P.ts(COL[:, 10:12], COL[:, 10:12], -1.0, None, ALU.mult)
        P.tt(GR3, GR3, COL[:, 10:12].unsqueeze(1).to_broadcast([128, NT, 2]), ALU.mult)
        B3 = BETA[:, 0:NT * 2].rearrange("p (t c) -> p t c", c=2)
        P.act(B3, AB3[:, :, 2:4], AF.Sigmoid)
        n2 = NT * 2
        P.mm(PS[7][:, 0:n2], k32("blktri"), GR[:, 0:n2])
        P.cp(G[:, 0:n2], PS[7][:, 0:n2])
        P.mm(PS[6][:, 0:n2], k32("blk64"), GR[:, 0:n2])
        P.cp(GL[:, 0:n2], PS[6][:, 0:n2])
        P.act(EG[:, 0:n2], G[:, 0:n2], AF.Exp)
        P.tt(BEG[:, 0:n2], EG[:, 0:n2], BETA[:, 0:n2], ALU.mult)
        P.tt(EGLG[:, 0:n2], GL[:, 0:n2], G[:, 0:n2], ALU.subtract)
        P.act(EGLG[:, 0:n2], EGLG[:, 0:n2], AF.Exp)
        P.ts(NEGG[:, 0:n2], G[:, 0:n2], -1.0, None, ALU.mult)
        REP = SMF[0]
        psd = PS[6]
        for t in range(NT):
            P.cp(REP[:, 0:128].rearrange("p (h c) -> p h c", h=2),
                 GL[:, t * 2:t * 2 + 2].unsqueeze(2).to_broadcast([128, 2, 64]))
            P.mm(psd[:, 256 + t * 2:256 + t * 2 + 2], REP[:, 0:128], k32("e2"))
        P.act(DL[:, 0:n2], psd[:, 256:256 + n2], AF.Exp)
        P.memset(SS[:, :], 0.0)
        P.memset(SSB[:, :], 0.0)
        VNZ = SMB[0]
        P.memset(VNZ[:, :], 0.0)
        KBGZ = SMB[1]
        P.memset(KBGZ[:, :], 0.0)
        KD0 = SMB[2]
        KTb = KT if KTs is None else KTs
        PC = [VB[:, 0:515], VB[:, 520:1035], KTb[:, 0:1030].bitcast(F32)]
        for b_ in PC:
            P.memset(b_[:, 0:3], 0.0)
        DIAG = []
        for bi_ in range(2):
            for j_ in range(4):
                dst_ = wab[:, bi_ * 4 + j_, :]
                P.ts(dst_, k16("ident"), par(l, "conv", (bi_ * 2 + p) * 4 + j_), None, ALU.mult, eng="pool")
                DIAG.append(dst_)
        ACC, CQ, SQ, RS = F[0], F[1], F[2], F[3]
        QN, KN, VT = H[0], H[1], H[2]
        convw = lambda blk, j: par(l, "conv", (blk * 2 + p) * 4 + j)
        hmb = k16("hm2").unsqueeze(2).to_broadcast([128, 2, 128])
        idb = k16("ident").unsqueeze(1).to_broadcast([128, 2, 128])

        def v3(x):
            return x[:, :].rearrange("p (h c) -> p h c", h=2)

        def prepinv(n, t, si):
            gt = 4 * n + t
            tc = slice(t * 128, (t + 1) * 128)
            KNZ, QNZ, D, DT, A, AQT, QD, AL, IZ, TQ, EGB = SMB[3 + 11 * si: 3 + 11 * si + 11]
            Tm, Tt = TTS[si][:, 0:256], TTS[si][:, 256:512]
            REPg = SMF[si]
            psGZ, psK, psT = PS[0 + si], PS[2 + si], PS[5 + si]
            P.tt(v3(KNZ), KN[:, tc].unsqueeze(1).to_broadcast([128, 2, 128]), hmb, ALU.mult)
            P.tt(v3(QNZ), QN[:, tc].unsqueeze(1).to_broadcast([128, 2, 128]), hmb, ALU.mult)
            P.cp(v3(REPg), GR[:, gt * 2:gt * 2 + 2].unsqueeze(2).to_broadcast([128, 2, 128]))
            yield
            for hp in range(2):
                hs = slice(hp * 128, (hp + 1) * 128)
                P.mm(psGZ[:, hs], REPg[:, hs], k32("blktri"))
                P.mm(psK[:, hs], KN[:, tc], KNZ[:, hs])
                P.mm(psK[:, 256 + hp * 128:256 + (hp + 1) * 128], KN[:, tc], QNZ[:, hs])
            yield
            for hp in range(2):
                hs = slice(hp * 128, (hp + 1) * 128)
                P.act(D[:, hs], psGZ[:, hs], AF.Exp, bias=G[:, gt * 2 + hp:gt * 2 + hp + 1], scale=-1.0)
                P.act(DT[:, hs], psGZ[:, hs], AF.Exp, bias=NEGG[:, gt * 2 + hp:gt * 2 + hp + 1], scale=1.0)
            P.act(EGB[:, :], psGZ[:, 0:256], AF.Exp)
            yield
            P.stt(v3(D), v3(D), 1e30, k16("m01").unsqueeze(1).to_broadcast([128, 2, 128]), ALU.min, ALU.mult)
            P.stt(v3(DT), v3(DT), 1e30, k16("m01T").unsqueeze(1).to_broadcast([128, 2, 128]), ALU.min, ALU.mult)
            yield
            for hp in range(2):
                hs = slice(hp * 128, (hp + 1) * 128)
                P.stt(A[:, hs], psK[:, hs], BETA[:, gt * 2 + hp:gt * 2 + hp + 1], D[:, hs], ALU.mult, ALU.mult)
            P.tt(AQT[:, :], psK[:, 256:512], DT[:, :], ALU.mult)
            P.tt(TQ[:, :], QNZ[:, :], EGB[:, :], ALU.mult)
            P.tt(QD[:, 0:128], TQ[:, 0:128], TQ[:, 128:256], ALU.add)
            yield
            for lv in range(6):
                lvm = k16("lvl")[:, lv * 128:(lv + 1) * 128].unsqueeze(1).to_broadcast([128, 2, 128])
                P.tt(v3(AL), v3(A), lvm, ALU.mult)
                yield
                if lv == 0:
                    P.stt(v3(Tm), v3(AL), -1.0, idb, ALU.mult, ALU.add)
                    for hp in range(2):
                        hs = slice(hp * 128, (hp + 1) * 128)
                        P.mm(psGZ[:, 256 + hp * 128:256 + (hp + 1) * 128], AL[:, hs], k16("ident"))
                    yield
                    P.stt(v3(Tt), psGZ[:, 256:512].rearrange("p (h c) -> p h c", h=2), -1.0, idb, ALU.mult, ALU.add)
                    yield
                    continue
                for hp in range(2):
                    hs = slice(hp * 128, (hp + 1) * 128)
                    P.mm(psGZ[:, 256 + hp * 128:256 + (hp + 1) * 128], AL[:, hs], Tt[:, hs])
                yield
                P.stt(v3(IZ), psGZ[:, 256:512].rearrange("p (h c) -> p h c", h=2), -1.0, idb, ALU.mult, ALU.add)
                yield
                for hp in range(2):
                    hs = slice(hp * 128, (hp + 1) * 128)
                    if lv < 5:
                        P.mm(psT[:, hs], IZ[:, hs], Tm[:, hs])
                    P.mm(psT[:, 256 + hp * 128:256 + (hp + 1) * 128], Tm[:, hs], IZ[:, hs])
                yield
                if lv < 5:
                    P.cp(TTS[si][:, :], psT[:, :], eng="act")
                else:
                    P.cp(Tt[:, :], psT[:, 256:512], eng="act")
                yield

        WT2 = [H[6], H[7]]
        U2 = [F[4], F[5]]
        KDs = [[KD0, H[3]], [H[8], H[9]]]
        AQT2 = [SMB[25], SMB[26]]
        QD2 = [SMB[27], SMB[28]]

        def post(n, t, si):
            gt = 4 * n + t
            tc = slice(t * 128, (t + 1) * 128)
            KNZ, QNZ, D, DT, A, AQT, QD, AL, IZ, TQ, EGB = SMB[3 + 11 * si: 3 + 11 * si + 11]
            Tm, Tt = TTS[si][:, 0:256], TTS[si][:, 256:512]
            pst = PS[2 + si]
            P.tr(pst[:, 0:64].bitcast(BF16), KN[:, tc], k16("ident"))
            P.tr(pst[:, 64:128].bitcast(BF16), VT[:, tc], k16("ident"))
            kt_ = pst[:, 0:64].bitcast(BF16)
            vt_ = pst[:, 64:128].bitcast(BF16)
            KD = KDs[si]
            VBt = H[4]
            KDall = H[5]
            P.cp(AQT2[si][:, :], AQT[:, :], eng="pool")
            P.cp(QD2[si][:, 0:128], QD[:, 0:128], eng="pool")
            yield
            for hp in range(2):
                cs = slice(hp * 64, (hp + 1) * 64)
                P.ts(KBGZ[:, hp * 128 + hp * 64: hp * 128 + hp * 64 + 64], kt_[:, cs], BEG[:, gt * 2 + hp:gt * 2 + hp + 1], None, ALU.mult)
                P.ts(KDall[:, cs], kt_[:, cs], EGLG[:, gt * 2 + hp:gt * 2 + hp + 1], None, ALU.mult)
                P.ts(VBt[:, cs], vt_[:, cs], BETA[:, gt * 2 + hp:gt * 2 + hp + 1], None, ALU.mult)
            for hf in range(2):
                P.ts(KD[hf][:, 0:128], KDall[:, 0:128], k32("rm")[:, hf:hf + 1], None, ALU.mult, eng="pool")
            for hp in range(2):
                P.mm(pst[:, 128:256], KBGZ[:, hp * 128:(hp + 1) * 128], Tt[:, hp * 128:(hp + 1) * 128], start=(hp == 0), stop=(hp == 1))
            for hp in range(2):
                P.mm(pst[:, 256 + hp * 64:256 + (hp + 1) * 64], Tt[:, hp * 128:(hp + 1) * 128], VBt[:, hp * 64:(hp + 1) * 64])
            yield
            P.cp(WT2[si][:, 0:128], pst[:, 128:256], eng="act")
            P.cp(U2[si][:, 0:128], pst[:, 256:384], eng="act")
            yield

        def scan_pair(n, tp, pso):
            for si in range(2):
                t = 2 * tp + si
                gt = 4 * n + t
                WT, U, KD, AQT, QD = WT2[si], U2[si], KDs[si], AQT2[si], QD2[si]
                for hf in range(2):
                    rows = slice(hf * 64, hf * 64 + 64)
                    ic = slice(hf * 64, hf * 64 + 64)
                    M = 64 if hf == 0 else 128
                    psws = PS[7]
                    P.mm(psws[0:M, 0:128], WT[:, 0:M], SSB[:, :])
                    yield
                    for hp in range(2):
                        cs = slice(hp * 64, (hp + 1) * 64)
                        P.tt(VNZ[rows, hp * 128 + hp * 64: hp * 128 + hp * 64 + 64], U[rows, cs], psws[rows, cs], ALU.subtract)
                    yield
                    oc = slice(t * 128 + hf * 64, t * 128 + hf * 64 + 64)
                    P.mm(pso[:, oc], SSB[:, :], QD[:, ic], start=True, stop=False)
                    for hp in range(2):
                        P.mm(pso[:, oc], VNZ[:, hp * 128:(hp + 1) * 128], AQT[:, hp * 128 + hf * 64: hp * 128 + hf * 64 + 64],
                             start=False, stop=(hp == 1))
                    pss = PS[7]
                    for hp in range(2):
                        P.mm(pss[:, 128:256], KD[hf][:, 0:128], VNZ[:, hp * 128:(hp + 1) * 128], start=(hp == 0), stop=(hp == 1))
                    yield
                    ch = gt * 2 + hf
                    for hp in range(2):
                        r = slice(hp * 64, hp * 64 + 64)
                        P.stt(SS[r, r], SS[r, r], DL[r, ch:ch + 1], pss[r, 128 + hp * 64:128 + hp * 64 + 64], ALU.mult, ALU.add)
                    yield
                    P.cp(SSB[:, :], SS[:, :], eng="act")
                    yield
            if tp == 1:
                cols = slice(n * 512, (n + 1) * 512)
                head_norm_gate(l, pso, par(l, "gdng"), Y[:, yblk, cols])

        def chain(*gs):
            for g_ in gs:
                yield from g_

        pso = PS[4]
        prev = None
        for n in range(NB):
            cols = slice(n * 512, (n + 1) * 512)
            for bi, (w_, dst) in enumerate(((wq, QN), (wk, KN), (wv, VT))):
                ps = PS[5 + (bi % 2)]
                proj_cm(ps, w_, HT, cols)
                pc = PC[bi]
                P.cp(pc[:, 3:515], ps[:, :], eng="act")
                if bi == 2:
                    P.ts(ACC[:, :], pc[:, 0:512], convw(bi, 0), None, ALU.mult)
                    for j in range(1, 4):
                        P.stt(ACC[:, :], pc[:, j:j + 512], convw(bi, j), ACC[:, :], ALU.mult, ALU.add)
                    P.cp(pc[:, 0:3], pc[:, 512:515])
                    P.act(VT[:, :], ACC[:, :], AF.Silu)
                else:
                    psc = PS[bi % 2]
                    for j in range(4):
                        P.mm(psc[:, :], DIAG[bi * 4 + j], pc[:, j:j + 512], start=(j == 0), stop=(j == 3))
                    P.cp(pc[:, 0:3], pc[:, 512:515], eng="pool")
                    P.act(CQ[:, :], psc[:, :], AF.Silu)
                    P.act(SQ[:, :], CQ[:, :], AF.Square)
                    P.mm(PS[7][:, :], k32("blk64"), SQ[:, :])
                    rsqrt_from(RS[:, :], PS[7][:, :], 1.0, EPS)
                    P.stt(dst[:, :], CQ[:, :], float(HD ** -0.5) if bi == 0 else 1.0, RS[:, :], ALU.mult, ALU.mult)
            for tp in range(2):
                gens = [chain(prepinv(n, 2 * tp + si, si), post(n, 2 * tp + si, si)) for si in range(2)]
                alive = list(gens)
                while alive:
                    for g_ in list(alive):
                        try:
                            next(g_)
                        except StopIteration:
                            alive.remove(g_)
                    if prev is not None:
                        try:
                            next(prev)
                        except StopIteration:
                            prev = None
                if prev is not None:
                    for _ in prev:
                        pass
                prev = scan_pair(n, tp, pso)
        for _ in prev:
            pass

    ONESB = P.sb("ONESB", [128, 128], BF16)
    P.memset(ONESB[:, :], 1.0)

    def outproj(l):
        KTb = KT if KTs is None else KTs
        OB = [F[i][:, :] for i in range(7)] + [KTb[:, 0:1024].bitcast(F32)]
        wviews = []
        for d in range(8):
            if d < 4:
                w_ = load_w(l, ("out", "w", d))
                wviews.append([w_[:, k, :] for k in range(8)])
            else:
                ha, hb = H[2 + 2 * (d - 4)], H[3 + 2 * (d - 4)]
                src = wD[l, CMB[("out", "w", d)]]
                P.dma(ha[:, :], src[:, 0:512], q="pool")
                P.dma(hb[:, :], src[:, 512:1024], q="pool")
                wviews.append([(ha if k < 4 else hb)[:, (k % 4) * 128:(k % 4 + 1) * 128] for k in range(8)])
        RSB = [VB[:, 0:1024].bitcast(F32), VB[:, 1024:2048].bitcast(F32)]

        def proj_d(n, d):
            cols = slice(n * 512, (n + 1) * 512)
            ps = PS[5 + (d % 2)]
            for k in range(8):
                P.mm(ps[:, :], wviews[d][k], Y[:, k, cols], start=(k == 0), stop=(k == 7))
            P.cp(OB[d], ps[:, :], eng="act")
            sq = H[d % 2]
            P.act(sq[:, :], ps[:, :], AF.Square)
            while pending:
                pending.pop(0)()
            pending.append(lambda n=n, d=d, sq=sq: P.mm(PS[n % 2][:, :], ONESB[:, :], sq[:, :],
                                                       start=(d == 0), stop=(d == 7)))

        pending = []

        def post_d(n, d):
            cols = slice(n * 512, (n + 1) * 512)
            rs = RSB[n % 2]
            P.stt(OB[d], OB[d], par(l, "postg", d), rs, ALU.mult, ALU.mult)
            P.tt(XT[:, d, cols], XT[:, d, cols], OB[d], ALU.add)

        for d in range(8):
            proj_d(0, d)
        for n in range(NB):
            while pending:
                pending.pop(0)()
            rsqrt_from(RSB[n % 2], PS[n % 2][:, :], 1.0 / D_MODEL, EPS)
            for d in range(8):
                post_d(n, d)
                if n + 1 < NB:
                    proj_d(n + 1, d)

    for s in range(NSEQ):
        for c in range(8):
            P.dma(XT[:, c, :], xD[s, :, c * T:(c + 1) * T])
        for l in range(DEPTH):
            P.mark("prenorm s%d l%d" % (s, l))
            prenorm(l)
            P.memset(Y[:, :, :], 0.0) if (len(branches) < 4) else None
            for p in range(2):
                if "gdn" in branches:
                    P.mark("gdn s%d l%d p%d" % (s, l, p))
                    gdn(l, p)
                if "hgrn" in branches:
                    P.mark("hgrn s%d l%d p%d" % (s, l, p))
                    hgrn(l, p)
                if "moba" in branches:
                    P.mark("moba s%d l%d p%d" % (s, l, p))
                    attention(l, p, "moba")
                if "diff" in branches:
                    P.mark("diff s%d l%d p%d" % (s, l, p))
                    attention(l, p, "diff")
            if tap:
                TP = F
                for c in range(8):
                    for n in range(NB):
                        cols = slice(n * 512, (n + 1) * 512)
                        P.cp(TP[(c * NB + n) % 4][:, :], Y[:, c, cols])
                        P.dma(tapD[s, l, :, c * T + n * 512: c * T + (n + 1) * 512], TP[(c * NB + n) % 4][:, :])
            P.mark("outproj s%d l%d" % (s, l))
            outproj(l)
        for c in range(8):
            P.dma(yD[s, :, c * T:(c + 1) * T], XT[:, c, :])
    P.mark("end")
    P.final_wait("sp", [XT] + ([F[0], F[1], F[2], F[3]] if tap else []))
    P.emit()
    return nc, P


_CACHE = {}


def _prep_inputs(inp, T, nseq_total):
    x = np.asarray(inp["x"], np.float32)
    B = x.shape[0]
    xT = np.ascontiguousarray(x.reshape(B, T, 8, 128).transpose(0, 3, 2, 1)).reshape(B, 128, 8 * T)
    w = pack_weights(np.asarray(inp["w_in"], np.float32), np.asarray(inp["w_out"], np.float32))
    par = pack_params({k: np.asarray(v, np.float32) for k, v in inp.items()})
    c32, c16 = make_consts()
    return xT, w, par, c32, c16


def kernel(**inputs):
    x = np.asarray(inputs["x"])
    B, T, D = x.shape
    depth = inputs["w_in"].shape[0]
    nseq = B // N_CORES
    key = (T, nseq, depth)
    if key not in _CACHE:
        _CACHE[key] = build(T=T, NSEQ=nseq, DEPTH=depth)[0]
    nc = _CACHE[key]
    xT, w, par, c32, c16 = _prep_inputs(inputs, T, B)
    in_maps = []
    for c in range(N_CORES):
        in_maps.append({"x": xT[c * nseq:(c + 1) * nseq], "w": w, "c32": c32, "c16": c16, "par": par})
    res = run_bass_kernel_spmd(nc, in_maps, core_ids=list(range(N_CORES)))
    outs = []
    for c in range(N_CORES):
        yT = np.asarray(res.results[c]["y"]).reshape(nseq, 128, 8, T)
        outs.append(yT.transpose(0, 3, 2, 1).reshape(nseq, T, D))
    return np.concatenate(outs, axis=0).astype(np.float32)
```

```python
import math
import numpy as np
import concourse.bass as bass
import concourse.mybir as mybir
from concourse.bass_utils import run_bass_kernel_spmd

F32 = mybir.dt.float32
BF16 = mybir.dt.bfloat16
AF = mybir.ActivationFunctionType
ALU = mybir.AluOpType
AX = mybir.AxisListType

D_MODEL = 1024
HD = 64
EPS = 1e-6
BIG = 30000.0
N_CORES = 8


class View:
    def __init__(self, buf, ap):
        self.buf = buf
        self.ap = ap

    def __getitem__(self, k):
        return View(self.buf, self.ap[k])

    def rearrange(self, *a, **k):
        return View(self.buf, self.ap.rearrange(*a, **k))

    def to_broadcast(self, *a, **k):
        return View(self.buf, self.ap.to_broadcast(*a, **k))

    def unsqueeze(self, *a, **k):
        return View(self.buf, self.ap.unsqueeze(*a, **k))

    def bitcast(self, *a, **k):
        return View(self.buf, self.ap.bitcast(*a, **k))


class Buf:
    def __init__(self, t, name):
        self.t = t
        self.name = name
        self.last_w = None
        self.reads = {}
        self.dma_sem = None
        self.is_psum = False

    def __getitem__(self, k):
        return View(self, self.t[k])


class Prog:
    ENG = ("pe", "act", "dve", "pool", "sp")

    def __init__(self, nc):
        self.nc = nc
        self.streams = {e: [] for e in self.ENG}
        self.sems = {}
        self.cnt = {}
        self.waited = {e: {} for e in self.ENG}
        self.marks = []
        for e in ("pe", "act", "dve", "pool"):
            self.sems[e] = nc.alloc_semaphore("s_" + e)
            self.cnt[e] = 0

    def mark(self, label):
        self.marks.append((label, dict(self.cnt)))

    def sb(self, name, shape, dtype=F32):
        return Buf(self.nc.alloc_sbuf_tensor(name, list(shape), dtype), name)

    def ps(self, name, shape, dtype=F32):
        b = Buf(self.nc.alloc_psum_tensor(name, list(shape), dtype), name)
        b.is_psum = True
        return b

    def dram(self, ap, name):
        return Buf(ap, name)

    def _deps(self, eng, reads, writes):
        need = {}

        def add(k, c):
            if k == "pe" and eng == "pe":
                return
            if need.get(k, 0) < c:
                need[k] = c
        for b in reads:
            if b.last_w is not None:
                add(*b.last_w)
            if b.is_psum:
                for k, c in b.reads.items():
                    if k != eng:
                        add(k, c)
        for b in writes:
            if b.last_w is not None:
                add(*b.last_w)
            for k, c in b.reads.items():
                add(k, c)
        out = []
        w = self.waited[eng]
        for k, c in need.items():
            if w.get(k, 0) < c:
                w[k] = c
                out.append((k, c))
        return out

    def op(self, eng, fn, reads=(), writes=()):
        reads = list(dict.fromkeys(reads))
        writes = list(dict.fromkeys(writes))
        waits = self._deps(eng, reads, writes)
        self.cnt[eng] += 1
        c = self.cnt[eng]
        self.streams[eng].append((waits, fn, (eng, 1)))
        for b in reads:
            b.reads[eng] = c
        for b in writes:
            b.last_w = (eng, c)
            b.reads = {}

    def dma(self, out, in_, q="sp"):
        ob, ib = out.buf, in_.buf
        owner = ob if ob.dma_sem is not None else (ib if ib.dma_sem is not None else ob)
        if owner.dma_sem is None:
            key = "dma%d" % len(self.sems)
            self.sems[key] = self.nc.alloc_semaphore(key)
            self.cnt[key] = 0
            owner.dma_sem = key
        key = owner.dma_sem
        waits = self._deps(q, [ib], [ob])
        self.cnt[key] += 16
        c = self.cnt[key]
        oa, ia = out.ap, in_.ap
        self.streams[q].append((waits, lambda e: e.dma_start(out=oa, in_=ia), (key, 16)))
        ib.reads[key] = c
        ob.last_w = (key, c)
        ob.reads = {}

    def final_wait(self, eng, bufs):
        waits = self._deps(eng, bufs, bufs)
        self.streams[eng].append((waits, None, None))

    def emit(self):
        nc = self.nc
        with nc.Block() as block:
            def mk(ename):
                def body(e):
                    for waits, fn, inc in self.streams[ename]:
                        for k, c in waits:
                            e.wait_ge(self.sems[k], c)
                        if fn is None:
                            continue
                        ins = fn(e)
                        ins.then_inc(self.sems[inc[0]], inc[1])
                return body
            if self.streams["pe"]:
                block.tensor(mk("pe"))
            if self.streams["act"]:
                block.scalar(mk("act"))
            if self.streams["dve"]:
                block.vector(mk("dve"))
            if self.streams["pool"]:
                block.gpsimd(mk("pool"))
            if self.streams["sp"]:
                block.sync(mk("sp"))

    def mm(self, out, lhsT, rhs, start=True, stop=True):
        o, l, r = out.ap, lhsT.ap, rhs.ap
        self.op("pe", lambda e: e.matmul(o, l, r, start=start, stop=stop),
                reads=[lhsT.buf, rhs.buf], writes=[out.buf])

    def tr(self, out, in_, ident):
        o, i, d = out.ap, in_.ap, ident.ap
        self.op("pe", lambda e: e.transpose(o, i, d), reads=[in_.buf, ident.buf], writes=[out.buf])

    def act(self, out, in_, func, bias=None, scale=1.0, eng="act"):
        o, i = out.ap, in_.ap
        rd = [in_.buf]
        kw = {}
        if isinstance(bias, View):
            rd.append(bias.buf)
            kw["bias"] = bias.ap
        elif bias is not None:
            kw["bias"] = float(bias)
        if isinstance(scale, View):
            rd.append(scale.buf)
            kw["scale"] = scale.ap
        else:
            kw["scale"] = float(scale)
        self.op("act", lambda e: e.activation(o, i, func, **kw), reads=rd, writes=[out.buf])

    def tt(self, out, in0, in1, op, eng="dve"):
        o, a, b = out.ap, in0.ap, in1.ap
        self.op(eng, lambda e: e.tensor_tensor(o, a, b, op), reads=[in0.buf, in1.buf], writes=[out.buf])

    def ts(self, out, in0, s1, s2, op0, op1=None, eng="dve"):
        o, a = out.ap, in0.ap
        rd = [in0.buf]
        v1 = s1
        if isinstance(s1, View):
            rd.append(s1.buf)
            v1 = s1.ap
        v2 = s2
        if isinstance(s2, View):
            rd.append(s2.buf)
            v2 = s2.ap
        if op1 is None:
            self.op(eng, lambda e: e.tensor_scalar(o, a, v1, None, op0), reads=rd, writes=[out.buf])
        else:
            self.op(eng, lambda e: e.tensor_scalar(o, a, v1, v2, op0, op1), reads=rd, writes=[out.buf])

    def stt(self, out, in0, scalar, in1, op0, op1, eng="dve"):
        o, a, b = out.ap, in0.ap, in1.ap
        rd = [in0.buf, in1.buf]
        sv = scalar
        if isinstance(scalar, View):
            rd.append(scalar.buf)
            sv = scalar.ap
        self.op(eng, lambda e: e.scalar_tensor_tensor(o, a, sv, b, op0, op1), reads=rd, writes=[out.buf])

    def cp(self, out, in_, eng="dve"):
        o, i = out.ap, in_.ap
        if eng == "act":
            self.op("act", lambda e: e.copy(o, i), reads=[in_.buf], writes=[out.buf])
        else:
            self.op(eng, lambda e: e.tensor_copy(o, i), reads=[in_.buf], writes=[out.buf])

    def memset(self, out, val, eng="dve"):
        o = out.ap
        self.op(eng, lambda e: e.memset(o, val), writes=[out.buf])

    def red(self, out, in_, op, eng="dve"):
        o, i = out.ap, in_.ap
        self.op(eng, lambda e: e.tensor_reduce(o, i, AX.X, op), reads=[in_.buf], writes=[out.buf])

    def recip(self, out, in_):
        o, i = out.ap, in_.ap
        self.op("dve", lambda e: e.reciprocal(o, i), reads=[in_.buf], writes=[out.buf])

    def scan(self, out, d0, d1):
        o, a, b = out.ap, d0.ap, d1.ap
        self.op("dve", lambda e: e.tensor_tensor_scan(o, a, b, 0.0, ALU.mult, ALU.add),
                reads=[d0.buf, d1.buf], writes=[out.buf])


C32 = {}
_o = 0
for _n, _w in (("ident", 128), ("blk64", 128), ("blktri", 128), ("scanm", 512), ("selden", 64), ("place", 256), ("e2", 2), ("rm", 2), ("arel", 16)):
    C32[_n] = (_o, _w)
    _o += _w
NC32 = _o
C16 = {}
_o = 0
for _n, _w in (("ident", 128), ("lvl", 6 * 128), ("caus01", 128), ("causneg", 128), ("hm2", 2), ("hm4", 4),
               ("ka", 16 * 128), ("oh", 16 * 128), ("qab", 512), ("m01", 128), ("m01T", 128)):
    C16[_n] = (_o, _w)
    _o += _w
NC16 = _o


def make_consts():
    c32 = np.zeros((128, NC32), np.float32)
    c16 = np.zeros((128, NC16), np.float32)
    p = np.arange(128)[:, None]
    c = np.arange(128)[None, :]
    same = (p // 64) == (c // 64)

    def put(dst, tab, name, arr):
        o, w = tab[name]
        dst[:, o:o + w] = arr
    put(c32, C32, "ident", (p == c).astype(np.float32))
    put(c32, C32, "blk64", same.astype(np.float32))
    put(c32, C32, "blktri", (same & (p <= c)).astype(np.float32))
    sm = np.ones((128, 512), np.float32)
    sm[:, ::64] = 0.0
    put(c32, C32, "scanm", sm)
    sd = np.zeros((128, 64), np.float32)
    sd[64, :] = 1.0
    put(c32, C32, "selden", sd)
    pl = np.zeros((128, 256), np.float32)
    for r in range(64):
        pl[r, r] = 1.0
        pl[r, 128 + 64 + r] = 1.0
    put(c32, C32, "place", pl)
    e2 = np.zeros((128, 2), np.float32)
    e2[0, 0] = 1.0
    e2[64, 1] = 1.0
    put(c32, C32, "e2", e2)
    arel = np.zeros((128, 16), np.float32)
    for r in range(16):
        arel[:, r] = np.arange(128) + 128.0 * (r - 12) - 256.0
    put(c32, C32, "arel", arel)
    rm = np.zeros((128, 2), np.float32)
    rm[:64, 0] = 1.0
    rm[64:, 1] = 1.0
    put(c32, C32, "rm", rm)

    put(c16, C16, "ident", (p == c).astype(np.float32))
    lv = np.zeros((128, 6, 128), np.float32)
    for l in range(1, 7):
        s = 2 ** l
        lv[:, l - 1, :] = ((p // s) == (c // s)) & ((p % s) >= s // 2) & ((c % s) < s // 2)
    put(c16, C16, "lvl", lv.reshape(128, 768))
    put(c16, C16, "m01", (same & (p >= c)).astype(np.float32))
    put(c16, C16, "m01T", (same & (c >= p)).astype(np.float32))
    put(c16, C16, "caus01", (p <= c).astype(np.float32))
    put(c16, C16, "causneg", np.where(p > c, -BIG, 0.0))
    hm2 = np.zeros((128, 2), np.float32)
    hm2[:64, 0] = 1.0
    hm2[64:, 1] = 1.0
    put(c16, C16, "hm2", hm2)
    hm4 = np.zeros((128, 4), np.float32)
    for g in range(4):
        hm4[g * 32:(g + 1) * 32, g] = 1.0
    put(c16, C16, "hm4", hm4)
    ka = np.zeros((128, 16, 128), np.float32)
    for ri in range(16):
        ka[0, ri, :] = np.arange(128)
        ka[1, ri, :] = 1.0
        ka[2, ri, :] = 128.0 * (ri - 12)
        ka[3, ri, :] = 1.0
    put(c16, C16, "ka", ka.reshape(128, 2048))
    oh = np.zeros((128, 16, 128), np.float32)
    for r in range(16):
        oh[r, r, :] = 1.0
    put(c16, C16, "oh", oh.reshape(128, 2048))
    qab = np.zeros((128, 512), np.float32)
    il = np.arange(512)
    qab[0, :] = 1.0
    qab[1, :] = -(il % 128)
    qab[2, :] = 1.0
    qab[3, :] = -128.0 * (il // 128)
    put(c16, C16, "qab", qab)
    return c32, c16


PAR = {}
_o = 0
for _n, _w in (("preg", 8), ("postg", 8), ("conv", 24), ("gdng", 1), ("hgrng", 1), ("diffg", 1),
               ("lbl", 2), ("lb0", 2), ("alog", 4), ("dtb", 4), ("lq1", 32), ("lk1", 32), ("lq2", 32), ("lk2", 32)):
    PAR[_n] = (_o, _w)
    _o += _w
NPAR = _o

CMB = {}
_i = 0
for _br, _names in (("gdn", ("q", "k", "v", "z")), ("hgrn", ("q", "f", "z")), ("moba", ("q", "k", "z")),
                    ("diff", ("q", "k", "z"))):
    for _nm in _names:
        for _p in range(2):
            CMB[(_br, _nm, _p)] = _i
            _i += 1
for _br in ("gdn", "hgrn", "moba", "diff"):
    for _p in range(2):
        CMB[(_br, "tm", _p)] = _i
        _i += 1
for _d in range(8):
    CMB[("out", "w", _d)] = _i
    _i += 1
NWB = _i

GDN_BASE, HGRN_BASE, MOBA_BASE, DIFF_BASE = 0, 1032, 2056, 3080


def pack_weights(w_in, w_out):
    depth = w_in.shape[0]
    out = np.zeros((depth, NWB, 128, 1024), np.float32)

    def blockify(wcols):
        return wcols.reshape(8, 128, 128).transpose(1, 0, 2).reshape(128, 1024)
    for l in range(depth):
        W = w_in[l]
        def cols(base, off, p):
            return W[:, base + off + p * 128: base + off + (p + 1) * 128]
        for p in range(2):
            out[l, CMB[("gdn", "q", p)]] = blockify(cols(GDN_BASE, 0, p))
            out[l, CMB[("gdn", "k", p)]] = blockify(cols(GDN_BASE, 256, p))
            out[l, CMB[("gdn", "v", p)]] = blockify(cols(GDN_BASE, 512, p))
            out[l, CMB[("gdn", "z", p)]] = blockify(cols(GDN_BASE, 776, p))
            ab = np.zeros((1024, 128), np.float32)
            ab[:, 0:2] = W[:, GDN_BASE + 768 + 2 * p: GDN_BASE + 768 + 2 * p + 2]
            ab[:, 2:4] = W[:, GDN_BASE + 772 + 2 * p: GDN_BASE + 772 + 2 * p + 2]
            out[l, CMB[("gdn", "tm", p)]] = blockify(ab)
            out[l, CMB[("hgrn", "q", p)]] = blockify(cols(HGRN_BASE, 0, p))
            out[l, CMB[("hgrn", "f", p)]] = blockify(cols(HGRN_BASE, 256, p))
            out[l, CMB[("hgrn", "tm", p)]] = blockify(cols(HGRN_BASE, 512, p))
            out[l, CMB[("hgrn", "z", p)]] = blockify(cols(HGRN_BASE, 768, p))
            for br, base in (("moba", MOBA_BASE), ("diff", DIFF_BASE)):
                out[l, CMB[(br, "q", p)]] = blockify(cols(base, 0, p))
                out[l, CMB[(br, "k", p)]] = blockify(cols(base, 256, p))
                out[l, CMB[(br, "tm", p)]] = blockify(cols(base, 512, p))
                out[l, CMB[(br, "z", p)]] = blockify(cols(base, 768, p))
        for d in range(8):
            out[l, CMB[("out", "w", d)]] = blockify(w_out[l][:, d * 128:(d + 1) * 128])
    return out


def pack_params(inp):
    depth = inp["pre_norm_g"].shape[0]
    par = np.zeros((depth, 128, NPAR), np.float32)

    def put(l, name, arr):
        o, w = PAR[name]
        par[l, :, o:o + w] = arr
    for l in range(depth):
        put(l, "preg", inp["pre_norm_g"][l].reshape(8, 128).T)
        put(l, "postg", inp["post_norm_g"][l].reshape(8, 128).T)
        cw = inp["conv_w"][l]
        put(l, "conv", cw.reshape(4, 6, 128).transpose(2, 1, 0).reshape(128, 24))
        put(l, "gdng", np.tile(inp["gdn_norm_g"][l], 2)[:, None])
        put(l, "hgrng", np.tile(inp["hgrn_norm_g"][l], 2)[:, None])
        put(l, "diffg", np.tile(inp["diff_norm_g"][l], 2)[:, None])
        put(l, "lbl", inp["hgrn_lb"][l].reshape(2, 128).T)
        put(l, "lb0", inp["hgrn_lb"][0].reshape(2, 128).T)
        put(l, "alog", np.broadcast_to(inp["gdn_a_log"][l][None, :], (128, 4)))
        put(l, "dtb", np.broadcast_to(inp["gdn_dt_bias"][l][None, :], (128, 4)))
        put(l, "lq1", np.broadcast_to(inp["diff_lq1"][l][None, :], (128, 32)))
        put(l, "lk1", np.broadcast_to(inp["diff_lk1"][l][None, :], (128, 32)))
        put(l, "lq2", np.broadcast_to(inp["diff_lq2"][l][None, :], (128, 32)))
        put(l, "lk2", np.broadcast_to(inp["diff_lk2"][l][None, :], (128, 32)))
    return par


def build(T=2048, NSEQ=2, DEPTH=2, branches=("gdn", "hgrn", "moba", "diff"), tap=False):
    assert T % 512 == 0
    NT = T // 128
    NB = T // 512
    nc = bass.Bass("TRN2", target_bir_lowering=False)
    x_d = nc.dram_tensor("x", [NSEQ, 128, 8 * T], F32, kind="ExternalInput").ap()
    w_d = nc.dram_tensor("w", [DEPTH, NWB, 128, 1024], F32, kind="ExternalInput").ap()
    c32_d = nc.dram_tensor("c32", [128, NC32], F32, kind="ExternalInput").ap()
    c16_d = nc.dram_tensor("c16", [128, NC16], F32, kind="ExternalInput").ap()
    par_d = nc.dram_tensor("par", [DEPTH, 128, NPAR], F32, kind="ExternalInput").ap()
    y_d = nc.dram_tensor("y", [NSEQ, 128, 8 * T], F32, kind="ExternalOutput").ap()
    if tap:
        tap_d = nc.dram_tensor("tap", [NSEQ, DEPTH, 128, 8 * T], F32, kind="ExternalOutput").ap()

    P = Prog(nc)
    xD, wD, c32D, c16D, parD, yD = (P.dram(x_d, "x"), P.dram(w_d, "w"), P.dram(c32_d, "c32"),
                                    P.dram(c16_d, "c16"), P.dram(par_d, "par"), P.dram(y_d, "y"))
    tapD = P.dram(tap_d, "tap") if tap else None
    XT = P.sb("XT", [128, 8, T])
    HT = P.sb("HT", [128, 8, T], BF16)
    Y = P.sb("Y", [128, 8, T], BF16)
    K32 = P.sb("K32", [128, NC32])
    K16 = P.sb("K16", [128, NC16], BF16)
    PARS = [P.sb("PAR%d" % l, [128, NPAR]) for l in range(DEPTH)]
    NWS = 4
    WS = [P.sb("WS%d" % i, [128, 8, 128], BF16) for i in range(NWS)]
    NF, NH = 7, 10
    F = [P.sb("F%d" % i, [128, 512]) for i in range(NF)]
    H = [P.sb("H%d" % i, [128, 512], BF16) for i in range(NH)]
    SMB = [P.sb("SMB%d" % i, [128, 256], BF16) for i in range(29)]
    TTS = [P.sb("TT%d" % i, [128, 512], BF16) for i in range(2)]
    SMF = [P.sb("SMF%d" % i, [128, 256]) for i in range(2)]
    KT = P.sb("KT", [128, T], BF16)
    VB = P.sb("VB", [128, max(NT * 2 * 65, 2080)], BF16)
    KTs = P.sb("KTs", [128, max(T, 1040)], BF16) if T < 1040 else None
    TOK = [P.sb("TOK%d" % i, [128, NT * 4 if i == 0 else NT * 2]) for i in range(10)]
    SS = P.sb("SS", [128, 128])
    SSB = P.sb("SSB", [128, 128], BF16)
    SSB2 = P.sb("SSB2", [128, 128], BF16)
    COL = P.sb("COL", [128, 16])
    KS = P.sb("KS", [128, 2, 8])
    KSr = P.sb("KSr", [128, 8])
    DL = P.sb("DL", [128, NT * 2])
    ABI = P.sb("ABI", [128, 2, 16])
    PS = [P.ps("PS%d" % i, [128, 512]) for i in range(8)]

    def k32(name):
        o, w = C32[name]
        return K32[:, o:o + w]

    def k16(name):
        o, w = C16[name]
        return K16[:, o:o + w]

    P.dma(K32[:, :], c32D[:, :])
    P.dma(K16[:, :], c16D[:, :], q="pool")
    for l in range(DEPTH):
        P.dma(PARS[l][:, :], parD[l])

    wstate = {"next_slot": 0}

    def load_w(l, key):
        s = WS[wstate["next_slot"] % NWS]
        wstate["next_slot"] += 1
        P.dma(s[:, :, :], wD[l, CMB[key]].rearrange("p (k c) -> p k c", k=8), q="pool")
        return s

    def par(l, name, j=0, w=1):
        o, _ = PAR[name]
        return PARS[l][:, o + j:o + j + w]

    def rsqrt_from(out, in_, scale, eps):
        P.act(out, in_, AF.Ln, bias=eps, scale=scale)
        P.act(out, out, AF.Exp, scale=-0.5)

    def proj_cm(ps, wslot, src, cols):
        for k in range(8):
            P.mm(ps[:, :], wslot[:, k, :], src[:, k, cols], start=(k == 0), stop=(k == 7))

    def head_norm_gate(l, ops, gcol, yv, extra=1.0):
        osb, sq, rs = F[4], F[5], F[6]
        P.cp(osb[:, :], ops[:, :], eng="act")
        P.act(sq[:, :], ops[:, :], AF.Square)
        P.mm(PS[7][:, :], k32("blk64"), sq[:, :])
        rsqrt_from(rs[:, :], PS[7][:, :], 1.0 / HD, EPS)
        P.stt(osb[:, :], osb[:, :], gcol, rs[:, :], ALU.mult, ALU.mult)
        if extra != 1.0:
            P.stt(yv, osb[:, :], float(extra), yv, ALU.mult, ALU.mult)
        else:
            P.tt(yv, osb[:, :], yv, ALU.mult)

    def z_gate(l, wz, yblk):
        for n in range(NB):
            cols = slice(n * 512, (n + 1) * 512)
            ps = PS[5 + (n % 2)]
            proj_cm(ps, wz, HT, cols)
            P.act(Y[:, yblk, cols], ps[:, :], AF.Silu)

    def prenorm(l):
        for n in range(NB):
            cols = slice(n * 512, (n + 1) * 512)
            for k in range(8):
                sq = H[k % 2]
                P.act(sq[:, :], XT[:, k, cols], AF.Square)
                P.mm(PS[n % 2][:, :], ONESB[:, :], sq[:, :], start=(k == 0), stop=(k == 7))
            rs = F[2 + (n % 2)]
            rsqrt_from(rs[:, :], PS[n % 2][:, :], 1.0 / D_MODEL, EPS)
            for k in range(8):
                P.stt(HT[:, k, cols], XT[:, k, cols], par(l, "preg", k), rs[:, :], ALU.mult, ALU.mult)

    def attention(l, p, kind):
        yblk = (4 if kind == "moba" else 6) + p
        slopes = [2.0 ** -(2 * h + 2) for h in range(4)] if kind == "moba" else [2.0 ** -(2 * h + 1) for h in range(4)]
        dh = 64 if kind == "moba" else 32
        scale = dh ** -0.5
        wq = load_w(l, (kind, "q", p))
        wk = load_w(l, (kind, "k", p))
        wz = load_w(l, (kind, "z", p))
        wv = load_w(l, (kind, "tm", p))
        z_gate(l, wz, yblk)
        if kind == "moba":
            P.memset(KSr[:, :], 0.0)
        for n in range(NB):
            cols = slice(n * 512, (n + 1) * 512)
            ps = PS[5 + (n % 2)]
            proj_cm(ps, wk, HT, cols)
            P.cp(KT[:, cols], ps[:, :], eng="act")
            if kind == "moba":
                P.cp(F[3][:, :], ps[:, :])
                P.red(KSr[:, 2 * n:2 * n + 2], F[3][:, :].rearrange("p (b c) -> p b c", c=256), ALU.add)
        if kind == "moba":
            for hp in range(2):
                P.ts(KS[:, hp, :], KSr[:, :], k32("rm")[:, hp:hp + 1], None, ALU.mult)
        VA = VB[:, 0:NT * 2 * 65].rearrange("p (t h c) -> p t h c", t=NT, h=2)
        P.memset(VA[:, :, :, 64:65], 1.0)
        for t in range(NT):
            ps = PS[5 + (t % 2)]
            for k in range(8):
                P.mm(ps[:, 0:128], HT[:, k, t * 128:(t + 1) * 128], wv[:, k, :], start=(k == 0), stop=(k == 7))
            P.cp(VA[:, t, :, 0:64], ps[:, 0:128].rearrange("p (h c) -> p h c", h=2), eng="act")
        if kind == "diff":
            lam_init = 0.8 - 0.6 * math.exp(-0.3 * l)
            tmp = SMF[0]
            P.tt(tmp[:, 0:32], par(l, "lq1", 0, 32), par(l, "lk1", 0, 32), ALU.mult)
            P.red(COL[:, 0:1], tmp[:, 0:32], ALU.add)
            P.tt(tmp[:, 32:64], par(l, "lq2", 0, 32), par(l, "lk2", 0, 32), ALU.mult)
            P.red(COL[:, 1:2], tmp[:, 32:64], ALU.add)
            P.act(COL[:, 2:4], COL[:, 0:2], AF.Exp)
            P.tt(COL[:, 4:5], COL[:, 3:4], COL[:, 2:3], ALU.subtract)
            P.ts(COL[:, 5:6], COL[:, 4:5], -lam_init, None, ALU.add)
            P.ts(COL[:, 6:7], par(l, "diffg"), 1.0 - lam_init, None, ALU.mult)
        nmask = 2 if kind == "moba" else 4
        mask_tab = k16("hm2") if kind == "moba" else k16("hm4")
        QZ = [H[0], H[1], H[2], H[3]]
        QA = [H[4], H[5]]
        PT = [H[6], H[7], H[8]]
        OC = [F[0], F[1]]
        RC = [F[2], F[3]]
        for b_ in OC:
            P.memset(b_[:, :], 0.0)
        for hp_, b_ in enumerate(QA):
            P.memset(b_[:, :], 0.0)
            P.ts(b_[0:4, :], k16("qab")[0:4, :], float(slopes[2 * p + hp_]), None, ALU.mult)
            P.ts(ABI[:, hp_, :], k32("arel"), float(slopes[2 * p + hp_]), None, ALU.mult)
        bias_mode = [slopes[2 * p + hp_] <= 2.0 ** -4 for hp_ in range(2)]
        SELT = H[9]
        P.memset(SELT[:, :], 0.0)
        nmap = 1 if kind == "moba" else 2
        GF = [SMB[i][:, :].bitcast(F32) for i in range(6)]

        def block_begin(n):
            cols = slice(n * 512, (n + 1) * 512)
            psq = PS[5]
            proj_cm(psq, wq, HT, cols)
            for m in range(nmask):
                P.stt(QZ[m][:, :], psq[:, :], float(scale), mask_tab[:, m:m + 1].to_broadcast([128, 512]),
                      ALU.mult, ALU.mult)
            if not (kind == "moba" and n >= 2):
                return
            q32 = F[1]
            P.cp(q32[:, :], psq[:, :], eng="act")
            W = 2 * n + 1
            psg = PS[7]
            for t in range(4):
                for hp in range(2):
                    P.mm(psg[:, (t * 2 + hp) * 8:(t * 2 + hp) * 8 + 8], q32[:, t * 128:(t + 1) * 128], KS[:, hp, :])
            g = GF[0]
            P.cp(g[:, 0:64], psg[:, 0:64])
            g3 = g[:, 0:64].rearrange("p (a b) -> p a b", b=8)
            P.memset(g3[:, 0:4, 2 * n:2 * n + 1], -1e30)
            cur = g3[:, :, 0:W]
            m_ = GF[1]
            for it in range(3):
                P.red(m_[:, 8 * it:8 * it + 8], cur, ALU.max)
                if it == 2:
                    break
                e_ = GF[2 + it]
                e3 = e_[:, 0:64].rearrange("p (a b) -> p a b", b=8)[:, :, 0:W]
                P.tt(e3, cur, m_[:, 8 * it:8 * it + 8].unsqueeze(2).to_broadcast([128, 8, W]), ALU.is_ge)
                P.stt(e3, e3, -1e30, cur, ALU.mult, ALU.add)
                cur = e3
            mv = GF[4]
            P.memset(mv[:, 0:64], 0.0)
            mv3 = mv[:, 0:64].rearrange("p (a b) -> p a b", b=8)
            P.tt(mv3[:, :, 0:W], g3[:, :, 0:W], m_[:, 16:24].unsqueeze(2).to_broadcast([128, 8, W]), ALU.is_ge)
            P.ts(mv3[:, :, 0:W], mv3[:, :, 0:W], BIG, -BIG, ALU.mult, ALU.add)
            P.memset(mv3[:, 0:4, 2 * n:2 * n + 1], 0.0)
            pst = PS[7]
            for t in range(4):
                P.tr(pst[0:16, t * 128:(t + 1) * 128], mv[:, t * 16:(t + 1) * 16], k32("ident"))
            P.cp(SELT[0:16, :], pst[0:16, :])

        jobs = [(n, hp, mp, jt) for n in range(NB) for hp in range(2) for mp in range(nmap) for jt in range(4 * n + 4)]
        deferred = []

        def emitA(job, gi):
            n, hp, mp, jt = job
            if hp == 0 and mp == 0 and jt == 0:
                block_begin(n)
            use_sel = (kind == "moba" and n >= 2)
            m = hp * nmap + mp if kind == "diff" else hp
            qa = QA[hp]
            c0 = max(0, jt - 4 * n) * 128
            sc = PS[gi % 3]
            pt = PT[gi % 3]
            blk = jt // 2
            masked = use_sel and blk <= 2 * n
            diag = jt >= 4 * n
            bm = bias_mode[hp]
            P.mm(sc[:, c0:512], KT[:, jt * 128:(jt + 1) * 128], QZ[m][:, c0:512], start=True,
                 stop=bm and not (masked or diag))
            if not bm:
                ka = k16("ka")[:, (jt - 4 * n + 12) * 128:(jt - 4 * n + 13) * 128]
                P.mm(sc[:, c0:512], ka, qa[:, c0:512], start=False, stop=not (masked or diag))
            if masked:
                oh = k16("oh")[:, (hp * 8 + blk) * 128:(hp * 8 + blk + 1) * 128]
                P.mm(sc[:, c0:512], oh, SELT[:, c0:512], start=False, stop=not diag)
            if diag:
                P.mm(sc[:, c0:c0 + 128], k16("ident"), k16("causneg"), start=False, stop=True)
            if bm:
                r_ = jt - 4 * n + 12
                P.act(pt[:, c0:512], sc[:, c0:512], AF.Exp, bias=ABI[:, hp, r_:r_ + 1])
            else:
                P.act(pt[:, c0:512], sc[:, c0:512], AF.Exp)

        def emitB(job, gi):
            n, hp, mp, jt = job
            njt = 4 * n + 4
            cols = slice(n * 512, (n + 1) * 512)
            c0 = max(0, jt - 4 * n) * 128
            pt = PT[gi % 3]
            acc = PS[3 + mp]
            P.mm(acc[0:65, c0:512], VA[:, jt, hp, :], pt[:, c0:512], start=(jt == 0), stop=(jt == njt - 1))
            if jt == njt - 1:
                oc = OC[mp]
                P.cp(oc[0:65, :], acc[0:65, :], eng="act")

                def f2(oc=oc, hp=hp, mp=mp, cols=cols):
                    P.mm(PS[7][0:64, :], k32("selden"), oc[:, :])
                    rc = RC[mp]
                    P.act(rc[0:64, :], PS[7][0:64, :], AF.Ln)
                    P.act(rc[0:64, :], rc[0:64, :], AF.Exp, scale=-1.0)
                    P.tt(oc[0:64, :], oc[0:64, :], rc[0:64, :], ALU.mult)
                    if mp == nmap - 1:
                        if kind == "diff":
                            P.stt(OC[0][0:64, :], OC[1][0:64, :], COL[0:64, 5:6], OC[0][0:64, :], ALU.mult, ALU.add)

                        def f5(hp=hp, cols=cols):
                            P.mm(PS[6][:, :], k32("place")[:, hp * 128:(hp + 1) * 128], OC[0][:, :],
                                 start=(hp == 0), stop=(hp == 1))
                            if hp == 1:
                                if kind == "moba":
                                    P.tt(Y[:, yblk, cols], PS[6][:, :], Y[:, yblk, cols], ALU.mult)
                                else:
                                    head_norm_gate(l, PS[6], COL[:, 6:7], Y[:, yblk, cols])
                        deferred.append([2, f5])
                deferred.append([2, f2])

        def run_deferred(flush=False):
            i = 0
            while i < len(deferred):
                deferred[i][0] -= 1
                if flush or deferred[i][0] <= 0:
                    fn = deferred.pop(i)[1]
                    fn()
                else:
                    i += 1

        LOOK = 3
        for i in range(min(LOOK, len(jobs))):
            emitA(jobs[i], i)
        for i in range(len(jobs)):
            emitB(jobs[i], i)
            if i + LOOK < len(jobs):
                emitA(jobs[i + LOOK], i + LOOK)
            run_deferred()
        while deferred:
            run_deferred(flush=True)

    def hgrn(l, p):
        yblk = 2 + p
        wq = load_w(l, ("hgrn", "q", p))
        wf = load_w(l, ("hgrn", "f", p))
        wz = load_w(l, ("hgrn", "z", p))
        wv = load_w(l, ("hgrn", "tm", p))
        z_gate(l, wz, yblk)
        VH = VB[:, 0:NT * 128].rearrange("p (t c) -> p t c", t=NT)
        for t in range(NT):
            ps = PS[5 + (t % 2)]
            for k in range(8):
                P.mm(ps[:, 0:128], HT[:, k, t * 128:(t + 1) * 128], wv[:, k, :], start=(k == 0), stop=(k == 7))
            P.cp(VH[:, t, :], ps[:, 0:128], eng="act")
        if l == 0:
            P.memset(COL[:, 8:9], 0.0)
        else:
            P.tt(COL[:, 8:9], par(l, "lbl", p), par(l, "lb0", p), ALU.subtract)
            P.act(COL[:, 8:9], COL[:, 8:9], AF.Sigmoid)
        P.ts(COL[:, 9:10], COL[:, 8:9], -1.0, 1.0, ALU.mult, ALU.add)
        P.memset(SS[:, :], 0.0)
        P.memset(SSB[:, :], 0.0)
        P.memset(SSB2[:, :], 0.0)
        SSBs = [SSB, SSB2]
        KTb = KT if KTs is None else KTs
        E = KTb[:, 0:1024].bitcast(F32)
        QS, FG, G, DG = F[0], F[1], F[2], F[3]
        HSET = [(H[0], H[1], H[2], H[3]), (H[4], H[5], H[6], H[7])]
        DLHs = [SMF[0], SMF[1]]
        TSETS = [SMB[5 * i:5 * i + 5] for i in range(2)]
        for st_ in TSETS:
            P.memset(st_[0][:, :], 0.0)
            P.memset(st_[1][:, :], 0.0)

        def block_prep(n):
            cols = slice(n * 512, (n + 1) * 512)
            QG, KG, QD, KDT = HSET[n % 2]
            DLH = DLHs[n % 2]
            proj_cm(PS[5], wq, HT, cols)
            P.act(QS[:, :], PS[5][:, :], AF.Silu)
            yield
            proj_cm(PS[6], wf, HT, cols)
            P.act(FG[:, :], PS[6][:, :], AF.Sigmoid)
            yield
            P.ts(FG[:, :], FG[:, :], COL[:, 9:10], COL[:, 8:9], ALU.mult, ALU.add)
            yield
            P.act(E, FG[:, :], AF.Ln)
            yield
            P.scan(G[:, :], k32("scanm"), E)
            P.ts(FG[:, :], FG[:, :], -1.0, 1.0, ALU.mult, ALU.add)
            yield
            G3 = G[:, :].rearrange("p (n c) -> p n c", c=64)
            DG3 = DG[:, :].rearrange("p (n c) -> p n c", c=64)
            P.tt(DG3, G3, G3[:, :, 32:33].to_broadcast([128, 8, 64]), ALU.subtract)
            yield
            P.act(E, DG[:, :], AF.Exp)
            yield
            P.tt(QG[:, :], QS[:, :], E, ALU.mult)
            yield
            P.act(E, DG[:, :], AF.Exp, scale=-1.0)
            yield
            P.tt(KG[:, :], FG[:, :], E, ALU.mult)
            yield
            P.act(E, G[:, :], AF.Exp)
            yield
            P.tt(QD[:, :], QS[:, :], E, ALU.mult)
            P.tt(DG3, G3[:, :, 63:64].to_broadcast([128, 8, 64]), G3, ALU.subtract)
            yield
            P.act(E, DG[:, :], AF.Exp)
            yield
            P.tt(KDT[:, :], FG[:, :], E, ALU.mult)
            P.act(DLH[:, 0:8], G3[:, :, 63], AF.Exp)
            yield

        def tile_pre(n, t):
            gt = 4 * n + t
            tc = slice(t * 128, (t + 1) * 128)
            ATM, VZ, KD0_, KD1_, QGZ = TSETS[gt % 2]
            KD = [KD0_, KD1_]
            QG, KG, QD, KDT = HSET[n % 2]
            pst = PS[7]
            psa = PS[gt % 2]
            pss = PS[2 + gt % 2]
            P.tr(pst[:, 0:64].bitcast(BF16), KDT[:, tc], k16("ident"))
            P.tt(QGZ[:, :].rearrange("p (h c) -> p h c", h=2),
                 QG[:, tc].unsqueeze(1).to_broadcast([128, 2, 128]),
                 k16("hm2").unsqueeze(2).to_broadcast([128, 2, 128]), ALU.mult)
            for hp in range(2):
                P.cp(VZ[:, hp * 128 + hp * 64: hp * 128 + hp * 64 + 64], VH[:, gt, hp * 64:(hp + 1) * 64], eng="act")
            for hf in range(2):
                P.ts(KD[hf][:, 0:128], pst[:, 0:64].bitcast(BF16), k32("rm")[:, hf:hf + 1], None, ALU.mult)
            yield
            for hf in range(2):
                M = 64 if hf == 0 else 128
                for hp in range(2):
                    P.mm(psa[0:M, hp * 128 + hf * 64: hp * 128 + hf * 64 + 64],
                         KG[:, t * 128:t * 128 + M], QGZ[:, hp * 128 + hf * 64: hp * 128 + hf * 64 + 64])
            for hf in range(2):
                for hp in range(2):
                    P.mm(pss[:, hf * 128:(hf + 1) * 128], KD[hf][:, 0:128], VZ[:, hp * 128:(hp + 1) * 128],
                         start=(hp == 0), stop=(hp == 1))
            yield
            A3 = ATM[:, :].rearrange("p (h c) -> p h c", h=2)
            for hf in range(2):
                ic = slice(hf * 64, hf * 64 + 64)
                rows = slice(hf * 64, hf * 64 + 64)
                P.tt(A3[rows, :, ic], psa[:, 0:256].rearrange("p (h c) -> p h c", h=2)[rows, :, ic],
                     k16("caus01")[rows, ic].unsqueeze(1).to_broadcast([64, 2, 64]), ALU.mult)
            yield

        def tile_chain(n, t, pso):
            gt = 4 * n + t
            ATM, VZ, KD0_, KD1_, QGZ = TSETS[gt % 2]
            QG, KG, QD, KDT = HSET[n % 2]
            DLH = DLHs[n % 2]
            pss = PS[2 + gt % 2]
            A3 = ATM[:, :].rearrange("p (h c) -> p h c", h=2)
            for hf in range(2):
                ic = slice(hf * 64, hf * 64 + 64)
                oc = slice(t * 128 + hf * 64, t * 128 + hf * 64 + 64)
                cg = gt * 2 + hf
                P.mm(pso[:, oc], SSBs[cg % 2][:, :], QD[:, oc], start=True, stop=False)
                for hp in range(2):
                    P.mm(pso[:, oc], VZ[:, hp * 128:(hp + 1) * 128], A3[:, hp, ic], start=False, stop=(hp == 1))
                yield
                ch = t * 2 + hf
                for hp in range(2):
                    r = slice(hp * 64, hp * 64 + 64)
                    P.stt(SS[r, r], SS[r, r], DLH[r, ch:ch + 1], pss[r, hf * 128 + hp * 64:hf * 128 + hp * 64 + 64], ALU.mult, ALU.add)
                yield
                P.cp(SSBs[(cg + 1) % 2][:, :], SS[:, :], eng="act")
                yield
            if t == 3:
                cols = slice(n * 512, (n + 1) * 512)
                head_norm_gate(l, pso, par(l, "hgrng"), Y[:, yblk, cols])

        def step(g_):
            try:
                next(g_)
                return True
            except StopIteration:
                return False

        pso = PS[4]
        for _ in block_prep(0):
            pass
        prev = None
        bg = None
        for n in range(NB):
            for t in range(4):
                if t == 0:
                    if bg is not None:
                        for _ in bg:
                            pass
                    bg = block_prep(n + 1) if n + 1 < NB else None
                cur = tile_pre(n, t)
                alive = True
                while alive:
                    alive = step(cur)
                    if prev is not None and not step(prev):
                        prev = None
                    if t >= 1 and bg is not None and not step(bg):
                        bg = None
                if prev is not None:
                    for _ in prev:
                        pass
                prev = tile_chain(n, t, pso)
        for _ in prev:
            pass

    def gdn(l, p):
        yblk = p
        wz = load_w(l, ("gdn", "z", p))
        wab = load_w(l, ("gdn", "tm", p))
        wq = load_w(l, ("gdn", "q", p))
        wk = load_w(l, ("gdn", "k", p))
        z_gate(l, wz, yblk)
        AB, GR, BETA, G, GL, EG, BEG, EGLG, NEGG, TMP = TOK
        psab = PS[7]
        for t in range(NT):
            for k in range(8):
                P.mm(psab[:, t * 4:t * 4 + 4], HT[:, k, t * 128:(t + 1) * 128], wab[:, k, 0:4], start=(k == 0), stop=(k == 7))
        P.cp(AB[:, :], psab[:, 0:NT * 4])
        wv = load_w(l, ("gdn", "v", p))
        AB3 = AB[:, :].rearrange("p (t c) -> p t c", c=4)
        GR3 = GR[:, 0:NT * 2].rearrange("p (t c) -> p t c", c=2)
        dtb = par(l, "dtb", 2 * p, 2).unsqueeze(1).to_broadcast([128, NT, 2])
        P.tt(GR3, AB3[:, :, 0:2], dtb, ALU.add)
        T3 = TMP[:, 0:NT * 2].rearrange("p (t c) -> p t c", c=2)
        P.ts(T3, GR3, -30.0, 0.0, ALU.add, ALU.max)
        P.ts(GR3, GR3, 30.0, None, ALU.min)
        P.act(GR[:, 0:NT * 2], GR[:, 0:NT * 2], AF.Exp)
        P.act(GR[:, 0:NT * 2], GR[:, 0:NT * 2], AF.Ln, bias=1.0)
        P.tt(GR3, GR3, T3, ALU.add)
        P.act(COL[:, 10:12], par(l, "alog", 2 * p, 2), AF.Exp)
        P.ts(COL[:, 10:12], COL[:, 10:12], -1.0, None, ALU.mult)
        P.tt(GR3, GR3, COL[:, 10:12].unsqueeze(1).to_broadcast([128, NT, 2]), ALU.mult)
        B3 = BETA[:, 0:NT * 2].rearrange("p (t c) -> p t c", c=2)
        P.act(B3, AB3[:, :, 2:4], AF.Sigmoid)
        n2 = NT * 2
        P.mm(PS[7][:, 0:n2], k32("blktri"), GR[:, 0:n2])
        P.cp(G[:, 0:n2], PS[7][:, 0:n2])
        P.mm(PS[6][:, 0:n2], k32("blk64"), GR[:, 0:n2])
        P.cp(GL[:, 0:n2], PS[6][:, 0:n2])
        P.act(EG[:, 0:n2], G[:, 0:n2], AF.Exp)
        P.tt(BEG[:, 0:n2], EG[:, 0:n2], BETA[:, 0:n2], ALU.mult)
        P.tt(EGLG[:, 0:n2], GL[:, 0:n2], G[:, 0:n2], ALU.subtract)
        P.act(EGLG[:, 0:n2], EGLG[:, 0:n2], AF.Exp)
        P.ts(NEGG[:, 0:n2], G[:, 0:n2], -1.0, None, ALU.mult)
        REP = SMF[0]
        psd = PS[6]
        for t in range(NT):
            P.cp(REP[:, 0:128].rearrange("p (h c) -> p h c", h=2),
                 GL[:, t * 2:t * 2 + 2].unsqueeze(2).to_broadcast([128, 2, 64]))
            P.mm(psd[:, 256 + t * 2:256 + t * 2 + 2], REP[:, 0:128], k32("e2"))
        P.act(DL[:, 0:n2], psd[:, 256:256 + n2], AF.Exp)
        P.memset(SS[:, :], 0.0)
        P.memset(SSB[:, :], 0.0)
        VNZ = SMB[0]
        P.memset(VNZ[:, :], 0.0)
        KBGZ = SMB[1]
        P.memset(KBGZ[:, :], 0.0)
        KD0 = SMB[2]
        KTb = KT if KTs is None else KTs
        PC = [VB[:, 0:515], VB[:, 520:1035], KTb[:, 0:1030].bitcast(F32)]
        for b_ in PC:
            P.memset(b_[:, 0:3], 0.0)
        DIAG = []
        for bi_ in range(2):
            for j_ in range(4):
                dst_ = wab[:, bi_ * 4 + j_, :]
                P.ts(dst_, k16("ident"), par(l, "conv", (bi_ * 2 + p) * 4 + j_), None, ALU.mult, eng="pool")
                DIAG.append(dst_)
        ACC, CQ, SQ, RS = F[0], F[1], F[2], F[3]
        QN, KN, VT = H[0], H[1], H[2]
        convw = lambda blk, j: par(l, "conv", (blk * 2 + p) * 4 + j)
        hmb = k16("hm2").unsqueeze(2).to_broadcast([128, 2, 128])
        idb = k16("ident").unsqueeze(1).to_broadcast([128, 2, 128])

        def v3(x):
            return x[:, :].rearrange("p (h c) -> p h c", h=2)

        def prepinv(n, t, si):
            gt = 4 * n + t
            tc = slice(t * 128, (t + 1) * 128)
            KNZ, QNZ, D, DT, A, AQT, QD, AL, IZ, TQ, EGB = SMB[3 + 11 * si: 3 + 11 * si + 11]
            Tm, Tt = TTS[si][:, 0:256], TTS[si][:, 256:512]
            REPg = SMF[si]
            psGZ, psK, psT = PS[0 + si], PS[2 + si], PS[5 + si]
            P.tt(v3(KNZ), KN[:, tc].unsqueeze(1).to_broadcast([128, 2, 128]), hmb, ALU.mult)
            P.tt(v3(QNZ), QN[:, tc].unsqueeze(1).to_broadcast([128, 2, 128]), hmb, ALU.mult)
            P.cp(v3(REPg), GR[:, gt * 2:gt * 2 + 2].unsqueeze(2).to_broadcast([128, 2, 128]))
            yield
            for hp in range(2):
                hs = slice(hp * 128, (hp + 1) * 128)
                P.mm(psGZ[:, hs], REPg[:, hs], k32("blktri"))
                P.mm(psK[:, hs], KN[:, tc], KNZ[:, hs])
                P.mm(psK[:, 256 + hp * 128:256 + (hp + 1) * 128], KN[:, tc], QNZ[:, hs])
            yield
            for hp in range(2):
                hs = slice(hp * 128, (hp + 1) * 128)
                P.act(D[:, hs], psGZ[:, hs], AF.Exp, bias=G[:, gt * 2 + hp:gt * 2 + hp + 1], scale=-1.0)
                P.act(DT[:, hs], psGZ[:, hs], AF.Exp, bias=NEGG[:, gt * 2 + hp:gt * 2 + hp + 1], scale=1.0)
            P.act(EGB[:, :], psGZ[:, 0:256], AF.Exp)
            yield
            P.stt(v3(D), v3(D), 1e30, k16("m01").unsqueeze(1).to_broadcast([128, 2, 128]), ALU.min, ALU.mult)
            P.stt(v3(DT), v3(DT), 1e30, k16("m01T").unsqueeze(1).to_broadcast([128, 2, 128]), ALU.min, ALU.mult)
            yield
            for hp in range(2):
                hs = slice(hp * 128, (hp + 1) * 128)
                P.stt(A[:, hs], psK[:, hs], BETA[:, gt * 2 + hp:gt * 2 + hp + 1], D[:, hs], ALU.mult, ALU.mult)
            P.tt(AQT[:, :], psK[:, 256:512], DT[:, :], ALU.mult)
            P.tt(TQ[:, :], QNZ[:, :], EGB[:, :], ALU.mult)
            P.tt(QD[:, 0:128], TQ[:, 0:128], TQ[:, 128:256], ALU.add)
            yield
            for lv in range(6):
                lvm = k16("lvl")[:, lv * 128:(lv + 1) * 128].unsqueeze(1).to_broadcast([128, 2, 128])
                P.tt(v3(AL), v3(A), lvm, ALU.mult)
                yield
                if lv == 0:
                    P.stt(v3(Tm), v3(AL), -1.0, idb, ALU.mult, ALU.add)
                    for hp in range(2):
                        hs = slice(hp * 128, (hp + 1) * 128)
                        P.mm(psGZ[:, 256 + hp * 128:256 + (hp + 1) * 128], AL[:, hs], k16("ident"))
                    yield
                    P.stt(v3(Tt), psGZ[:, 256:512].rearrange("p (h c) -> p h c", h=2), -1.0, idb, ALU.mult, ALU.add)
                    yield
                    continue
                for hp in range(2):
                    hs = slice(hp * 128, (hp + 1) * 128)
                    P.mm(psGZ[:, 256 + hp * 128:256 + (hp + 1) * 128], AL[:, hs], Tt[:, hs])
                yield
                P.stt(v3(IZ), psGZ[:, 256:512].rearrange("p (h c) -> p h c", h=2), -1.0, idb, ALU.mult, ALU.add)
                yield
                for hp in range(2):
                    hs = slice(hp * 128, (hp + 1) * 128)
                    if lv < 5:
                        P.mm(psT[:, hs], IZ[:, hs], Tm[:, hs])
                    P.mm(psT[:, 256 + hp * 128:256 + (hp + 1) * 128], Tm[:, hs], IZ[:, hs])
                yield
                if lv < 5:
                    P.cp(TTS[si][:, :], psT[:, :], eng="act")
                else:
                    P.cp(Tt[:, :], psT[:, 256:512], eng="act")
                yield

        WT2 = [H[6], H[7]]
        U2 = [F[4], F[5]]
        KDs = [[KD0, H[3]], [H[8], H[9]]]
        AQT2 = [SMB[25], SMB[26]]
        QD2 = [SMB[27], SMB[28]]

        def post(n, t, si):
            gt = 4 * n + t
            tc = slice(t * 128, (t + 1) * 128)
            KNZ, QNZ, D, DT, A, AQT, QD, AL, IZ, TQ, EGB = SMB[3 + 11 * si: 3 + 11 * si + 11]
            Tm, Tt = TTS[si][:, 0:256], TTS[si][:, 256:512]
            pst = PS[2 + si]
            P.tr(pst[:, 0:64].bitcast(BF16), KN[:, tc], k16("ident"))
            P.tr(pst[:, 64:128].bitcast(BF16), VT[:, tc], k16("ident"))
            kt_ = pst[:, 0:64].bitcast(BF16)
            vt_ = pst[:, 64:128].bitcast(BF16)
            KD = KDs[si]
            VBt = H[4]
            KDall = H[5]
            P.cp(AQT2[si][:, :], AQT[:, :], eng="pool")
            P.cp(QD2[si][:, 0:128], QD[:, 0:128], eng="pool")
            yield
            for hp in range(2):
                cs = slice(hp * 64, (hp + 1) * 64)
                P.ts(KBGZ[:, hp * 128 + hp * 64: hp * 128 + hp * 64 + 64], kt_[:, cs], BEG[:, gt * 2 + hp:gt * 2 + hp + 1], None, ALU.mult)
                P.ts(KDall[:, cs], kt_[:, cs], EGLG[:, gt * 2 + hp:gt * 2 + hp + 1], None, ALU.mult)
                P.ts(VBt[:, cs], vt_[:, cs], BETA[:, gt * 2 + hp:gt * 2 + hp + 1], None, ALU.mult)
            for hf in range(2):
                P.ts(KD[hf][:, 0:128], KDall[:, 0:128], k32("rm")[:, hf:hf + 1], None, ALU.mult, eng="pool")
            for hp in range(2):
                P.mm(pst[:, 128:256], KBGZ[:, hp * 128:(hp + 1) * 128], Tt[:, hp * 128:(hp + 1) * 128], start=(hp == 0), stop=(hp == 1))
            for hp in range(2):
                P.mm(pst[:, 256 + hp * 64:256 + (hp + 1) * 64], Tt[:, hp * 128:(hp + 1) * 128], VBt[:, hp * 64:(hp + 1) * 64])
            yield
            P.cp(WT2[si][:, 0:128], pst[:, 128:256], eng="act")
            P.cp(U2[si][:, 0:128], pst[:, 256:384], eng="act")
            yield

        def scan_pair(n, tp, pso):
            for si in range(2):
                t = 2 * tp + si
                gt = 4 * n + t
                WT, U, KD, AQT, QD = WT2[si], U2[si], KDs[si], AQT2[si], QD2[si]
                for hf in range(2):
                    rows = slice(hf * 64, hf * 64 + 64)
                    ic = slice(hf * 64, hf * 64 + 64)
                    M = 64 if hf == 0 else 128
                    psws = PS[7]
                    P.mm(psws[0:M, 0:128], WT[:, 0:M], SSB[:, :])
                    yield
                    for hp in range(2):
                        cs = slice(hp * 64, (hp + 1) * 64)
                        P.tt(VNZ[rows, hp * 128 + hp * 64: hp * 128 + hp * 64 + 64], U[rows, cs], psws[rows, cs], ALU.subtract)
                    yield
                    oc = slice(t * 128 + hf * 64, t * 128 + hf * 64 + 64)
                    P.mm(pso[:, oc], SSB[:, :], QD[:, ic], start=True, stop=False)
                    for hp in range(2):
                        P.mm(pso[:, oc], VNZ[:, hp * 128:(hp + 1) * 128], AQT[:, hp * 128 + hf * 64: hp * 128 + hf * 64 + 64],
                             start=False, stop=(hp == 1))
                    pss = PS[7]
                    for hp in range(2):
                        P.mm(pss[:, 128:256], KD[hf][:, 0:128], VNZ[:, hp * 128:(hp + 1) * 128], start=(hp == 0), stop=(hp == 1))
                    yield
                    ch = gt * 2 + hf
                    for hp in range(2):
                        r = slice(hp * 64, hp * 64 + 64)
                        P.stt(SS[r, r], SS[r, r], DL[r, ch:ch + 1], pss[r, 128 + hp * 64:128 + hp * 64 + 64], ALU.mult, ALU.add)
                    yield
                    P.cp(SSB[:, :], SS[:, :], eng="act")
                    yield
            if tp == 1:
                cols = slice(n * 512, (n + 1) * 512)
                head_norm_gate(l, pso, par(l, "gdng"), Y[:, yblk, cols])

        def chain(*gs):
            for g_ in gs:
                yield from g_

        pso = PS[4]
        prev = None
        for n in range(NB):
            cols = slice(n * 512, (n + 1) * 512)
            def proj_blk(bi, w_):
                ps = PS[5 + (bi % 2)]
                proj_cm(ps, w_, HT, cols)
                P.cp(PC[bi][:, 3:515], ps[:, :], eng="act")
            proj_blk(0, wq)
            proj_blk(1, wk)
            for bi, (w_, dst) in enumerate(((wq, QN), (wk, KN), (wv, VT))):
                pc = PC[bi]
                if bi == 2:
                    proj_blk(2, wv)
                if bi == 2:
                    P.ts(ACC[:, :], pc[:, 0:512], convw(bi, 0), None, ALU.mult)
                    for j in range(1, 4):
                        P.stt(ACC[:, :], pc[:, j:j + 512], convw(bi, j), ACC[:, :], ALU.mult, ALU.add)
                    P.cp(pc[:, 0:3], pc[:, 512:515])
                    P.act(VT[:, :], ACC[:, :], AF.Silu)
                else:
                    psc = PS[bi % 2]
                    for j in range(4):
                        P.mm(psc[:, :], DIAG[bi * 4 + j], pc[:, j:j + 512], start=(j == 0), stop=(j == 3))
                    P.cp(pc[:, 0:3], pc[:, 512:515], eng="pool")
                    P.act(CQ[:, :], psc[:, :], AF.Silu)
                    P.act(SQ[:, :], CQ[:, :], AF.Square)
                    P.mm(PS[7][:, :], k32("blk64"), SQ[:, :])
                    rsqrt_from(RS[:, :], PS[7][:, :], 1.0, EPS)
                    P.stt(dst[:, :], CQ[:, :], float(HD ** -0.5) if bi == 0 else 1.0, RS[:, :], ALU.mult, ALU.mult)
            for tp in range(2):
                gens = [chain(prepinv(n, 2 * tp + si, si), post(n, 2 * tp + si, si)) for si in range(2)]
                alive = list(gens)
                while alive:
                    for g_ in list(alive):
                        try:
                            next(g_)
                        except StopIteration:
                            alive.remove(g_)
                    if prev is not None:
                        try:
                            next(prev)
                        except StopIteration:
                            prev = None
                if prev is not None:
                    for _ in prev:
                        pass
                prev = scan_pair(n, tp, pso)
        for _ in prev:
            pass

    ONESB = P.sb("ONESB", [128, 128], BF16)
    P.memset(ONESB[:, :], 1.0)

    def outproj(l):
        KTb = KT if KTs is None else KTs
        OB = [F[i][:, :] for i in range(7)] + [KTb[:, 0:1024].bitcast(F32)]
        wviews = []
        for d in range(8):
            if d < 4:
                w_ = load_w(l, ("out", "w", d))
                wviews.append([w_[:, k, :] for k in range(8)])
            else:
                ha, hb = H[2 + 2 * (d - 4)], H[3 + 2 * (d - 4)]
                src = wD[l, CMB[("out", "w", d)]]
                P.dma(ha[:, :], src[:, 0:512], q="pool")
                P.dma(hb[:, :], src[:, 512:1024], q="pool")
                wviews.append([(ha if k < 4 else hb)[:, (k % 4) * 128:(k % 4 + 1) * 128] for k in range(8)])
        RSB = [VB[:, 0:1024].bitcast(F32), VB[:, 1024:2048].bitcast(F32)]

        def proj_d(n, d):
            cols = slice(n * 512, (n + 1) * 512)
            ps = PS[5 + (d % 2)]
            for k in range(8):
                P.mm(ps[:, :], wviews[d][k], Y[:, k, cols], start=(k == 0), stop=(k == 7))
            P.cp(OB[d], ps[:, :], eng="act")
            sq = H[d % 2]
            P.act(sq[:, :], ps[:, :], AF.Square)
            while pending:
                pending.pop(0)()
            pending.append(lambda n=n, d=d, sq=sq: P.mm(PS[n % 2][:, :], ONESB[:, :], sq[:, :],
                                                       start=(d == 0), stop=(d == 7)))

        pending = []

        def post_d(n, d):
            cols = slice(n * 512, (n + 1) * 512)
            rs = RSB[n % 2]
            P.stt(OB[d], OB[d], par(l, "postg", d), rs, ALU.mult, ALU.mult)
            P.tt(XT[:, d, cols], XT[:, d, cols], OB[d], ALU.add)

        for d in range(8):
            proj_d(0, d)
        for n in range(NB):
            while pending:
                pending.pop(0)()
            rsqrt_from(RSB[n % 2], PS[n % 2][:, :], 1.0 / D_MODEL, EPS)
            for d in range(8):
                post_d(n, d)
                if n + 1 < NB:
                    proj_d(n + 1, d)

    for s in range(NSEQ):
        for c in range(8):
            P.dma(XT[:, c, :], xD[s, :, c * T:(c + 1) * T])
        for l in range(DEPTH):
            P.mark("prenorm s%d l%d" % (s, l))
            prenorm(l)
            P.memset(Y[:, :, :], 0.0) if (len(branches) < 4) else None
            for p in range(2):
                if "gdn" in branches:
                    P.mark("gdn s%d l%d p%d" % (s, l, p))
                    gdn(l, p)
                if "hgrn" in branches:
                    P.mark("hgrn s%d l%d p%d" % (s, l, p))
                    hgrn(l, p)
                if "moba" in branches:
                    P.mark("moba s%d l%d p%d" % (s, l, p))
                    attention(l, p, "moba")
                if "diff" in branches:
                    P.mark("diff s%d l%d p%d" % (s, l, p))
                    attention(l, p, "diff")
            if tap:
                TP = F
                for c in range(8):
                    for n in range(NB):
                        cols = slice(n * 512, (n + 1) * 512)
                        P.cp(TP[(c * NB + n) % 4][:, :], Y[:, c, cols])
                        P.dma(tapD[s, l, :, c * T + n * 512: c * T + (n + 1) * 512], TP[(c * NB + n) % 4][:, :])
            P.mark("outproj s%d l%d" % (s, l))
            outproj(l)
        for c in range(8):
            P.dma(yD[s, :, c * T:(c + 1) * T], XT[:, c, :])
    P.mark("end")
    P.final_wait("sp", [XT] + ([F[0], F[1], F[2], F[3]] if tap else []))
    P.emit()
    return nc, P


_CACHE = {}


def _prep_inputs(inp, T, nseq_total):
    x = np.asarray(inp["x"], np.float32)
    B = x.shape[0]
    xT = np.ascontiguousarray(x.reshape(B, T, 8, 128).transpose(0, 3, 2, 1)).reshape(B, 128, 8 * T)
    w = pack_weights(np.asarray(inp["w_in"], np.float32), np.asarray(inp["w_out"], np.float32))
    par = pack_params({k: np.asarray(v, np.float32) for k, v in inp.items()})
    c32, c16 = make_consts()
    return xT, w, par, c32, c16


def kernel(**inputs):
    x = np.asarray(inputs["x"])
    B, T, D = x.shape
    depth = inputs["w_in"].shape[0]
    nseq = B // N_CORES
    key = (T, nseq, depth)
    if key not in _CACHE:
        _CACHE[key] = build(T=T, NSEQ=nseq, DEPTH=depth)[0]
    nc = _CACHE[key]
    xT, w, par, c32, c16 = _prep_inputs(inputs, T, B)
    in_maps = []
    for c in range(N_CORES):
        in_maps.append({"x": xT[c * nseq:(c + 1) * nseq], "w": w, "c32": c32, "c16": c16, "par": par})
    res = run_bass_kernel_spmd(nc, in_maps, core_ids=list(range(N_CORES)))
    outs = []
    for c in range(N_CORES):
        yT = np.asarray(res.results[c]["y"]).reshape(nseq, 128, 8, T)
        outs.append(yT.transpose(0, 3, 2, 1).reshape(nseq, T, D))
    return np.concatenate(outs, axis=0).astype(np.float32)
```
